# Optimizing a Trainium2 kernel written in Bass

```python
import math
import jax, jax.numpy as jnp
from jax import lax
import numpy as np


D_MODEL = 1024
BATCH = 2
SEQ = 8192
DEPTH = 4

HEAD_DIM = 64
N_HEADS = D_MODEL // HEAD_DIM
SWA_Q_HEADS = N_HEADS // 2
SWA_KV_HEADS = max(1, SWA_Q_HEADS // 4)
MOBA_HEADS = N_HEADS - SWA_Q_HEADS
SWA_WIDTH = SWA_Q_HEADS * HEAD_DIM
SWA_KV_WIDTH = SWA_KV_HEADS * HEAD_DIM
MOBA_WIDTH = MOBA_HEADS * HEAD_DIM
MIX_WIDTH = SWA_WIDTH + MOBA_WIDTH
IN_COLS = SWA_WIDTH + 2 * SWA_KV_WIDTH + 3 * MOBA_WIDTH
IN_SPLITS = (SWA_WIDTH,
             SWA_WIDTH + SWA_KV_WIDTH,
             SWA_WIDTH + 2 * SWA_KV_WIDTH,
             SWA_WIDTH + 2 * SWA_KV_WIDTH + MOBA_WIDTH,
             SWA_WIDTH + 2 * SWA_KV_WIDTH + 2 * MOBA_WIDTH)
SWA_WINDOW = 128
SWA_BLOCK = 128
MOBA_BLOCK = 256
MOBA_TOPK = 3
MOBA_Q_CHUNK = 128
T5_BUCKETS = 32
T5_MAX_DIST = 128
D_FF = ((8 * D_MODEL // 3 + 255) // 256) * 256
N_MOD = 9
EPS = 1e-6
NEG = -1e30

kernel_name = 'hybrid_swa_moba_macaron_trunk'


def rmsnorm(x, g):
    xf = x.astype(jnp.float32)
    y = xf * lax.rsqrt(jnp.mean(xf * xf, axis=-1, keepdims=True) + EPS)
    return (y * g.astype(jnp.float32)).astype(x.dtype)


def t5_bucket(dist):
    n = jnp.maximum(dist, 0)
    max_exact = T5_BUCKETS // 2
    nf = jnp.maximum(n, 1).astype(jnp.float32)
    large = max_exact + (jnp.log(nf / max_exact) / math.log(T5_MAX_DIST / max_exact)
                         * (T5_BUCKETS - max_exact)).astype(jnp.int32)
    large = jnp.minimum(large, T5_BUCKETS - 1)
    return jnp.where(n < max_exact, n, large)


def swiglu(h, w_gate, w_up, w_down):
    return (jax.nn.silu(h @ w_gate) * (h @ w_up)) @ w_down


def swa_attention(q, k, v, sinks, table):
    B_, S, Hq, dh = q.shape
    Hkv = k.shape[2]
    G = Hq // Hkv
    L = SWA_BLOCK
    nblk = S // L
    qb = q.reshape(B_, nblk, L, Hkv, G, dh)
    kb = k.reshape(B_, nblk, L, Hkv, dh)
    vb = v.reshape(B_, nblk, L, Hkv, dh)
    pad = ((0, 0), (1, 0), (0, 0), (0, 0), (0, 0))
    kw = jnp.concatenate([jnp.pad(kb, pad)[:, :-1], kb], axis=2)
    vw = jnp.concatenate([jnp.pad(vb, pad)[:, :-1], vb], axis=2)
    s = jnp.einsum('bnqhgd,bnkhd->bnhgqk', qb, kw).astype(jnp.float32) * (dh ** -0.5)
    qi = jnp.arange(L)[:, None]
    kj = jnp.arange(2 * L)[None, :]
    dist = qi + L - kj
    bias = table.T[:, t5_bucket(dist)].astype(jnp.float32).reshape(Hkv, G, L, 2 * L)
    kpos = jnp.arange(nblk)[:, None, None] * L - L + kj[None]
    valid = (dist >= 0)[None] & (dist < SWA_WINDOW)[None] & (kpos >= 0)
    s = jnp.where(valid[None, :, None, None], s + bias, NEG)
    sink = jnp.broadcast_to(sinks.astype(jnp.float32).reshape(1, 1, Hkv, G, 1, 1),
                            s.shape[:-1] + (1,))
    p = jax.nn.softmax(jnp.concatenate([s, sink], axis=-1), axis=-1)[..., :-1]
    o = jnp.einsum('bnhgqk,bnkhd->bnqhgd', p.astype(v.dtype), vw)
    return o.reshape(B_, S, Hq, dh)


def moba_attention(q, k, v, table):
    B_, S, H, dh = q.shape
    L = MOBA_BLOCK
    C = MOBA_Q_CHUNK
    nb = -(-S // L)
    Sp = nb * L
    padw = ((0, 0), (0, Sp - S), (0, 0), (0, 0))
    q, k, v = jnp.pad(q, padw), jnp.pad(k, padw), jnp.pad(v, padw)
    kb = k.reshape(B_, nb, L, H, dh).transpose(0, 3, 1, 2, 4)
    vb = v.reshape(B_, nb, L, H, dh).transpose(0, 3, 1, 2, 4)
    kmean = jnp.mean(kb, axis=3)
    topk = min(MOBA_TOPK, nb)
    nchunk = Sp // C
    qc = q.reshape(B_, nchunk, C, H, dh).transpose(1, 0, 3, 2, 4)
    scale = dh ** -0.5
    tableT = table.T.astype(jnp.float32)
    bi = jnp.arange(B_)[:, None, None, None]
    hi = jnp.arange(H)[None, :, None, None]
    hi5 = jnp.arange(H)[None, :, None, None, None]

    def chunk_fn(args):
        qblk, ci = args
        q_pos = ci * C + jnp.arange(C)
        cur = (ci * C) // L
        gate = jnp.einsum('bhcd,bhnd->bhcn', qblk, kmean)
        gate = jnp.where(jnp.arange(nb) < cur, gate, NEG)
        _, sel = lax.top_k(gate, topk)
        sel_ok = jnp.arange(topk) < cur
        k_sel = kb[bi, hi, sel]
        v_sel = vb[bi, hi, sel].reshape(B_, H, C, topk * L, dh)
        s_sel = jnp.einsum('bhcd,bhctld->bhctl', qblk, k_sel).astype(jnp.float32) * scale
        k_pos_sel = sel[..., None] * L + jnp.arange(L)
        dist_sel = q_pos[None, None, :, None, None] - k_pos_sel
        s_sel = s_sel + tableT[hi5, t5_bucket(dist_sel)]
        s_sel = jnp.where(sel_ok[None, None, None, :, None], s_sel, NEG)
        s_sel = s_sel.reshape(B_, H, C, topk * L)
        k_own = lax.dynamic_index_in_dim(kb, cur, axis=2, keepdims=False)
        v_own = lax.dynamic_index_in_dim(vb, cur, axis=2, keepdims=False)
        dist_own = q_pos[:, None] - (cur * L + jnp.arange(L))[None, :]
        s_own = jnp.einsum('bhcd,bhld->bhcl', qblk, k_own).astype(jnp.float32) * scale
        s_own = jnp.where(dist_own >= 0, s_own + tableT[:, t5_bucket(dist_own)], NEG)
        p = jax.nn.softmax(jnp.concatenate([s_sel, s_own], axis=-1), axis=-1).astype(v.dtype)
        o = (jnp.einsum('bhck,bhckd->bhcd', p[..., :topk * L], v_sel)
             + jnp.einsum('bhcl,bhld->bhcd', p[..., topk * L:], v_own))
        return o

    out = lax.map(chunk_fn, (qc, jnp.arange(nchunk)))
    out = out.transpose(1, 0, 3, 2, 4).reshape(B_, Sp, H, dh)
    return out[:, :S]


def hybrid_mixer(h, w_in, b_in, w_out, sinks, group_gain, rel_bias):
    B_, S, _ = h.shape
    proj = h @ w_in + b_in
    qa, ka, va, qb, kb, vb = jnp.split(proj, IN_SPLITS, axis=-1)
    qa = qa.reshape(B_, S, SWA_Q_HEADS, HEAD_DIM)
    ka = ka.reshape(B_, S, SWA_KV_HEADS, HEAD_DIM)
    va = va.reshape(B_, S, SWA_KV_HEADS, HEAD_DIM)
    qb = qb.reshape(B_, S, MOBA_HEADS, HEAD_DIM)
    kb = kb.reshape(B_, S, MOBA_HEADS, HEAD_DIM)
    vb = vb.reshape(B_, S, MOBA_HEADS, HEAD_DIM)
    ya = swa_attention(qa, ka, va, sinks, rel_bias[:, :SWA_Q_HEADS]).reshape(B_, S, SWA_WIDTH)
    yb = moba_attention(qb, kb, vb, rel_bias[:, SWA_Q_HEADS:]).reshape(B_, S, MOBA_WIDTH)
    y = jnp.concatenate([rmsnorm(ya, group_gain[:SWA_WIDTH]),
                         rmsnorm(yb, group_gain[SWA_WIDTH:])], axis=-1)
    return y @ w_out


def setup_inputs(seed: int = 0) -> dict:
    key = jax.random.key(seed)
    ks = jax.random.split(key, 16)
    f = jnp.float32

    def w(k, shape, fan_in, gain=1.0):
        return jax.random.normal(k, shape, f) * (gain * fan_in ** -0.5)

    return {
        'x': jax.random.normal(ks[0], (BATCH, SEQ, D_MODEL), f),
        'c': jax.random.normal(ks[1], (BATCH, D_MODEL), f),
        'rel_bias': 0.5 * jax.random.normal(ks[2], (T5_BUCKETS, N_HEADS), f),
        'ada_w': w(ks[3], (DEPTH, D_MODEL, N_MOD * D_MODEL), D_MODEL, 0.5),
        'ada_b': 0.01 * jax.random.normal(ks[4], (DEPTH, N_MOD * D_MODEL), f),
        'norm_pre': 1.0 + 0.05 * jax.random.normal(ks[5], (DEPTH, 3, D_MODEL), f),
        'norm_post': 1.0 + 0.05 * jax.random.normal(ks[6], (DEPTH, 3, D_MODEL), f),
        'ffn_w_gate': w(ks[7], (DEPTH, 2, D_MODEL, D_FF), D_MODEL),
        'ffn_w_up': w(ks[8], (DEPTH, 2, D_MODEL, D_FF), D_MODEL),
        'ffn_w_down': w(ks[9], (DEPTH, 2, D_FF, D_MODEL), D_FF),
        'mix_w_in': w(ks[10], (DEPTH, D_MODEL, IN_COLS), D_MODEL),
        'mix_b_in': 0.01 * jax.random.normal(ks[11], (DEPTH, IN_COLS), f),
        'mix_w_out': w(ks[12], (DEPTH, MIX_WIDTH, D_MODEL), MIX_WIDTH),
        'attn_sinks': jax.random.normal(ks[13], (DEPTH, SWA_Q_HEADS), f),
        'group_gain': 1.0 + 0.05 * jax.random.normal(ks[14], (DEPTH, MIX_WIDTH), f),
    }


def reference(x, c, rel_bias, ada_w, ada_b, norm_pre, norm_post, ffn_w_gate, ffn_w_up,
              ffn_w_down, mix_w_in, mix_b_in, mix_w_out, attn_sinks, group_gain):
    c_act = jax.nn.silu(c)
    for l in range(DEPTH):
        mod = (c_act @ ada_w[l] + ada_b[l])[:, None, :]
        sh1, sc1, g1, sh2, sc2, g2, sh3, sc3, g3 = jnp.split(mod, N_MOD, axis=-1)
        h = rmsnorm(x, norm_pre[l, 0]) * (1.0 + sc1) + sh1
        y = swiglu(h, ffn_w_gate[l, 0], ffn_w_up[l, 0], ffn_w_down[l, 0])
        x = x + 0.5 * g1 * rmsnorm(y, norm_post[l, 0])
        h = rmsnorm(x, norm_pre[l, 1]) * (1.0 + sc2) + sh2
        y = hybrid_mixer(h, mix_w_in[l], mix_b_in[l], mix_w_out[l], attn_sinks[l],
                         group_gain[l], rel_bias)
        x = x + g2 * rmsnorm(y, norm_post[l, 1])
        h = rmsnorm(x, norm_pre[l, 2]) * (1.0 + sc3) + sh3
        y = swiglu(h, ffn_w_gate[l, 1], ffn_w_up[l, 1], ffn_w_down[l, 1])
        x = x + 0.5 * g3 * rmsnorm(y, norm_post[l, 2])
    return x
```

```python
import math
import numpy as np
from contextlib import ExitStack
import concourse.bass as bass
import concourse.mybir as mybir
from concourse.bass_utils import run_bass_kernel_spmd

F32 = mybir.dt.float32
BF16 = mybir.dt.bfloat16
ALU = mybir.AluOpType
AF = mybir.ActivationFunctionType
AX = mybir.AxisListType

DEPTH = 4
DEBUG_CUT = 99
EPS = 1e-6
MASKV = 240000.0


class Tok:
    __slots__ = ("sem", "key", "val", "eng")

    def __init__(self, sem, key, val, eng):
        self.sem, self.key, self.val, self.eng = sem, key, val, eng


class Buf:
    def __init__(self, name):
        self.name = name
        self.w = None
        self.r = {}
        self.dsem = None
        self.dkey = None
        self.dcnt = 0


class Sched:
    def __init__(self, nc, es):
        self.nc, self.es = nc, es
        self.E = {"pe": nc.tensor, "act": nc.scalar, "dve": nc.vector, "pool": nc.gpsimd, "sp": nc.sync}
        self.sem, self.key, self.cnt = {}, {}, {}
        self.known = {e: {} for e in self.E}
        self.nsem = 0
        self.bufs = {}
        for e in self.E:
            self.new_epoch(e)

    def newsem(self, name):
        self.nsem += 1
        nm = f"{name}_{self.nsem}"
        return self.es.enter_context(self.nc.semaphore(nm)), nm

    def new_epoch(self, e):
        self.sem[e], self.key[e] = self.newsem("e" + e)
        self.cnt[e] = 0

    def B(self, name):
        b = self.bufs.get(name)
        if b is None:
            b = self.bufs[name] = Buf(name)
        return b

    def _wait(self, e, toks, skip_pe=False, defer=False):
        need = {}
        for t in toks:
            if t is None:
                continue
            if skip_pe and t.eng == "pe":
                continue
            cur = need.get(t.key)
            if cur is None or cur.val < t.val:
                need[t.key] = t
        kn = self.known[e]
        todo = [t for k, t in need.items() if kn.get(k, 0) < t.val]
        last = None
        if defer and todo:
            last = todo.pop()
            kn[last.key] = last.val
        for t in todo:
            self.E[e].wait_ge(t.sem, t.val)
            kn[t.key] = t.val
        return last

    @staticmethod
    def _deps(rd, wr):
        toks = []
        for b in rd:
            toks.append(b.w)
        for b in wr:
            toks.append(b.w)
            toks.extend(b.r.values())
        return toks

    @staticmethod
    def _record(tok, rd, wr):
        for b in rd:
            c = b.r.get(tok.key)
            if c is None or c.val < tok.val:
                b.r[tok.key] = tok
        for b in wr:
            b.w = tok
            b.r = {}

    def op(self, e, fn, rd=(), wr=(), inc=True):
        last = self._wait(e, self._deps(rd, wr), skip_pe=(e == "pe"), defer=True)
        ins = fn(self.E[e])
        if last is not None:
            ins._wait_ge(last.sem, last.val)
        if inc:
            ins.then_inc(self.sem[e], 1)
            self.cnt[e] += 1
            tok = Tok(self.sem[e], self.key[e], self.cnt[e], e)
        else:
            tok = Tok(self.sem[e], self.key[e], self.cnt[e] + 1, e)
        self._record(tok, rd, wr)
        return tok

    def dma(self, q, pairs, rd, wr, owner):
        if owner.dsem is None:
            owner.dsem, owner.dkey = self.newsem("d")
        last = self._wait(q, self._deps(rd, wr), defer=True)
        for (o, i) in pairs:
            ins = self.E[q].dma_start(out=o, in_=i)
            if last is not None:
                ins._wait_ge(last.sem, last.val)
                last = None
            ins.then_inc(owner.dsem, 16)
            owner.dcnt += 16
        tok = Tok(owner.dsem, owner.dkey, owner.dcnt, "dma")
        self._record(tok, rd, wr)
        return tok

    def allgather(self, src_ap, dst_ap, rd, wr, name):
        if not hasattr(self, "ccs"):
            self.ccs = {}
        if name not in self.ccs:
            self.ccs[name] = list(self.newsem("cc")) + [0]
        ent = self.ccs[name]
        ent[2] += 1
        sem, key = ent[0], ent[1]
        self._wait("pool", self._deps(rd, wr))
        self.nc.gpsimd.collective_compute(
            "AllGather", ALU.bypass, replica_groups=[[0, 1, 2, 3], [4, 5, 6, 7]],
            ins=[src_ap.opt()], outs=[dst_ap.opt()],
        ).then_inc(sem)
        tok = Tok(sem, key, ent[2], "cc")
        self._record(tok, rd, wr)
        return tok

    def release(self, bufs):
        toks = []
        for b in bufs:
            toks.append(b.w)
            toks.extend(b.r.values())
        for e in self.E:
            self._wait(e, toks)

    def wait_all(self, e, bufs):
        toks = []
        for b in bufs:
            toks.append(b.w)
            toks.extend(b.r.values())
        self._wait(e, toks)


class Rot:
    def __init__(self, items):
        self.items, self.i = items, 0

    def next(self):
        it = self.items[self.i % len(self.items)]
        self.i += 1
        return it


class WStream:
    def __init__(self, S, slots, srcs):
        self.S, self.slots, self.srcs = S, slots, srcs
        self.issued = 0
        self.consumed = 0

    def prefetch(self, ahead=None):
        n = len(self.slots) if ahead is None else ahead
        while self.issued < len(self.srcs) and self.issued < self.consumed + n:
            t, b = self.slots[self.issued % len(self.slots)]
            self.S.dma("pool", [(t[:], self.srcs[self.issued])], rd=[], wr=[b], owner=b)
            self.issued += 1

    def get(self):
        self.prefetch()
        it = self.slots[self.consumed % len(self.slots)]
        self.consumed += 1
        return it


def build(L=DEPTH, nsub=None):
    if nsub is None:
        nsub = 3 * L
    nc = bass.Bass("TRN2", target_bir_lowering=False)
    es = ExitStack()
    es.enter_context(nc.allow_low_precision("bf16 matmuls with fp32 accumulation"))

    def din(name, shape, dt=F32):
        return nc.dram_tensor(name, shape, dt, kind="ExternalInput").ap()

    xT_d = din("xT", [1024, 2048])
    cT_d = din("cT", [128, 8])
    wgu_d = din("wgu", [L * 2 * 22 * 128, 2048])
    wd_d = din("wd", [L * 2 * 8 * 128, 2816])
    wqk_d = din("wqk", [L * 4 * 128, 3584])
    wv_d = din("wv", [L * 128, 6144])
    bqk_d = din("bqk", [64, L * 28])
    bv_d = din("bv", [L * 128, 768])
    wout_d = din("wout", [L * 128, 8192])
    adaw_d = din("adaw", [L * 36 * 128, 2048])
    vecs_d = din("vecs", [128, L * 128])
    ident_d = din("ident", [128, 128])
    blkind_d = din("blkind", [32, 8192])
    stair_d = din("stair", [128, 128])
    gmoba_d = din("gmoba", [128, 2 * 1152])
    cfar_d = din("cfar", [128, 2])
    gswa_d = din("gswa", [128, 4 * 256])
    snk_d = din("snk", [1, L * 2])
    out_d = nc.dram_tensor("outT", [1024, 2048], F32, kind="ExternalOutput").ap()
    send1 = nc.dram_tensor("send1", [3072, 2048], BF16)
    recv1 = nc.dram_tensor("recv1", [12 * 1024, 2048], BF16)
    send2 = nc.dram_tensor("send2", [256, 8192], BF16)
    recv2 = nc.dram_tensor("recv2", [4 * 256, 8192], BF16)
    mine1 = nc.dram_tensor("mine1", [3 * 1024, 2048], BF16)
    mine2 = nc.dram_tensor("mine2", [4 * 256, 2048], BF16)

    S = Sched(nc, es)
    B = S.B

    uid = [0]

    def sb(name, shape, dt, stack=es):
        uid[0] += 1
        return stack.enter_context(nc.sbuf_tensor(f"s{uid[0]}_{name}", shape, dt))

    xT = sb("xTs", [128, 8, 2048], F32)
    xb = [[B(f"x{dc}_{tg}") for tg in range(4)] for dc in range(8)]
    ident = sb("ident", [128, 128], F32)
    ones_bf = sb("ones_bf", [128, 128], BF16)
    onesf = sb("onesf", [128, 64], F32)
    stair = sb("stair", [128, 128], F32)
    vecs = sb("vecs", [128, L * 128], F32)
    cTs = sb("cTs", [128, 8], F32)
    cact = sb("cact", [128, 8], BF16)
    modt = sb("modt", [128, L * 72], F32)
    Acf = sb("Acf", [128, L * 24], F32)
    Bcf = sb("Bcf", [128, L * 24], F32)
    epsc = sb("epsc", [128, 1], F32)
    gmoba = sb("gmoba", [128, 2 * 1152], F32)
    cfar = sb("cfar", [128, 2], F32)
    gswa = sb("gswa", [128, 4 * 256], F32)
    snk = sb("snk", [65, L * 2], F32)
    esnk = sb("esnk", [65, L * 2], F32)
    bqk = sb("bqk", [64, L * 28], F32)
    constb = B("consts")
    wgu_slots = [(sb(f"wgu{i}", [128, 2048], BF16), B(f"wgu{i}")) for i in range(3)]
    wd_slots = [(sb(f"wd{i}", [128, 2816], BF16), B(f"wd{i}")) for i in range(2)]
    adw_slots = [(sb(f"adw{i}", [128, 2048], BF16), B(f"adw{i}")) for i in range(2)]
    sq_rot = Rot([(sb(f"sq{i}", [128, 512], BF16), B(f"sq{i}")) for i in range(2)])
    t32_rot = Rot([(sb(f"t32{i}", [128, 512], F32), B(f"t32{i}")) for i in range(2)])
    sd_t, sd_b = sb("sd", [128, 512], F32), B("sd")
    rs_rot = Rot([(sb(f"rs{i}", [128, 512], F32), B(f"rs{i}")) for i in range(2)])

    pbt = [es.enter_context(nc.psum_tensor(f"pb{i}", [128, 512], F32)) for i in range(8)]
    pbb = [B(f"pb{i}") for i in range(8)]

    pid = nc.partition_id()
    gidx = pid % 4

    wgu_srcs, wd_srcs = [], []
    for l in range(L):
        for wi in range(2):
            if 3 * l + 2 * wi >= nsub:
                continue
            for ps in range(2):
                for fc in range(22):
                    r0 = ((l * 2 + wi) * 22 + fc) * 128
                    wgu_srcs.append(wgu_d[r0:r0 + 128, :])
                for dc in range(8):
                    r0 = ((l * 2 + wi) * 8 + dc) * 128
                    wd_srcs.append(wd_d[r0:r0 + 128, :])
    wgu_st = WStream(S, wgu_slots, wgu_srcs)
    wd_st = WStream(S, wd_slots, wd_srcs)

    S.dma("sp", [(xT[:, dc, :], xT_d[dc * 128:(dc + 1) * 128, :]) for dc in range(8)],
          rd=[], wr=[b for row in xb for b in row], owner=B("xload"))
    S.dma("sp", [(ident[:], ident_d[:, :]), (stair[:], stair_d[:, :]), (vecs[:], vecs_d[:, :]),
                 (cTs[:], cT_d[:, :]), (gmoba[:], gmoba_d[:, :]), (cfar[:], cfar_d[:, :]),
                 (gswa[:], gswa_d[:, :]), (snk[64:65, :], snk_d[:, :]), (bqk[:], bqk_d[:, :])],
          rd=[], wr=[constb], owner=constb)
    cb2 = B("consts2")
    S.op("dve", lambda e: e.memset(ones_bf[:], 1.0), wr=[cb2])
    S.op("dve", lambda e: e.memset(onesf[:], 1.0), wr=[cb2])
    S.op("dve", lambda e: e.memset(epsc[:], EPS), wr=[cb2])
    S.op("act", lambda e: e.activation(out=cact[:], in_=cTs[:], func=AF.Silu), rd=[constb], wr=[cb2])
    S.op("act", lambda e: e.activation(out=esnk[64:65, :], in_=snk[64:65, :], func=AF.Exp), rd=[constb], wr=[cb2])
    CB = [constb, cb2]

    def mod_task(l):
        mb = B(f"mod{l}")
        adw_st = WStream(S, adw_slots, [adaw_d[(l * 36 + hb) * 128:(l * 36 + hb + 1) * 128, :] for hb in range(36)])
        for hb in range(36):
            wt, wb = adw_st.get()
            for j4 in range(2):
                col = hb * 2 + j4
                for dc in range(8):
                    S.op("pe", lambda e, dc=dc, j4=j4, col=col: e.matmul(
                        pbt[6][:, 128 + col:129 + col], lhsT=wt[:, dc * 256 + j4 * 128: dc * 256 + (j4 + 1) * 128],
                        rhs=cact[:, dc:dc + 1], start=(dc == 0), stop=(dc == 7)),
                        rd=[wb] + CB, wr=[pbb[6]], inc=(dc == 7))
            yield
        mo = modt[:, l * 72:(l + 1) * 72]
        S.op("dve", lambda e: e.tensor_tensor(out=mo, in0=pbt[6][:, 128:200], in1=vecs[:, l * 128: l * 128 + 72], op=ALU.add),
             rd=[pbb[6]] + CB, wr=[mb])
        coef = [0.5, 1.0, 0.5]
        for s in range(3):
            a = Acf[:, (l * 3 + s) * 8:(l * 3 + s + 1) * 8]
            bb = Bcf[:, (l * 3 + s) * 8:(l * 3 + s + 1) * 8]
            sc = modt[:, l * 72 + s * 24 + 8: l * 72 + s * 24 + 16]
            gt = modt[:, l * 72 + s * 24 + 16: l * 72 + s * 24 + 24]
            npre = vecs[:, l * 128 + 72 + s * 8: l * 128 + 72 + s * 8 + 8]
            npost = vecs[:, l * 128 + 96 + s * 8: l * 128 + 96 + s * 8 + 8]
            S.op("dve", lambda e, a=a, sc=sc, npre=npre: e.scalar_tensor_tensor(
                out=a, in0=sc, scalar=1.0, in1=npre, op0=ALU.add, op1=ALU.mult), rd=[mb] + CB, wr=[mb])
            S.op("dve", lambda e, bb=bb, gt=gt, npost=npost, cf=coef[s]: e.scalar_tensor_tensor(
                out=bb, in0=gt, scalar=cf, in1=npost, op0=ALU.mult, op1=ALU.mult), rd=[mb] + CB, wr=[mb])
        yield

    def run_task(t):
        for _ in t:
            pass

    def rstd_from(ssbank_i, scale):
        S.op("act", lambda e: e.activation(out=sd_t[:], in_=pbt[ssbank_i][:, :], func=AF.Sqrt,
                                           bias=epsc[:, 0:1], scale=scale), rd=[pbb[ssbank_i]] + CB, wr=[sd_b])
        rt, rb = rs_rot.next()
        S.op("dve", lambda e: e.reciprocal(out=rt[:], in_=sd_t[:]), rd=[sd_b], wr=[rb])
        return rt, rb

    def norm_x_to_h(l, s, tg, dst_fn, dst_bufs, ssbank_i):
        mb = B(f"mod{l}")
        tok = slice(tg * 512, (tg + 1) * 512)
        for dc in range(8):
            qt, qb = sq_rot.next()
            S.op("act", lambda e, dc=dc, qt=qt: e.activation(out=qt[:], in_=xT[:, dc, tok], func=AF.Square),
                 rd=[xb[dc][tg]], wr=[qb])
            S.op("pe", lambda e, dc=dc, qt=qt: e.matmul(pbt[ssbank_i][:, :], lhsT=ones_bf[:], rhs=qt[:],
                                                         start=(dc == 0), stop=(dc == 7)),
                 rd=[qb] + CB, wr=[pbb[ssbank_i]], inc=True)
        rt, rb = rstd_from(ssbank_i, 1.0 / 1024.0)
        for dc in range(8):
            tt, tb = t32_rot.next()
            acol = Acf[:, (l * 3 + s) * 8 + dc:(l * 3 + s) * 8 + dc + 1]
            shcol = modt[:, l * 72 + s * 24 + dc: l * 72 + s * 24 + dc + 1]
            S.op("dve", lambda e, dc=dc, tt=tt, acol=acol: e.scalar_tensor_tensor(
                out=tt[:], in0=xT[:, dc, tok], scalar=acol, in1=rt[:], op0=ALU.mult, op1=ALU.mult),
                rd=[xb[dc][tg], rb, mb], wr=[tb])
            S.op("act", lambda e, dc=dc, tt=tt, shcol=shcol: e.activation(
                out=dst_fn(dc), in_=tt[:], func=AF.Identity, bias=shcol, scale=1.0),
                rd=[tb, mb], wr=[dst_bufs[dc]])

    def post_residual(l, s, tg, ybf_fn, ybufs, ssbank_i):
        mb = B(f"mod{l}")
        tok = slice(tg * 512, (tg + 1) * 512)
        rt, rb = rstd_from(ssbank_i, 1.0 / 1024.0)
        for dc in range(8):
            tt, tb = t32_rot.next()
            bcol = Bcf[:, (l * 3 + s) * 8 + dc:(l * 3 + s) * 8 + dc + 1]
            S.op("dve", lambda e, dc=dc, tt=tt, bcol=bcol: e.scalar_tensor_tensor(
                out=tt[:], in0=ybf_fn(dc), scalar=bcol, in1=rt[:], op0=ALU.mult, op1=ALU.mult),
                rd=[ybufs[dc], rb, mb], wr=[tb])
            S.op("pool", lambda e, dc=dc, tt=tt: e.tensor_tensor(out=xT[:, dc, tok], in0=xT[:, dc, tok], in1=tt[:], op=ALU.add),
                 rd=[tb, xb[dc][tg]], wr=[xb[dc][tg]])

    def ffn(l, wi, s):
        with ExitStack() as fs:
            hT = sb("hT", [128, 8, 1024], BF16, fs)
            aT = sb("aT", [128, 22, 1024], BF16, fs)
            sg_rot = Rot([(sb(f"sg{i}", [128, 512], BF16, fs), B(f"sg{i}")) for i in range(2)])
            ysq_rot = Rot([(sb(f"ysq{i}", [128, 512], BF16, fs), B(f"ysq{i}")) for i in range(2)])
            hb = [[B(f"h{dc}_{t}") for t in range(2)] for dc in range(8)]
            ab = [[B(f"a{fc}_{t}") for t in range(2)] for fc in range(22)]
            scoped = [b for r in hb for b in r] + [b for r in ab for b in r] + [x[1] for x in sg_rot.items + ysq_rot.items]
            for ps in range(2):
                if DEBUG_CUT < 99 and ps == 1:
                    break
                wd_st.prefetch()
                for t in range(2):
                    tg = 2 * ps + t
                    norm_x_to_h(l, s, tg, lambda dc, t=t: hT[:, dc, t * 512:(t + 1) * 512], [hb[dc][t] for dc in range(8)], 6 + t)
                if DEBUG_CUT <= 1:
                    break
                for fc in range(22):
                    wt, wb = wgu_st.get()
                    for t in range(2):
                        for gu in range(2):
                            bk = 2 * gu + t
                            for dc in range(8):
                                S.op("pe", lambda e, dc=dc, gu=gu, bk=bk, t=t: e.matmul(
                                    pbt[bk][:, :], lhsT=wt[:, gu * 1024 + dc * 128: gu * 1024 + (dc + 1) * 128],
                                    rhs=hT[:, dc, t * 512:(t + 1) * 512], start=(dc == 0), stop=(dc == 7)),
                                    rd=[wb, hb[dc][t]], wr=[pbb[bk]], inc=(dc == 7))
                        st, sbf = sg_rot.next()
                        S.op("act", lambda e, st=st, t=t: e.activation(out=st[:], in_=pbt[t][:, :], func=AF.Silu),
                             rd=[pbb[t]], wr=[sbf])
                        S.op("dve", lambda e, st=st, t=t, fc=fc: e.tensor_tensor(
                            out=aT[:, fc, t * 512:(t + 1) * 512], in0=st[:], in1=pbt[2 + t][:, :], op=ALU.mult),
                            rd=[sbf, pbb[2 + t]], wr=[ab[fc][t]])
                wgu_st.prefetch()
                if DEBUG_CUT <= 2:
                    break
                for dc in range(8):
                    wt, wb = wd_st.get()
                    for t in range(2):
                        bk = 4 + t
                        for fc in range(22):
                            S.op("pe", lambda e, fc=fc, bk=bk, t=t: e.matmul(
                                pbt[bk][:, :], lhsT=wt[:, fc * 128:(fc + 1) * 128],
                                rhs=aT[:, fc, t * 512:(t + 1) * 512], start=(fc == 0), stop=(fc == 21)),
                                rd=[wb, ab[fc][t]], wr=[pbb[bk]], inc=(fc == 21))
                        yt, yb = ysq_rot.next()
                        S.op("act", lambda e, yt=yt, bk=bk: e.activation(out=yt[:], in_=pbt[bk][:, :], func=AF.Square),
                             rd=[pbb[bk]], wr=[yb])
                        S.op("act", lambda e, bk=bk, dc=dc, t=t: e.activation(
                            out=hT[:, dc, t * 512:(t + 1) * 512], in_=pbt[bk][:, :], func=AF.Copy),
                            rd=[pbb[bk]], wr=[hb[dc][t]])
                        S.op("pe", lambda e, yt=yt, t=t, dc=dc: e.matmul(
                            pbt[6 + t][:, :], lhsT=ones_bf[:], rhs=yt[:], start=(dc == 0), stop=(dc == 7)),
                            rd=[yb] + CB, wr=[pbb[6 + t]], inc=True)
                if DEBUG_CUT <= 3:
                    break
                for t in range(2):
                    post_residual(l, s, 2 * ps + t, lambda dc, t=t: hT[:, dc, t * 512:(t + 1) * 512],
                                  [hb[dc][t] for dc in range(8)], 6 + t)
            S.release(scoped)

    send1_b = [B(f"send1_{c}") for c in range(12)]
    recv1_b = [B(f"recv1_{c}") for c in range(12)]
    send2_b = [B(f"send2_{c}") for c in range(4)]
    recv2_b = [B(f"recv2_{c}") for c in range(4)]
    s1 = send1.ap()
    s1q = s1.rearrange("(g c k d) t -> g c k d t", g=4, c=3, k=4, d=64)
    s1v = s1.rearrange("(g c k d) (pl t m) -> g c k (d pl) t m", g=4, c=3, k=4, d=64, pl=2, t=16, m=64)
    r1g = recv1.ap().rearrange("(g c x) t -> g c x t", g=4, c=3)
    m1q = mine1.ap().rearrange("(c r k d) t -> c k d r t", c=3, r=4, k=4, d=64)
    m1v = mine1.ap().rearrange("(c r k d) (pl x) -> c k (d pl) r x", c=3, r=4, k=4, d=64, pl=2)
    s2 = send2.ap()
    m2 = mine2.ap().rearrange("(c r d) t -> c d r t", c=4, r=4, d=64)
    mine1_b = [B(f"mine1_{c}") for c in range(3)]
    mine2_b = [B(f"mine2_{c}") for c in range(4)]

    def ld_q(ct, k):
        return m1q[ct, k, :, :, :]

    def ld_v(ct, k):
        return m1v[ct, k, :, :, :]

    def gather1():
        for c in range(12):
            S.allgather(s1[c * 256:(c + 1) * 256, :], recv1.ap()[c * 1024:(c + 1) * 1024, :], rd=[], wr=[send1_b[c], recv1_b[c]], name=f"g1_{c}")
        for ct in range(3):
            S.dma("sp", [(mine1.ap()[ct * 1024:(ct + 1) * 1024, :],
                          r1g[bass.ds(gidx, 1), ct, :, :].rearrange("o x t -> (o x) t"))],
                  rd=[recv1_b[g_ * 3 + ct] for g_ in range(4)], wr=[mine1_b[ct]], owner=mine1_b[ct])

    def gather2(c):
        S.allgather(s2[c * 64:(c + 1) * 64, :], recv2.ap()[c * 256:(c + 1) * 256, :], rd=[], wr=[send2_b[c], recv2_b[c]], name=f"g2_{c}")
        S.dma("sp", [(mine2.ap()[c * 256:(c + 1) * 256, :],
                      recv2.ap()[c * 256:(c + 1) * 256, :].rearrange("x (g t) -> x g t", g=4)[:, bass.ds(gidx, 1), :].rearrange("x o t -> x (o t)"))],
              rd=[recv2_b[c]], wr=[mine2_b[c]], owner=mine2_b[c])

    def normalize_store(obank_i, exps_col, slot, tgi, ost_rot, bcbank_i, rden_t, rden_b, of_t, of_b):
        ob = pbb[obank_i]
        if exps_col is not None:
            S.op("dve", lambda e: e.tensor_scalar(out=rden_t[64:65, :], in0=pbt[obank_i][64:65, :], scalar1=exps_col,
                                                  scalar2=None, op0=ALU.add), rd=[ob] + CB, wr=[rden_b])
        else:
            S.op("dve", lambda e: e.tensor_copy(out=rden_t[64:65, :], in_=pbt[obank_i][64:65, :]), rd=[ob], wr=[rden_b])
        S.op("dve", lambda e: e.reciprocal(out=rden_t[64:65, :], in_=rden_t[64:65, :]), rd=[rden_b], wr=[rden_b])
        S.op("pe", lambda e: e.matmul(pbt[bcbank_i][0:64, :], lhsT=onesf[64:65, 0:64], rhs=rden_t[64:65, :],
                                      start=True, stop=True), rd=[rden_b] + CB, wr=[pbb[bcbank_i]], inc=True)
        S.op("act", lambda e: e.activation(out=of_t[0:64, :], in_=pbt[obank_i][0:64, :], func=AF.Copy), rd=[ob], wr=[of_b])
        ot, obf = ost_rot.next()
        S.op("dve", lambda e: e.tensor_tensor(out=ot[0:64, :], in0=of_t[0:64, :], in1=pbt[bcbank_i][0:64, :], op=ALU.mult),
             rd=[of_b, pbb[bcbank_i]], wr=[obf])
        S.dma("sp", [(s2[slot * 64:(slot + 1) * 64, tgi * 512:(tgi + 1) * 512], ot[0:64, :])],
              rd=[obf, send2_b[slot]], wr=[], owner=obf)

    def attention(l, bg):
        s = 1
        mb = B(f"mod{l}")
        with ExitStack() as fs:
            hT4 = sb("hT4", [128, 8, 1024], BF16, fs)
            h4b = [[B(f"h4_{dc}_{t}") for t in range(2)] for dc in range(8)]
            wv = sb("wv", [128, 6144], BF16, fs)
            wvb = B("wv")
            bv = sb("bv", [128, 768], F32, fs)
            bvb = B("bv")
            wqk_slots = [(sb(f"wqk{i}", [128, 3584], BF16, fs), B(f"wqk{i}")) for i in range(2)]
            stq_rot = Rot([(sb(f"stq{i}", [64, 7, 512], BF16, fs), B(f"stq{i}")) for i in range(2)])
            stv_rot = Rot([(sb(f"stv{i}", [128, 12, 4, 64], BF16, fs), B(f"stv{i}")) for i in range(2)])
            scoped = [b for r in h4b for b in r] + [wvb, bvb] + [x[1] for x in wqk_slots + stq_rot.items + stv_rot.items]
            S.dma("pool", [(wv[:], wv_d[l * 128:(l + 1) * 128, :])], rd=[], wr=[wvb], owner=wvb)
            S.dma("sp", [(bv[:], bv_d[l * 128:(l + 1) * 128, :])], rd=[], wr=[bvb], owner=bvb)
            wqk_st = WStream(S, wqk_slots, [wqk_d[(l * 4 + gq) * 128:(l * 4 + gq + 1) * 128, :] for hf in range(2) for gq in range(4)])
            wqk_st.prefetch()
            for hf in range(2):
                for t in range(2):
                    tg = 2 * hf + t
                    norm_x_to_h(l, s, tg, lambda dc, t=t: hT4[:, dc, t * 512:(t + 1) * 512],
                                [h4b[dc][t] for dc in range(8)], 6 + t)
                for t in range(2):
                    tg = 2 * hf + t
                    vt, vb_ = stv_rot.next()
                    for tl in range(4):
                        c0 = t * 512 + tl * 128
                        for (bk, lo, hi) in ((0 + 2 * (tl % 2), 0, 512), (1 + 2 * (tl % 2), 512, 768)):
                            for dc in range(8):
                                S.op("pe", lambda e, dc=dc, bk=bk, lo=lo, hi=hi, c0=c0: e.matmul(
                                    pbt[bk][:, 0:hi - lo], lhsT=hT4[:, dc, c0:c0 + 128], rhs=wv[:, dc * 768 + lo: dc * 768 + hi],
                                    start=(dc == 0), stop=(dc == 7)), rd=[h4b[dc][t], wvb], wr=[pbb[bk]], inc=(dc == 7))
                            nvs = (hi - lo) // 64
                            S.op("dve", lambda e, bk=bk, lo=lo, hi=hi, nvs=nvs, tl=tl, vt=vt: e.tensor_tensor(
                                out=vt[:, lo // 64: lo // 64 + nvs, tl, :],
                                in0=pbt[bk][:, 0:hi - lo].rearrange("p (v m) -> p v m", m=64),
                                in1=bv[:, lo:hi].rearrange("p (v m) -> p v m", m=64), op=ALU.add),
                                rd=[pbb[bk], bvb], wr=[vb_])
                    vt5 = vt[:, :, :, :].rearrange("p (g j) t m -> p g j t m", j=3)
                    S.dma("sp", [(s1v[:, cj, kj, :, tg * 4:(tg + 1) * 4, :].rearrange("g p t m -> p g t m"), vt5[:, :, j, :, :])
                                 for (j, cj, kj) in ((0, 0, 3), (1, 1, 2), (2, 2, 2))],
                          rd=[vb_] + send1_b, wr=[], owner=vb_)
                for gq in range(4):
                    wt, wb = wqk_st.get()
                    for t in range(2):
                        tg = 2 * hf + t
                        qt, qb = stq_rot.next()
                        for sl in range(7):
                            bk = 4 + (sl % 2)
                            for dc in range(8):
                                S.op("pe", lambda e, dc=dc, sl=sl, bk=bk, t=t: e.matmul(
                                    pbt[bk][0:64, :], lhsT=wt[:, (sl * 8 + dc) * 64:(sl * 8 + dc + 1) * 64],
                                    rhs=hT4[:, dc, t * 512:(t + 1) * 512], start=(dc == 0), stop=(dc == 7)),
                                    rd=[wb, h4b[dc][t]], wr=[pbb[bk]], inc=(dc == 7))
                            bcol = bqk[:, l * 28 + gq * 7 + sl: l * 28 + gq * 7 + sl + 1]
                            S.op("act", lambda e, sl=sl, bk=bk, qt=qt, bcol=bcol: e.activation(
                                out=qt[:, sl, :], in_=pbt[bk][0:64, :], func=AF.Identity, bias=bcol, scale=1.0),
                                rd=[pbb[bk]] + CB, wr=[qb])
                        cs = slice(tg * 512, (tg + 1) * 512)
                        S.dma("sp", [(s1q[gq, 0, 0:3, :, cs].rearrange("k d t -> d k t"), qt[:, 0:3, :]),
                                     (s1q[gq, 1, 0:2, :, cs].rearrange("k d t -> d k t"), qt[:, 3:5, :]),
                                     (s1q[gq, 2, 0:2, :, cs].rearrange("k d t -> d k t"), qt[:, 5:7, :])],
                              rd=[qb] + send1_b[gq * 3:gq * 3 + 3], wr=[], owner=qb)
            S.release(scoped)
        if DEBUG_CUT == 11:
            return
        gather1()
        if DEBUG_CUT == 12:
            S.wait_all("sp", mine1_b)
            return

        with ExitStack() as fs:
            QTs = [sb(f"QTs{i}", [64, 8192], BF16, fs) for i in range(2)]
            KTs = sb("KTs", [64, 128 + 8192], BF16, fs)
            Vs = sb("Vs", [128, 65, 65], BF16, fs)
            vst = sb("vst", [128, 4, 1024], BF16, fs)
            qsb, ksb, vsb, vstb = B("QTs"), B("KTs"), B("Vs"), B("vst")
            P_rot = Rot([(sb(f"Ps{i}", [128, 256], BF16, fs), B(f"Ps{i}")) for i in range(3)])
            sb_rot = Rot([(sb(f"sbs{i}", [128, 256], F32, fs), B(f"sbs{i}")) for i in range(2)])
            ost_rot = Rot([(sb(f"ost{i}", [64, 512], BF16, fs), B(f"ost{i}")) for i in range(2)])
            rden_t, rden_b = sb("rden", [65, 512], F32, fs), B("rden")
            of_t, of_b = sb("of", [64, 512], F32, fs), B("of")
            scoped = [qsb, ksb, vsb, vstb, rden_b, of_b] + [x[1] for x in P_rot.items + sb_rot.items + ost_rot.items]
            S.dma("sp", [(QTs[i][:, :].rearrange("d (r t) -> d r t", r=4), ld_q(0, i)) for i in range(2)],
                  rd=[mine1_b[0]], wr=[qsb], owner=qsb)
            S.op("dve", lambda e: e.memset(KTs[:, 0:128], 0.0), wr=[ksb])
            S.dma("sp", [(KTs[:, 128:].rearrange("d (r t) -> d r t", r=4), ld_q(0, 2))],
                  rd=[mine1_b[0]], wr=[ksb], owner=ksb)
            S.dma("sp", [(vst[:, :, :], ld_v(0, 3))], rd=[mine1_b[0]], wr=[vstb], owner=vstb)
            S.op("pool", lambda e: e.memset(Vs[:, :, 64:65], 1.0), wr=[vsb])
            S.op("pool", lambda e: e.memset(Vs[:, 0, 0:64], 0.0), wr=[vsb])
            S.op("pool", lambda e: e.tensor_copy(out=Vs[:, 1:65, 0:64], in_=vst[:, :, :].rearrange("p r (t m) -> p (r t) m", m=64)),
                 rd=[vstb], wr=[vsb])
            for i in range(2):
                for tgi in range(16):
                    obk = 4 + (tgi % 2)
                    for qq in range(4):
                        T = tgi * 4 + qq
                        sbk = T % 3
                        for half in range(2):
                            S.op("pe", lambda e, T=T, half=half, sbk=sbk: e.matmul(
                                pbt[sbk][:, half * 128:(half + 1) * 128], lhsT=KTs[:, (T + half) * 128:(T + half + 1) * 128],
                                rhs=QTs[i][:, T * 128:(T + 1) * 128], start=True, stop=True),
                                rd=[ksb, qsb], wr=[pbb[sbk]], inc=(half == 1))
                        st, stb = sb_rot.next()
                        var = i * 2 + (1 if T == 0 else 0)
                        S.op("dve", lambda e, st=st, sbk=sbk, var=var: e.scalar_tensor_tensor(
                            out=st[:], in0=pbt[sbk][:, 0:256], scalar=0.125, in1=gswa[:, var * 256:(var + 1) * 256],
                            op0=ALU.mult, op1=ALU.add), rd=[pbb[sbk]] + CB, wr=[stb])
                        pt, ptb = P_rot.next()
                        S.op("act", lambda e, st=st, pt=pt: e.activation(out=pt[:], in_=st[:], func=AF.Exp), rd=[stb], wr=[ptb])
                        for half in range(2):
                            S.op("pe", lambda e, T=T, half=half, obk=obk, pt=pt, qq=qq: e.matmul(
                                pbt[obk][0:65, qq * 128:(qq + 1) * 128], lhsT=Vs[:, T + half, 0:65],
                                rhs=pt[:, half * 128:(half + 1) * 128], start=(half == 0), stop=(half == 1)),
                                rd=[vsb, ptb], wr=[pbb[obk]], inc=(half == 1))
                    normalize_store(obk, esnk[64:65, l * 2 + i: l * 2 + i + 1], i, tgi, ost_rot, 6 + (tgi % 2),
                                    rden_t, rden_b, of_t, of_b)
                gather2(i)
            S.release(scoped)

        if DEBUG_CUT == 13:
            return
        for i in range(2):
            with ExitStack() as fs:
                QTa = sb("QTa", [96, 8192], BF16, fs)
                KTa = sb("KTa", [96, 8192], BF16, fs)
                Va = sb("Va", [128, 64, 65], BF16, fs)
                vst = sb("vstm", [128, 4, 1024], BF16, fs)
                qab, kab, kib, vab, vstb = B("QTa"), B("KTa"), B("KTi"), B("Va"), B("vstm")
                qmb = [B(f"qm{t}") for t in range(16)]
                km32 = sb("km32", [64, 32], F32, fs)
                kmd = sb("kmd", [64, 32], F32, fs)
                kmh = sb("kmh", [64, 32], BF16, fs)
                kml = sb("kml", [64, 32], BF16, fs)
                kmb = B("km")
                gb_rot = Rot([(sb(f"gb{k}", [128, 32], F32, fs), B(f"gb{k}")) for k in range(2)])
                t8_rot = Rot([(sb(f"t8{k}", [128, 8], F32, fs), B(f"t8{k}")) for k in range(2)])
                mbt_rot = Rot([(sb(f"mbt{k}", [128, 4, 96], F32, fs), B(f"mbt{k}")) for k in range(2)])
                P_rot = Rot([(sb(f"Pm{k}", [128, 512], BF16, fs), B(f"Pm{k}")) for k in range(3)])
                sb_rot = Rot([(sb(f"sbm{k}", [128, 512], F32, fs), B(f"sbm{k}")) for k in range(2)])
                ost_rot = Rot([(sb(f"ostm{k}", [64, 512], BF16, fs), B(f"ostm{k}")) for k in range(2)])
                rden_t, rden_b = sb("rdenm", [65, 512], F32, fs), B("rdenm")
                of_t, of_b = sb("ofm", [64, 512], F32, fs), B("ofm")
                scoped = ([qab, kab, kib, vab, vstb, kmb, rden_b, of_b] + qmb +
                          [x[1] for x in gb_rot.items + t8_rot.items + mbt_rot.items + P_rot.items + sb_rot.items + ost_rot.items])
                S.dma("sp", [(QTa[0:64, :].rearrange("d (r t) -> d r t", r=4), ld_q(1 + i, 0))],
                      rd=[mine1_b[1 + i]], wr=[qab], owner=qab)
                S.dma("sp", [(KTa[0:64, :].rearrange("d (r t) -> d r t", r=4), ld_q(1 + i, 1))],
                      rd=[mine1_b[1 + i]], wr=[kab], owner=kab)
                S.dma("pool", [(KTa[64:96, :], blkind_d[:, :])], rd=[], wr=[kib], owner=kib)
                S.dma("sp", [(vst[:, :, :], ld_v(1 + i, 2))], rd=[mine1_b[1 + i]], wr=[vstb], owner=vstb)
                S.op("pool", lambda e: e.memset(Va[:, :, 64:65], 1.0), wr=[vab])
                S.op("pool", lambda e: e.tensor_copy(out=Va[:, :, 0:64], in_=vst[:, :, :].rearrange("p r (t m) -> p (r t) m", m=64)),
                     rd=[vstb], wr=[vab])
                S.op("dve", lambda e: e.tensor_reduce(out=km32[:, :], in_=KTa[0:64, :].rearrange("d (b k) -> d b k", k=256),
                                                      axis=AX.X, op=ALU.add), rd=[kab], wr=[kmb])
                S.op("dve", lambda e: e.tensor_copy(out=kmh[:, :], in_=km32[:, :]), rd=[kmb], wr=[kmb])
                S.op("dve", lambda e: e.tensor_tensor(out=kmd[:, :], in0=km32[:, :], in1=kmh[:, :], op=ALU.subtract), rd=[kmb], wr=[kmb])
                S.op("dve", lambda e: e.tensor_copy(out=kml[:, :], in_=kmd[:, :]), rd=[kmb], wr=[kmb])
                for tgi in range(16):
                    mt, mtb = mbt_rot.next()
                    for qq in range(4):
                        T = tgi * 4 + qq
                        cur = T // 2
                        if cur >= 3:
                            S.op("pe", lambda e, T=T, qq=qq: e.matmul(pbt[6][:, qq * 32:(qq + 1) * 32], lhsT=QTa[0:64, T * 128:(T + 1) * 128],
                                                                      rhs=kmh[:, :], start=True, stop=False), rd=[qab, kmb], wr=[pbb[6]], inc=False)
                            S.op("pe", lambda e, T=T, qq=qq: e.matmul(pbt[6][:, qq * 32:(qq + 1) * 32], lhsT=QTa[0:64, T * 128:(T + 1) * 128],
                                                                      rhs=kml[:, :], start=False, stop=True), rd=[qab, kmb], wr=[pbb[6]], inc=True)
                            gt, gtb = gb_rot.next()
                            S.op("dve", lambda e, gt=gt, qq=qq, cur=cur: e.tensor_tensor(
                                out=gt[:], in0=pbt[6][:, qq * 32:(qq + 1) * 32], in1=stair[:, 32 - cur:64 - cur], op=ALU.add),
                                rd=[pbb[6]] + CB, wr=[gtb])
                            t8, t8b = t8_rot.next()
                            S.op("dve", lambda e, gt=gt, t8=t8: e.max(out=t8[:], in_=gt[:]), rd=[gtb], wr=[t8b])
                            S.op("dve", lambda e, gt=gt, t8=t8, mt=mt, qq=qq: e.tensor_scalar(
                                out=mt[:, qq, 64:96], in0=gt[:], scalar1=t8[:, 2:3], scalar2=1.0, op0=ALU.is_ge, op1=ALU.subtract),
                                rd=[gtb, t8b], wr=[mtb])
                            S.op("dve", lambda e, mt=mt, qq=qq, cur=cur: e.memset(mt[:, qq, 64 + cur:65 + cur], 0.0), wr=[mtb])
                        else:
                            S.op("dve", lambda e, mt=mt, qq=qq, cur=cur: e.tensor_copy(
                                out=mt[:, qq, 64:96], in_=stair[:, 64 + 31 - cur:64 + 63 - cur]), rd=CB, wr=[mtb])
                        S.op("pe", lambda e, mt=mt, qq=qq: e.transpose(out=pbt[7][0:96, qq * 128:(qq + 1) * 128], in_=mt[:, qq, :],
                                                                       identity=ident[:, :]), rd=[mtb] + CB, wr=[pbb[7]], inc=True)
                    S.op("act", lambda e, tgi=tgi: e.activation(out=QTa[64:96, tgi * 512:(tgi + 1) * 512], in_=pbt[7][64:96, :], func=AF.Copy),
                         rd=[pbb[7]], wr=[qmb[tgi]])
                for tgi in range(16):
                    obk = 4 + (tgi % 2)
                    nkt = 4 * tgi + 4
                    for kt in range(nkt):
                        sbk = kt % 3
                        S.op("pe", lambda e, kt=kt, sbk=sbk, tgi=tgi: e.matmul(
                            pbt[sbk][:, :], lhsT=KTa[0:96, kt * 128:(kt + 1) * 128], rhs=QTa[0:96, tgi * 512:(tgi + 1) * 512],
                            start=True, stop=True), rd=[kab, kib, qab, qmb[tgi]], wr=[pbb[sbk]], inc=True)
                        pt, ptb = P_rot.next()
                        rel = kt - 4 * tgi
                        if rel >= -2:
                            j0 = i * 1152 + 384 - 128 * rel
                            st, stb = sb_rot.next()
                            S.op("dve", lambda e, st=st, sbk=sbk, j0=j0: e.scalar_tensor_tensor(
                                out=st[:], in0=pbt[sbk][:, :], scalar=0.125, in1=gmoba[:, j0:j0 + 512], op0=ALU.mult, op1=ALU.add),
                                rd=[pbb[sbk]] + CB, wr=[stb])
                            S.op("act", lambda e, st=st, pt=pt: e.activation(out=pt[:], in_=st[:], func=AF.Exp), rd=[stb], wr=[ptb])
                        else:
                            S.op("act", lambda e, pt=pt, sbk=sbk: e.activation(out=pt[:], in_=pbt[sbk][:, :], func=AF.Exp,
                                                                              bias=cfar[:, i:i + 1], scale=0.125), rd=[pbb[sbk]] + CB, wr=[ptb])
                        S.op("pe", lambda e, kt=kt, obk=obk, pt=pt, nkt=nkt: e.matmul(
                            pbt[obk][0:65, :], lhsT=Va[:, kt, 0:65], rhs=pt[:], start=(kt == 0), stop=(kt == nkt - 1)),
                            rd=[vab, ptb], wr=[pbb[obk]], inc=(kt == nkt - 1))
                    normalize_store(obk, None, 2 + i, tgi, ost_rot, 3, rden_t, rden_b, of_t, of_b)
                    if bg is not None:
                        next(bg, None)
                        next(bg, None)
                gather2(2 + i)
                S.release(scoped)
        if bg is not None:
            run_task(bg)
        if DEBUG_CUT == 14:
            return

        with ExitStack() as fs:
            wo = sb("wo", [128, 8192], BF16, fs)
            wob = B("wo")
            OT_rot = Rot([(sb(f"OT{k}", [128, 8, 512], BF16, fs), B(f"OT{k}")) for k in range(2)])
            osq = sb("osq", [128, 8, 512], BF16, fs)
            osqb = B("osq")
            OnT = sb("OnT", [128, 8, 512], BF16, fs)
            onb = [B(f"on{k}") for k in range(8)]
            ybf = sb("ybf", [128, 8, 512], BF16, fs)
            ybb = [B(f"yb{k}") for k in range(8)]
            ysq_rot = Rot([(sb(f"ysqo{k}", [128, 512], BF16, fs), B(f"ysqo{k}")) for k in range(2)])
            rsa_t, rsa_b = sb("rsa", [128, 512], F32, fs), B("rsa")
            scoped = [wob, osqb, rsa_b] + onb + ybb + [x[1] for x in OT_rot.items + ysq_rot.items]
            S.dma("pool", [(wo[:], wout_d[l * 128:(l + 1) * 128, :])], rd=[], wr=[wob], owner=wob)
            for tg in range(4):
                ot, otb = OT_rot.next()
                S.dma("sp", [(ot[:, :, :].rearrange("p (r par) t -> p r par t", par=2)[(sl % 2) * 64:(sl % 2) * 64 + 64, :, sl // 2, :], m2[sl, :, :, tg * 512:(tg + 1) * 512])
                             for sl in range(4)], rd=mine2_b, wr=[otb], owner=otb)
                S.op("dve", lambda e, ot=ot: e.tensor_tensor(out=osq[:, :, :], in0=ot[:, :, :], in1=ot[:, :, :], op=ALU.mult),
                     rd=[otb], wr=[osqb])
                for par in range(2):
                    for k in range(4):
                        kc = 2 * k + par
                        S.op("pe", lambda e, kc=kc, k=k, par=par: e.matmul(pbt[6 + par][:, :], lhsT=ones_bf[:], rhs=osq[:, kc, :],
                                                                         start=(k == 0), stop=(k == 3)),
                             rd=[osqb] + CB, wr=[pbb[6 + par]], inc=(k == 3))
                S.op("act", lambda e: e.activation(out=sd_t[:], in_=pbt[6][:, :], func=AF.Sqrt, bias=epsc[:, 0:1], scale=1.0 / 512.0),
                     rd=[pbb[6]] + CB, wr=[sd_b])
                S.op("dve", lambda e: e.reciprocal(out=rsa_t[:], in_=sd_t[:]), rd=[sd_b], wr=[rsa_b])
                rbt, rbb = rstd_from(7, 1.0 / 512.0)
                for kc in range(8):
                    rr, rrb = (rsa_t, rsa_b) if kc % 2 == 0 else (rbt, rbb)
                    gcol = vecs[:, l * 128 + 120 + kc: l * 128 + 121 + kc]
                    S.op("dve", lambda e, kc=kc, rr=rr, gcol=gcol, ot=ot: e.scalar_tensor_tensor(
                        out=OnT[:, kc, :], in0=ot[:, kc, :], scalar=gcol, in1=rr[:], op0=ALU.mult, op1=ALU.mult),
                        rd=[otb, rrb] + CB, wr=[onb[kc]])
                for dm in range(8):
                    bk = 4 + (dm % 2)
                    for kc in range(8):
                        S.op("pe", lambda e, kc=kc, dm=dm, bk=bk: e.matmul(
                            pbt[bk][:, :], lhsT=wo[:, kc * 1024 + dm * 128: kc * 1024 + (dm + 1) * 128], rhs=OnT[:, kc, :],
                            start=(kc == 0), stop=(kc == 7)), rd=[wob, onb[kc]], wr=[pbb[bk]], inc=(kc == 7))
                    yt, yb = ysq_rot.next()
                    S.op("act", lambda e, yt=yt, bk=bk: e.activation(out=yt[:], in_=pbt[bk][:, :], func=AF.Square), rd=[pbb[bk]], wr=[yb])
                    S.op("act", lambda e, dm=dm, bk=bk: e.activation(out=ybf[:, dm, :], in_=pbt[bk][:, :], func=AF.Copy),
                         rd=[pbb[bk]], wr=[ybb[dm]])
                    S.op("pe", lambda e, yt=yt, dm=dm: e.matmul(pbt[3][:, :], lhsT=ones_bf[:], rhs=yt[:], start=(dm == 0), stop=(dm == 7)),
                         rd=[yb] + CB, wr=[pbb[3]], inc=True)
                post_residual(l, s, tg, lambda dc: ybf[:, dc, :], ybb, 3)
            S.release(scoped)

    with ExitStack() as zs:
        zt = sb("zpad", [64, 2048], BF16, zs)
        zb = B("zpad")
        S.op("pool", lambda e: e.memset(zt[:], 0.0), wr=[zb])
        S.dma("sp", [(s1q[g_, ct, 3, :, :], zt[:]) for g_ in range(4) for ct in (1, 2)],
              rd=[zb] + send1_b, wr=[], owner=zb)
        S.release([zb])
    run_task(mod_task(0))
    sub = 0
    for l in range(L):
        if sub < nsub:
            ffn(l, 0, 0)
            sub += 1
        if sub < nsub:
            bg = mod_task(l + 1) if (l + 1 < L and 3 * (l + 1) < nsub) else None
            attention(l, bg)
            sub += 1
        elif l + 1 < L:
            pass
        if sub < nsub:
            ffn(l, 1, 2)
            sub += 1
    allx = [b for row in xb for b in row]
    outb = B("outb")
    S.dma("sp", [(out_d[dc * 128:(dc + 1) * 128, :], xT[:, dc, :]) for dc in range(8)], rd=allx, wr=[outb], owner=outb)
    S.wait_all("sp", [outb])
    es.close()
    return nc


def _t5_bucket_np(dist):
    n = np.maximum(dist, 0)
    nf = np.maximum(n, 1).astype(np.float32)
    large = 16 + (np.log(nf / np.float32(16)) / np.float32(math.log(128 / 16)) * np.float32(16)).astype(np.int32)
    large = np.minimum(large, 31)
    return np.where(n < 16, n, large)


def _slot_cols():
    qk, v = [], []
    for g in range(4):
        qk += [64 * (2 * g), 64 * (2 * g + 1), 512 + 64 * (g // 2), 768 + 64 * (2 * g), 1280 + 64 * (2 * g),
               768 + 64 * (2 * g + 1), 1280 + 64 * (2 * g + 1)]
        v += [640 + 64 * (g // 2), 1792 + 64 * (2 * g), 1792 + 64 * (2 * g + 1)]
    return qk, v


def prep_inputs(inp, L=DEPTH, l0=0, x=None):
    f = np.float32
    x = np.asarray(inp["x"], f) if x is None else x
    c = np.asarray(inp["c"], f)
    rel = np.asarray(inp["rel_bias"], f)
    ada_w = np.asarray(inp["ada_w"][l0:l0 + L], f)
    ada_b = np.asarray(inp["ada_b"][l0:l0 + L], f)
    npre = np.asarray(inp["norm_pre"][l0:l0 + L], f)
    npost = np.asarray(inp["norm_post"][l0:l0 + L], f)
    wg = np.asarray(inp["ffn_w_gate"][l0:l0 + L], f)
    wu = np.asarray(inp["ffn_w_up"][l0:l0 + L], f)
    wdn = np.asarray(inp["ffn_w_down"][l0:l0 + L], f)
    w_in = np.asarray(inp["mix_w_in"][l0:l0 + L], f)
    b_in = np.asarray(inp["mix_b_in"][l0:l0 + L], f)
    w_out = np.asarray(inp["mix_w_out"][l0:l0 + L], f)
    sinks = np.asarray(inp["attn_sinks"][l0:l0 + L], f)
    gain = np.asarray(inp["group_gain"][l0:l0 + L], f)

    g6 = wg.reshape(L, 2, 8, 128, 22, 128)
    u6 = wu.reshape(L, 2, 8, 128, 22, 128)
    gu = np.stack([g6, u6], axis=2)
    wgu = np.ascontiguousarray(gu.transpose(0, 1, 5, 4, 2, 3, 6)).reshape(L * 2 * 22 * 128, 2048)
    d6 = wdn.reshape(L, 2, 22, 128, 8, 128)
    wd = np.ascontiguousarray(d6.transpose(0, 1, 4, 3, 2, 5)).reshape(L * 2 * 8 * 128, 2816)
    qk_cols, v_cols = _slot_cols()
    qk_idx = np.concatenate([np.arange(cb, cb + 64) for cb in qk_cols])
    v_idx = np.concatenate([np.arange(cb, cb + 64) for cb in v_cols])
    wq = w_in[:, :, qk_idx].reshape(L, 8, 128, 4, 7, 64)
    wqk = np.ascontiguousarray(wq.transpose(0, 3, 2, 4, 1, 5)).reshape(L * 4 * 128, 3584)
    wvv = w_in[:, :, v_idx].reshape(L, 8, 128, 768)
    wv = np.ascontiguousarray(wvv.transpose(0, 2, 1, 3)).reshape(L * 128, 6144)
    bqk = np.ascontiguousarray(b_in[:, qk_idx].reshape(L, 28, 64).transpose(2, 0, 1)).reshape(64, L * 28)
    bv = np.ascontiguousarray(np.broadcast_to(b_in[:, None, v_idx], (L, 128, 768))).reshape(L * 128, 768)
    rowperm = np.concatenate([np.concatenate([np.arange(128 * g, 128 * g + 128), np.arange(512 + 128 * g, 512 + 128 * g + 128)])
                              for g in range(4)])
    wo = w_out[:, rowperm, :].reshape(L, 8, 128, 1024)
    wout = np.ascontiguousarray(wo.transpose(0, 2, 1, 3)).reshape(L * 128, 8192)
    aw = ada_w.reshape(L, 8, 128, 36, 256)
    adaw = np.ascontiguousarray(aw.transpose(0, 3, 2, 1, 4)).reshape(L * 36 * 128, 2048)
    vecs = np.zeros((128, L * 128), f)
    for l in range(L):
        vecs[:, l * 128: l * 128 + 72] = ada_b[l].reshape(72, 128).T
        vecs[:, l * 128 + 72: l * 128 + 96] = npre[l].reshape(24, 128).T
        vecs[:, l * 128 + 96: l * 128 + 120] = npost[l].reshape(24, 128).T
        vecs[:, l * 128 + 120: l * 128 + 128] = gain[l][rowperm].reshape(8, 128).T
    ident = np.eye(128, dtype=f)
    blkind = np.zeros((32, 8192), f)
    for b in range(32):
        blkind[b, b * 256:(b + 1) * 256] = MASKV
    stair = np.zeros((128, 128), f)
    stair[:, 32:64] = -1e30
    stair[:, 96:128] = -1.0

    kk = np.arange(128)[:, None]
    shared = dict(wgu=wgu, wd=wd, wqk=wqk, wv=wv, bqk=bqk, bv=bv, wout=wout, adaw=adaw, vecs=vecs,
                  ident=ident, blkind=blkind, stair=stair)
    in_maps = []
    for core in range(8):
        bt, g = core // 4, core % 4
        m = dict(shared)
        m["xT"] = np.ascontiguousarray(x[bt, g * 2048:(g + 1) * 2048, :].T)
        m["cT"] = np.ascontiguousarray(c[bt].reshape(8, 128).T)
        gm = np.zeros((128, 2 * 1152), f)
        cf = np.zeros((128, 2), f)
        for i in range(2):
            h = 8 + 2 * g + i
            d = np.arange(1152)[None, :] - 384 - kk
            val = rel[_t5_bucket_np(d), h]
            gm[:, i * 1152:(i + 1) * 1152] = np.where(d >= 0, val, f(-30000.0))
            cf[:, i] = rel[31, h]
        m["gmoba"] = gm
        m["cfar"] = cf
        gs = np.zeros((128, 4 * 256), f)
        qq = np.arange(128)[None, :]
        for i in range(2):
            h = 2 * g + i
            d0 = qq + 128 - kk
            d1 = qq - kk
            a0 = np.where((d0 >= 0) & (d0 < 128), rel[_t5_bucket_np(d0), h], f(-30000.0))
            a1 = np.where((d1 >= 0) & (d1 < 128), rel[_t5_bucket_np(d1), h], f(-30000.0))
            gs[:, (i * 2) * 256:(i * 2) * 256 + 128] = a0
            gs[:, (i * 2) * 256 + 128:(i * 2 + 1) * 256] = a1
            gs[:, (i * 2 + 1) * 256:(i * 2 + 1) * 256 + 128] = f(-30000.0)
            gs[:, (i * 2 + 1) * 256 + 128:(i * 2 + 2) * 256] = a1
        m["gswa"] = gs
        m["snk"] = np.ascontiguousarray(sinks[:, 2 * g:2 * g + 2].reshape(1, L * 2))
        in_maps.append(m)
    return in_maps


_NC_CACHE = {}


def run(inputs, L=DEPTH, nsub=None, l0=0, x=None):
    key = (L, nsub)
    if key not in _NC_CACHE:
        _NC_CACHE[key] = build(L, nsub)
    nc = _NC_CACHE[key]
    in_maps = prep_inputs(inputs, L, l0, x)
    res = run_bass_kernel_spmd(nc, in_maps, core_ids=list(range(8)))
    out = np.zeros((2, 8192, 1024), np.float32)
    for core in range(8):
        bt, g = core // 4, core % 4
        out[bt, g * 2048:(g + 1) * 2048, :] = np.asarray(res.results[core]["outT"], np.float32).T
    return out


LAYERS_PER_LAUNCH = 4


def kernel(**inputs):
    x = None
    for l0 in range(0, DEPTH, LAYERS_PER_LAUNCH):
        x = run(inputs, LAYERS_PER_LAUNCH, None, l0, x)
    return x
```

```python
import math
import numpy as np
from contextlib import ExitStack
import concourse.bass as bass
import concourse.mybir as mybir
from concourse.bass_utils import run_bass_kernel_spmd

F32 = mybir.dt.float32
BF16 = mybir.dt.bfloat16
ALU = mybir.AluOpType
AF = mybir.ActivationFunctionType
AX = mybir.AxisListType

DEPTH = 4
DEBUG_CUT = 99
EPS = 1e-6
MASKV = 240000.0


class Tok:
    __slots__ = ("sem", "key", "val", "eng")

    def __init__(self, sem, key, val, eng):
        self.sem, self.key, self.val, self.eng = sem, key, val, eng


class Buf:
    def __init__(self, name):
        self.name = name
        self.w = None
        self.r = {}
        self.dsem = None
        self.dkey = None
        self.dcnt = 0


class Sched:
    def __init__(self, nc, es):
        self.nc, self.es = nc, es
        self.E = {"pe": nc.tensor, "act": nc.scalar, "dve": nc.vector, "pool": nc.gpsimd, "sp": nc.sync}
        self.sem, self.key, self.cnt = {}, {}, {}
        self.known = {e: {} for e in self.E}
        self.nsem = 0
        self.bufs = {}
        for e in self.E:
            self.new_epoch(e)

    def newsem(self, name):
        self.nsem += 1
        nm = f"{name}_{self.nsem}"
        return self.es.enter_context(self.nc.semaphore(nm)), nm

    def new_epoch(self, e):
        self.sem[e], self.key[e] = self.newsem("e" + e)
        self.cnt[e] = 0

    def B(self, name):
        b = self.bufs.get(name)
        if b is None:
            b = self.bufs[name] = Buf(name)
        return b

    def _wait(self, e, toks, skip_pe=False, defer=False):
        need = {}
        for t in toks:
            if t is None:
                continue
            if skip_pe and t.eng == "pe":
                continue
            cur = need.get(t.key)
            if cur is None or cur.val < t.val:
                need[t.key] = t
        kn = self.known[e]
        todo = [t for k, t in need.items() if kn.get(k, 0) < t.val]
        last = None
        if defer and todo:
            last = todo.pop()
            kn[last.key] = last.val
        for t in todo:
            self.E[e].wait_ge(t.sem, t.val)
            kn[t.key] = t.val
        return last

    @staticmethod
    def _deps(rd, wr):
        toks = []
        for b in rd:
            toks.append(b.w)
        for b in wr:
            toks.append(b.w)
            toks.extend(b.r.values())
        return toks

    @staticmethod
    def _record(tok, rd, wr):
        for b in rd:
            c = b.r.get(tok.key)
            if c is None or c.val < tok.val:
                b.r[tok.key] = tok
        for b in wr:
            b.w = tok
            b.r = {}

    def op(self, e, fn, rd=(), wr=(), inc=True):
        last = self._wait(e, self._deps(rd, wr), skip_pe=(e == "pe"), defer=True)
        ins = fn(self.E[e])
        if last is not None:
            ins._wait_ge(last.sem, last.val)
        if inc:
            ins.then_inc(self.sem[e], 1)
            self.cnt[e] += 1
            tok = Tok(self.sem[e], self.key[e], self.cnt[e], e)
        else:
            tok = Tok(self.sem[e], self.key[e], self.cnt[e] + 1, e)
        self._record(tok, rd, wr)
        return tok

    def dma(self, q, pairs, rd, wr, owner):
        if owner.dsem is None:
            owner.dsem, owner.dkey = self.newsem("d")
        last = self._wait(q, self._deps(rd, wr), defer=True)
        for (o, i) in pairs:
            ins = self.E[q].dma_start(out=o, in_=i)
            if last is not None:
                ins._wait_ge(last.sem, last.val)
                last = None
            ins.then_inc(owner.dsem, 16)
            owner.dcnt += 16
        tok = Tok(owner.dsem, owner.dkey, owner.dcnt, "dma")
        self._record(tok, rd, wr)
        return tok

    def allgather(self, src_ap, dst_ap, rd, wr, name):
        if not hasattr(self, "ccs"):
            self.ccs = {}
        if name not in self.ccs:
            self.ccs[name] = list(self.newsem("cc")) + [0]
        ent = self.ccs[name]
        ent[2] += 1
        sem, key = ent[0], ent[1]
        self._wait("pool", self._deps(rd, wr))
        self.nc.gpsimd.collective_compute(
            "AllGather", ALU.bypass, replica_groups=[[0, 1, 2, 3], [4, 5, 6, 7]],
            ins=[src_ap.opt()], outs=[dst_ap.opt()],
        ).then_inc(sem)
        tok = Tok(sem, key, ent[2], "cc")
        self._record(tok, rd, wr)
        return tok

    def release(self, bufs):
        toks = []
        for b in bufs:
            toks.append(b.w)
            toks.extend(b.r.values())
        for e in self.E:
            self._wait(e, toks)

    def wait_all(self, e, bufs):
        toks = []
        for b in bufs:
            toks.append(b.w)
            toks.extend(b.r.values())
        self._wait(e, toks)


class Rot:
    def __init__(self, items):
        self.items, self.i = items, 0

    def next(self):
        it = self.items[self.i % len(self.items)]
        self.i += 1
        return it


class WStream:
    def __init__(self, S, slots, srcs):
        self.S, self.slots, self.srcs = S, slots, srcs
        self.issued = 0
        self.consumed = 0

    def prefetch(self, ahead=None):
        n = len(self.slots) if ahead is None else ahead
        while self.issued < len(self.srcs) and self.issued < self.consumed + n:
            t, b = self.slots[self.issued % len(self.slots)]
            self.S.dma("pool", [(t[:], self.srcs[self.issued])], rd=[], wr=[b], owner=b)
            self.issued += 1

    def get(self):
        self.prefetch()
        it = self.slots[self.consumed % len(self.slots)]
        self.consumed += 1
        return it


def build(L=DEPTH, nsub=None):
    if nsub is None:
        nsub = 3 * L
    nc = bass.Bass("TRN2", target_bir_lowering=False)
    es = ExitStack()
    es.enter_context(nc.allow_low_precision("bf16 matmuls with fp32 accumulation"))

    def din(name, shape, dt=F32):
        return nc.dram_tensor(name, shape, dt, kind="ExternalInput").ap()

    xT_d = din("xT", [1024, 2048])
    cT_d = din("cT", [128, 8])
    wgu_d = din("wgu", [L * 2 * 22 * 128, 2048])
    wd_d = din("wd", [L * 2 * 8 * 128, 2816])
    wqk_d = din("wqk", [L * 4 * 128, 3584])
    wv_d = din("wv", [L * 128, 6144])
    bqk_d = din("bqk", [64, L * 28])
    bv_d = din("bv", [L * 128, 768])
    wout_d = din("wout", [L * 128, 8192])
    adaw_d = din("adaw", [L * 36 * 128, 2048])
    vecs_d = din("vecs", [128, L * 128])
    ident_d = din("ident", [128, 128])
    blkind_d = din("blkind", [32, 8192])
    stair_d = din("stair", [128, 128])
    gmoba_d = din("gmoba", [128, 2 * 1152])
    cfar_d = din("cfar", [128, 2])
    gswa_d = din("gswa", [128, 4 * 256])
    snk_d = din("snk", [1, L * 2])
    out_d = nc.dram_tensor("outT", [1024, 2048], F32, kind="ExternalOutput").ap()
    send1 = nc.dram_tensor("send1", [3072, 2048], BF16)
    recv1 = nc.dram_tensor("recv1", [12 * 1024, 2048], BF16)
    send2 = nc.dram_tensor("send2", [256, 8192], BF16)
    recv2 = nc.dram_tensor("recv2", [4 * 256, 8192], BF16)
    mine1 = nc.dram_tensor("mine1", [3 * 1024, 2048], BF16)
    mine2 = nc.dram_tensor("mine2", [4 * 256, 2048], BF16)

    S = Sched(nc, es)
    B = S.B

    uid = [0]

    def sb(name, shape, dt, stack=es):
        uid[0] += 1
        return stack.enter_context(nc.sbuf_tensor(f"s{uid[0]}_{name}", shape, dt))

    xT = sb("xTs", [128, 8, 2048], F32)
    xb = [[B(f"x{dc}_{tg}") for tg in range(4)] for dc in range(8)]
    ident = sb("ident", [128, 128], F32)
    ones_bf = sb("ones_bf", [128, 128], BF16)
    onesf = sb("onesf", [128, 64], F32)
    stair = sb("stair", [128, 128], F32)
    vecs = sb("vecs", [128, L * 128], F32)
    cTs = sb("cTs", [128, 8], F32)
    cact = sb("cact", [128, 8], BF16)
    modt = sb("modt", [128, L * 72], F32)
    Acf = sb("Acf", [128, L * 24], F32)
    Bcf = sb("Bcf", [128, L * 24], F32)
    epsc = sb("epsc", [128, 1], F32)
    gmoba = sb("gmoba", [128, 2 * 1152], F32)
    cfar = sb("cfar", [128, 2], F32)
    gswa = sb("gswa", [128, 4 * 256], F32)
    snk = sb("snk", [65, L * 2], F32)
    esnk = sb("esnk", [65, L * 2], F32)
    bqk = sb("bqk", [64, L * 28], F32)
    constb = B("consts")
    wgu_slots = [(sb(f"wgu{i}", [128, 2048], BF16), B(f"wgu{i}")) for i in range(3)]
    wd_slots = [(sb(f"wd{i}", [128, 2816], BF16), B(f"wd{i}")) for i in range(2)]
    adw_slots = [(sb(f"adw{i}", [128, 2048], BF16), B(f"adw{i}")) for i in range(2)]
    sq_rot = Rot([(sb(f"sq{i}", [128, 512], BF16), B(f"sq{i}")) for i in range(2)])
    t32_rot = Rot([(sb(f"t32{i}", [128, 512], F32), B(f"t32{i}")) for i in range(2)])
    sd_t, sd_b = sb("sd", [128, 512], F32), B("sd")
    rs_rot = Rot([(sb(f"rs{i}", [128, 512], F32), B(f"rs{i}")) for i in range(2)])

    pbt = [es.enter_context(nc.psum_tensor(f"pb{i}", [128, 512], F32)) for i in range(8)]
    pbb = [B(f"pb{i}") for i in range(8)]

    pid = nc.partition_id()
    gidx = pid % 4

    wgu_srcs, wd_srcs = [], []
    for l in range(L):
        for wi in range(2):
            if 3 * l + 2 * wi >= nsub:
                continue
            for ps in range(2):
                for fc in range(22):
                    r0 = ((l * 2 + wi) * 22 + fc) * 128
                    wgu_srcs.append(wgu_d[r0:r0 + 128, :])
                for dc in range(8):
                    r0 = ((l * 2 + wi) * 8 + dc) * 128
                    wd_srcs.append(wd_d[r0:r0 + 128, :])
    wgu_st = WStream(S, wgu_slots, wgu_srcs)
    wd_st = WStream(S, wd_slots, wd_srcs)

    S.dma("sp", [(xT[:, dc, :], xT_d[dc * 128:(dc + 1) * 128, :]) for dc in range(8)],
          rd=[], wr=[b for row in xb for b in row], owner=B("xload"))
    S.dma("sp", [(ident[:], ident_d[:, :]), (stair[:], stair_d[:, :]), (vecs[:], vecs_d[:, :]),
                 (cTs[:], cT_d[:, :]), (gmoba[:], gmoba_d[:, :]), (cfar[:], cfar_d[:, :]),
                 (gswa[:], gswa_d[:, :]), (snk[64:65, :], snk_d[:, :]), (bqk[:], bqk_d[:, :])],
          rd=[], wr=[constb], owner=constb)
    cb2 = B("consts2")
    S.op("dve", lambda e: e.memset(ones_bf[:], 1.0), wr=[cb2])
    S.op("dve", lambda e: e.memset(onesf[:], 1.0), wr=[cb2])
    S.op("dve", lambda e: e.memset(epsc[:], EPS), wr=[cb2])
    S.op("act", lambda e: e.activation(out=cact[:], in_=cTs[:], func=AF.Silu), rd=[constb], wr=[cb2])
    S.op("act", lambda e: e.activation(out=esnk[64:65, :], in_=snk[64:65, :], func=AF.Exp), rd=[constb], wr=[cb2])
    CB = [constb, cb2]

    def mod_task(l):
        mb = B(f"mod{l}")
        adw_st = WStream(S, adw_slots, [adaw_d[(l * 36 + hb) * 128:(l * 36 + hb + 1) * 128, :] for hb in range(36)])
        for hb in range(36):
            wt, wb = adw_st.get()
            for j4 in range(2):
                col = hb * 2 + j4
                for dc in range(8):
                    S.op("pe", lambda e, dc=dc, j4=j4, col=col: e.matmul(
                        pbt[6][:, 128 + col:129 + col], lhsT=wt[:, dc * 256 + j4 * 128: dc * 256 + (j4 + 1) * 128],
                        rhs=cact[:, dc:dc + 1], start=(dc == 0), stop=(dc == 7)),
                        rd=[wb] + CB, wr=[pbb[6]], inc=(dc == 7))
            yield
        mo = modt[:, l * 72:(l + 1) * 72]
        S.op("dve", lambda e: e.tensor_tensor(out=mo, in0=pbt[6][:, 128:200], in1=vecs[:, l * 128: l * 128 + 72], op=ALU.add),
             rd=[pbb[6]] + CB, wr=[mb])
        coef = [0.5, 1.0, 0.5]
        for s in range(3):
            a = Acf[:, (l * 3 + s) * 8:(l * 3 + s + 1) * 8]
            bb = Bcf[:, (l * 3 + s) * 8:(l * 3 + s + 1) * 8]
            sc = modt[:, l * 72 + s * 24 + 8: l * 72 + s * 24 + 16]
            gt = modt[:, l * 72 + s * 24 + 16: l * 72 + s * 24 + 24]
            npre = vecs[:, l * 128 + 72 + s * 8: l * 128 + 72 + s * 8 + 8]
            npost = vecs[:, l * 128 + 96 + s * 8: l * 128 + 96 + s * 8 + 8]
            S.op("dve", lambda e, a=a, sc=sc, npre=npre: e.scalar_tensor_tensor(
                out=a, in0=sc, scalar=1.0, in1=npre, op0=ALU.add, op1=ALU.mult), rd=[mb] + CB, wr=[mb])
            S.op("dve", lambda e, bb=bb, gt=gt, npost=npost, cf=coef[s]: e.scalar_tensor_tensor(
                out=bb, in0=gt, scalar=cf, in1=npost, op0=ALU.mult, op1=ALU.mult), rd=[mb] + CB, wr=[mb])
        yield

    def run_task(t):
        for _ in t:
            pass

    def rstd_from(ssbank_i, scale):
        S.op("act", lambda e: e.activation(out=sd_t[:], in_=pbt[ssbank_i][:, :], func=AF.Sqrt,
                                           bias=epsc[:, 0:1], scale=scale), rd=[pbb[ssbank_i]] + CB, wr=[sd_b])
        rt, rb = rs_rot.next()
        S.op("dve", lambda e: e.reciprocal(out=rt[:], in_=sd_t[:]), rd=[sd_b], wr=[rb])
        return rt, rb

    def norm_x_to_h(l, s, tg, dst_fn, dst_bufs, ssbank_i):
        mb = B(f"mod{l}")
        tok = slice(tg * 512, (tg + 1) * 512)
        for dc in range(8):
            qt, qb = sq_rot.next()
            S.op("act", lambda e, dc=dc, qt=qt: e.activation(out=qt[:], in_=xT[:, dc, tok], func=AF.Square),
                 rd=[xb[dc][tg]], wr=[qb])
            S.op("pe", lambda e, dc=dc, qt=qt: e.matmul(pbt[ssbank_i][:, :], lhsT=ones_bf[:], rhs=qt[:],
                                                         start=(dc == 0), stop=(dc == 7)),
                 rd=[qb] + CB, wr=[pbb[ssbank_i]], inc=True)
        rt, rb = rstd_from(ssbank_i, 1.0 / 1024.0)
        for dc in range(8):
            tt, tb = t32_rot.next()
            acol = Acf[:, (l * 3 + s) * 8 + dc:(l * 3 + s) * 8 + dc + 1]
            shcol = modt[:, l * 72 + s * 24 + dc: l * 72 + s * 24 + dc + 1]
            S.op("dve", lambda e, dc=dc, tt=tt, acol=acol: e.scalar_tensor_tensor(
                out=tt[:], in0=xT[:, dc, tok], scalar=acol, in1=rt[:], op0=ALU.mult, op1=ALU.mult),
                rd=[xb[dc][tg], rb, mb], wr=[tb])
            S.op("act", lambda e, dc=dc, tt=tt, shcol=shcol: e.activation(
                out=dst_fn(dc), in_=tt[:], func=AF.Identity, bias=shcol, scale=1.0),
                rd=[tb, mb], wr=[dst_bufs[dc]])

    def post_residual(l, s, tg, ybf_fn, ybufs, ssbank_i):
        mb = B(f"mod{l}")
        tok = slice(tg * 512, (tg + 1) * 512)
        rt, rb = rstd_from(ssbank_i, 1.0 / 1024.0)
        for dc in range(8):
            tt, tb = t32_rot.next()
            bcol = Bcf[:, (l * 3 + s) * 8 + dc:(l * 3 + s) * 8 + dc + 1]
            S.op("dve", lambda e, dc=dc, tt=tt, bcol=bcol: e.scalar_tensor_tensor(
                out=tt[:], in0=ybf_fn(dc), scalar=bcol, in1=rt[:], op0=ALU.mult, op1=ALU.mult),
                rd=[ybufs[dc], rb, mb], wr=[tb])
            S.op("dve", lambda e, dc=dc, tt=tt: e.tensor_tensor(out=xT[:, dc, tok], in0=xT[:, dc, tok], in1=tt[:], op=ALU.add),
                 rd=[tb, xb[dc][tg]], wr=[xb[dc][tg]])

    def ffn(l, wi, s):
        with ExitStack() as fs:
            hT = sb("hT", [128, 8, 1024], BF16, fs)
            aT = sb("aT", [128, 22, 1024], BF16, fs)
            sg_rot = Rot([(sb(f"sg{i}", [128, 512], BF16, fs), B(f"sg{i}")) for i in range(2)])
            ysq_rot = Rot([(sb(f"ysq{i}", [128, 512], BF16, fs), B(f"ysq{i}")) for i in range(2)])
            hb = [[B(f"h{dc}_{t}") for t in range(2)] for dc in range(8)]
            ab = [[B(f"a{fc}_{t}") for t in range(2)] for fc in range(22)]
            scoped = [b for r in hb for b in r] + [b for r in ab for b in r] + [x[1] for x in sg_rot.items + ysq_rot.items]
            for ps in range(2):
                if DEBUG_CUT < 99 and ps == 1:
                    break
                wd_st.prefetch()
                for t in range(2):
                    tg = 2 * ps + t
                    norm_x_to_h(l, s, tg, lambda dc, t=t: hT[:, dc, t * 512:(t + 1) * 512], [hb[dc][t] for dc in range(8)], 6 + t)
                if DEBUG_CUT <= 1:
                    break
                for fc in range(22):
                    wt, wb = wgu_st.get()
                    for t in range(2):
                        for gu in range(2):
                            bk = 2 * gu + t
                            for dc in range(8):
                                S.op("pe", lambda e, dc=dc, gu=gu, bk=bk, t=t: e.matmul(
                                    pbt[bk][:, :], lhsT=wt[:, gu * 1024 + dc * 128: gu * 1024 + (dc + 1) * 128],
                                    rhs=hT[:, dc, t * 512:(t + 1) * 512], start=(dc == 0), stop=(dc == 7)),
                                    rd=[wb, hb[dc][t]], wr=[pbb[bk]], inc=(dc == 7))
                        st, sbf = sg_rot.next()
                        S.op("act", lambda e, st=st, t=t: e.activation(out=st[:], in_=pbt[t][:, :], func=AF.Silu),
                             rd=[pbb[t]], wr=[sbf])
                        S.op("dve", lambda e, st=st, t=t, fc=fc: e.tensor_tensor(
                            out=aT[:, fc, t * 512:(t + 1) * 512], in0=st[:], in1=pbt[2 + t][:, :], op=ALU.mult),
                            rd=[sbf, pbb[2 + t]], wr=[ab[fc][t]])
                wgu_st.prefetch()
                if DEBUG_CUT <= 2:
                    break
                for dc in range(8):
                    wt, wb = wd_st.get()
                    for t in range(2):
                        bk = 4 + t
                        for fc in range(22):
                            S.op("pe", lambda e, fc=fc, bk=bk, t=t: e.matmul(
                                pbt[bk][:, :], lhsT=wt[:, fc * 128:(fc + 1) * 128],
                                rhs=aT[:, fc, t * 512:(t + 1) * 512], start=(fc == 0), stop=(fc == 21)),
                                rd=[wb, ab[fc][t]], wr=[pbb[bk]], inc=(fc == 21))
                        yt, yb = ysq_rot.next()
                        S.op("act", lambda e, yt=yt, bk=bk: e.activation(out=yt[:], in_=pbt[bk][:, :], func=AF.Square),
                             rd=[pbb[bk]], wr=[yb])
                        S.op("act", lambda e, bk=bk, dc=dc, t=t: e.activation(
                            out=hT[:, dc, t * 512:(t + 1) * 512], in_=pbt[bk][:, :], func=AF.Copy),
                            rd=[pbb[bk]], wr=[hb[dc][t]])
                        S.op("pe", lambda e, yt=yt, t=t, dc=dc: e.matmul(
                            pbt[6 + t][:, :], lhsT=ones_bf[:], rhs=yt[:], start=(dc == 0), stop=(dc == 7)),
                            rd=[yb] + CB, wr=[pbb[6 + t]], inc=True)
                if DEBUG_CUT <= 3:
                    break
                for t in range(2):
                    post_residual(l, s, 2 * ps + t, lambda dc, t=t: hT[:, dc, t * 512:(t + 1) * 512],
                                  [hb[dc][t] for dc in range(8)], 6 + t)
            S.release(scoped)

    send1_b = [B(f"send1_{c}") for c in range(12)]
    recv1_b = [B(f"recv1_{c}") for c in range(12)]
    send2_b = [B(f"send2_{c}") for c in range(4)]
    recv2_b = [B(f"recv2_{c}") for c in range(4)]
    s1 = send1.ap()
    s1q = s1.rearrange("(g c k d) t -> g c k d t", g=4, c=3, k=4, d=64)
    s1v = s1.rearrange("(g c k d) (pl t m) -> g c k (d pl) t m", g=4, c=3, k=4, d=64, pl=2, t=16, m=64)
    r1g = recv1.ap().rearrange("(g c x) t -> g c x t", g=4, c=3)
    m1q = mine1.ap().rearrange("(c r k d) t -> c k d r t", c=3, r=4, k=4, d=64)
    m1v = mine1.ap().rearrange("(c r k d) (pl x) -> c k (d pl) r x", c=3, r=4, k=4, d=64, pl=2)
    s2 = send2.ap()
    m2 = mine2.ap().rearrange("(c r d) t -> c d r t", c=4, r=4, d=64)
    mine1_b = [B(f"mine1_{c}") for c in range(3)]
    mine2_b = [B(f"mine2_{c}") for c in range(4)]

    def ld_q(ct, k):
        return m1q[ct, k, :, :, :]

    def ld_v(ct, k):
        return m1v[ct, k, :, :, :]

    def gather1():
        for c in range(12):
            S.allgather(s1[c * 256:(c + 1) * 256, :], recv1.ap()[c * 1024:(c + 1) * 1024, :], rd=[], wr=[send1_b[c], recv1_b[c]], name=f"g1_{c}")
        for ct in range(3):
            S.dma("sp", [(mine1.ap()[ct * 1024:(ct + 1) * 1024, :],
                          r1g[bass.ds(gidx, 1), ct, :, :].rearrange("o x t -> (o x) t"))],
                  rd=[recv1_b[g_ * 3 + ct] for g_ in range(4)], wr=[mine1_b[ct]], owner=mine1_b[ct])

    def gather2(c):
        S.allgather(s2[c * 64:(c + 1) * 64, :], recv2.ap()[c * 256:(c + 1) * 256, :], rd=[], wr=[send2_b[c], recv2_b[c]], name=f"g2_{c}")
        S.dma("sp", [(mine2.ap()[c * 256:(c + 1) * 256, :],
                      recv2.ap()[c * 256:(c + 1) * 256, :].rearrange("x (g t) -> x g t", g=4)[:, bass.ds(gidx, 1), :].rearrange("x o t -> x (o t)"))],
              rd=[recv2_b[c]], wr=[mine2_b[c]], owner=mine2_b[c])

    def normalize_store(obank_i, exps_col, slot, tgi, ost_rot, bcbank_i, rden_t, rden_b, of_t, of_b):
        ob = pbb[obank_i]
        if exps_col is not None:
            S.op("dve", lambda e: e.tensor_scalar(out=rden_t[64:65, :], in0=pbt[obank_i][64:65, :], scalar1=exps_col,
                                                  scalar2=None, op0=ALU.add), rd=[ob] + CB, wr=[rden_b])
        else:
            S.op("dve", lambda e: e.tensor_copy(out=rden_t[64:65, :], in_=pbt[obank_i][64:65, :]), rd=[ob], wr=[rden_b])
        S.op("dve", lambda e: e.reciprocal(out=rden_t[64:65, :], in_=rden_t[64:65, :]), rd=[rden_b], wr=[rden_b])
        S.op("pe", lambda e: e.matmul(pbt[bcbank_i][0:64, :], lhsT=onesf[64:65, 0:64], rhs=rden_t[64:65, :],
                                      start=True, stop=True), rd=[rden_b] + CB, wr=[pbb[bcbank_i]], inc=True)
        S.op("act", lambda e: e.activation(out=of_t[0:64, :], in_=pbt[obank_i][0:64, :], func=AF.Copy), rd=[ob], wr=[of_b])
        ot, obf = ost_rot.next()
        S.op("dve", lambda e: e.tensor_tensor(out=ot[0:64, :], in0=of_t[0:64, :], in1=pbt[bcbank_i][0:64, :], op=ALU.mult),
             rd=[of_b, pbb[bcbank_i]], wr=[obf])
        S.dma("sp", [(s2[slot * 64:(slot + 1) * 64, tgi * 512:(tgi + 1) * 512], ot[0:64, :])],
              rd=[obf, send2_b[slot]], wr=[], owner=obf)

    def attention(l, bg):
        s = 1
        mb = B(f"mod{l}")
        with ExitStack() as fs:
            hT4 = sb("hT4", [128, 8, 1024], BF16, fs)
            h4b = [[B(f"h4_{dc}_{t}") for t in range(2)] for dc in range(8)]
            wv = sb("wv", [128, 6144], BF16, fs)
            wvb = B("wv")
            bv = sb("bv", [128, 768], F32, fs)
            bvb = B("bv")
            wqk_slots = [(sb(f"wqk{i}", [128, 3584], BF16, fs), B(f"wqk{i}")) for i in range(2)]
            stq_rot = Rot([(sb(f"stq{i}", [64, 7, 512], BF16, fs), B(f"stq{i}")) for i in range(2)])
            stv_rot = Rot([(sb(f"stv{i}", [128, 12, 4, 64], BF16, fs), B(f"stv{i}")) for i in range(2)])
            scoped = [b for r in h4b for b in r] + [wvb, bvb] + [x[1] for x in wqk_slots + stq_rot.items + stv_rot.items]
            S.dma("pool", [(wv[:], wv_d[l * 128:(l + 1) * 128, :])], rd=[], wr=[wvb], owner=wvb)
            S.dma("sp", [(bv[:], bv_d[l * 128:(l + 1) * 128, :])], rd=[], wr=[bvb], owner=bvb)
            wqk_st = WStream(S, wqk_slots, [wqk_d[(l * 4 + gq) * 128:(l * 4 + gq + 1) * 128, :] for hf in range(2) for gq in range(4)])
            wqk_st.prefetch()
            for hf in range(2):
                for t in range(2):
                    tg = 2 * hf + t
                    norm_x_to_h(l, s, tg, lambda dc, t=t: hT4[:, dc, t * 512:(t + 1) * 512],
                                [h4b[dc][t] for dc in range(8)], 6 + t)
                for t in range(2):
                    tg = 2 * hf + t
                    vt, vb_ = stv_rot.next()
                    for tl in range(4):
                        c0 = t * 512 + tl * 128
                        for (bk, lo, hi) in ((0 + 2 * (tl % 2), 0, 512), (1 + 2 * (tl % 2), 512, 768)):
                            for dc in range(8):
                                S.op("pe", lambda e, dc=dc, bk=bk, lo=lo, hi=hi, c0=c0: e.matmul(
                                    pbt[bk][:, 0:hi - lo], lhsT=hT4[:, dc, c0:c0 + 128], rhs=wv[:, dc * 768 + lo: dc * 768 + hi],
                                    start=(dc == 0), stop=(dc == 7)), rd=[h4b[dc][t], wvb], wr=[pbb[bk]], inc=(dc == 7))
                            nvs = (hi - lo) // 64
                            S.op("dve", lambda e, bk=bk, lo=lo, hi=hi, nvs=nvs, tl=tl, vt=vt: e.tensor_tensor(
                                out=vt[:, lo // 64: lo // 64 + nvs, tl, :],
                                in0=pbt[bk][:, 0:hi - lo].rearrange("p (v m) -> p v m", m=64),
                                in1=bv[:, lo:hi].rearrange("p (v m) -> p v m", m=64), op=ALU.add),
                                rd=[pbb[bk], bvb], wr=[vb_])
                    vt5 = vt[:, :, :, :].rearrange("p (g j) t m -> p g j t m", j=3)
                    S.dma("sp", [(s1v[:, cj, kj, :, tg * 4:(tg + 1) * 4, :].rearrange("g p t m -> p g t m"), vt5[:, :, j, :, :])
                                 for (j, cj, kj) in ((0, 0, 3), (1, 1, 2), (2, 2, 2))],
                          rd=[vb_] + send1_b, wr=[], owner=vb_)
                for gq in range(4):
                    wt, wb = wqk_st.get()
                    for t in range(2):
                        tg = 2 * hf + t
                        qt, qb = stq_rot.next()
                        for sl in range(7):
                            bk = 4 + (sl % 2)
                            for dc in range(8):
                                S.op("pe", lambda e, dc=dc, sl=sl, bk=bk, t=t: e.matmul(
                                    pbt[bk][0:64, :], lhsT=wt[:, (sl * 8 + dc) * 64:(sl * 8 + dc + 1) * 64],
                                    rhs=hT4[:, dc, t * 512:(t + 1) * 512], start=(dc == 0), stop=(dc == 7)),
                                    rd=[wb, h4b[dc][t]], wr=[pbb[bk]], inc=(dc == 7))
                            bcol = bqk[:, l * 28 + gq * 7 + sl: l * 28 + gq * 7 + sl + 1]
                            S.op("act", lambda e, sl=sl, bk=bk, qt=qt, bcol=bcol: e.activation(
                                out=qt[:, sl, :], in_=pbt[bk][0:64, :], func=AF.Identity, bias=bcol, scale=1.0),
                                rd=[pbb[bk]] + CB, wr=[qb])
                        cs = slice(tg * 512, (tg + 1) * 512)
                        S.dma("sp", [(s1q[gq, 0, 0:3, :, cs].rearrange("k d t -> d k t"), qt[:, 0:3, :]),
                                     (s1q[gq, 1, 0:2, :, cs].rearrange("k d t -> d k t"), qt[:, 3:5, :]),
                                     (s1q[gq, 2, 0:2, :, cs].rearrange("k d t -> d k t"), qt[:, 5:7, :])],
                              rd=[qb] + send1_b[gq * 3:gq * 3 + 3], wr=[], owner=qb)
            S.release(scoped)
        if DEBUG_CUT == 11:
            return
        gather1()
        if DEBUG_CUT == 12:
            S.wait_all("sp", mine1_b)
            return

        with ExitStack() as fs:
            QTs = [sb(f"QTs{i}", [64, 8192], BF16, fs) for i in range(2)]
            KTs = sb("KTs", [64, 128 + 8192], BF16, fs)
            Vs = sb("Vs", [128, 65, 65], BF16, fs)
            vst = sb("vst", [128, 4, 1024], BF16, fs)
            qsb, ksb, vsb, vstb = B("QTs"), B("KTs"), B("Vs"), B("vst")
            P_rot = Rot([(sb(f"Ps{i}", [128, 256], BF16, fs), B(f"Ps{i}")) for i in range(3)])
            sb_rot = Rot([(sb(f"sbs{i}", [128, 256], F32, fs), B(f"sbs{i}")) for i in range(2)])
            ost_rot = Rot([(sb(f"ost{i}", [64, 512], BF16, fs), B(f"ost{i}")) for i in range(2)])
            rden_t, rden_b = sb("rden", [65, 512], F32, fs), B("rden")
            of_t, of_b = sb("of", [64, 512], F32, fs), B("of")
            scoped = [qsb, ksb, vsb, vstb, rden_b, of_b] + [x[1] for x in P_rot.items + sb_rot.items + ost_rot.items]
            S.dma("sp", [(QTs[i][:, :].rearrange("d (r t) -> d r t", r=4), ld_q(0, i)) for i in range(2)],
                  rd=[mine1_b[0]], wr=[qsb], owner=qsb)
            S.op("dve", lambda e: e.memset(KTs[:, 0:128], 0.0), wr=[ksb])
            S.dma("sp", [(KTs[:, 128:].rearrange("d (r t) -> d r t", r=4), ld_q(0, 2))],
                  rd=[mine1_b[0]], wr=[ksb], owner=ksb)
            S.dma("sp", [(vst[:, :, :], ld_v(0, 3))], rd=[mine1_b[0]], wr=[vstb], owner=vstb)
            S.op("dve", lambda e: e.memset(Vs[:, :, 64:65], 1.0), wr=[vsb])
            S.op("dve", lambda e: e.memset(Vs[:, 0, 0:64], 0.0), wr=[vsb])
            S.op("dve", lambda e: e.tensor_copy(out=Vs[:, 1:65, 0:64], in_=vst[:, :, :].rearrange("p r (t m) -> p (r t) m", m=64)),
                 rd=[vstb], wr=[vsb])
            for i in range(2):
                for tgi in range(16):
                    obk = 4 + (tgi % 2)
                    for qq in range(4):
                        T = tgi * 4 + qq
                        sbk = T % 3
                        for half in range(2):
                            S.op("pe", lambda e, T=T, half=half, sbk=sbk: e.matmul(
                                pbt[sbk][:, half * 128:(half + 1) * 128], lhsT=KTs[:, (T + half) * 128:(T + half + 1) * 128],
                                rhs=QTs[i][:, T * 128:(T + 1) * 128], start=True, stop=True),
                                rd=[ksb, qsb], wr=[pbb[sbk]], inc=(half == 1))
                        st, stb = sb_rot.next()
                        var = i * 2 + (1 if T == 0 else 0)
                        S.op("dve", lambda e, st=st, sbk=sbk, var=var: e.scalar_tensor_tensor(
                            out=st[:], in0=pbt[sbk][:, 0:256], scalar=0.125, in1=gswa[:, var * 256:(var + 1) * 256],
                            op0=ALU.mult, op1=ALU.add), rd=[pbb[sbk]] + CB, wr=[stb])
                        pt, ptb = P_rot.next()
                        S.op("act", lambda e, st=st, pt=pt: e.activation(out=pt[:], in_=st[:], func=AF.Exp), rd=[stb], wr=[ptb])
                        for half in range(2):
                            S.op("pe", lambda e, T=T, half=half, obk=obk, pt=pt, qq=qq: e.matmul(
                                pbt[obk][0:65, qq * 128:(qq + 1) * 128], lhsT=Vs[:, T + half, 0:65],
                                rhs=pt[:, half * 128:(half + 1) * 128], start=(half == 0), stop=(half == 1)),
                                rd=[vsb, ptb], wr=[pbb[obk]], inc=(half == 1))
                    normalize_store(obk, esnk[64:65, l * 2 + i: l * 2 + i + 1], i, tgi, ost_rot, 6 + (tgi % 2),
                                    rden_t, rden_b, of_t, of_b)
                gather2(i)
            S.release(scoped)

        if DEBUG_CUT == 13:
            return
        for i in range(2):
            with ExitStack() as fs:
                QTa = sb("QTa", [96, 8192], BF16, fs)
                KTa = sb("KTa", [96, 8192], BF16, fs)
                Va = sb("Va", [128, 64, 65], BF16, fs)
                vst = sb("vstm", [128, 4, 1024], BF16, fs)
                qab, kab, kib, vab, vstb = B("QTa"), B("KTa"), B("KTi"), B("Va"), B("vstm")
                qmb = [B(f"qm{t}") for t in range(16)]
                km32 = sb("km32", [64, 32], F32, fs)
                kmd = sb("kmd", [64, 32], F32, fs)
                kmh = sb("kmh", [64, 32], BF16, fs)
                kml = sb("kml", [64, 32], BF16, fs)
                kmb = B("km")
                gb_rot = Rot([(sb(f"gb{k}", [128, 32], F32, fs), B(f"gb{k}")) for k in range(2)])
                t8_rot = Rot([(sb(f"t8{k}", [128, 8], F32, fs), B(f"t8{k}")) for k in range(2)])
                mbt_rot = Rot([(sb(f"mbt{k}", [128, 4, 96], F32, fs), B(f"mbt{k}")) for k in range(2)])
                P_rot = Rot([(sb(f"Pm{k}", [128, 512], BF16, fs), B(f"Pm{k}")) for k in range(3)])
                sb_rot = Rot([(sb(f"sbm{k}", [128, 512], F32, fs), B(f"sbm{k}")) for k in range(2)])
                ost_rot = Rot([(sb(f"ostm{k}", [64, 512], BF16, fs), B(f"ostm{k}")) for k in range(2)])
                rden_t, rden_b = sb("rdenm", [65, 512], F32, fs), B("rdenm")
                of_t, of_b = sb("ofm", [64, 512], F32, fs), B("ofm")
                scoped = ([qab, kab, kib, vab, vstb, kmb, rden_b, of_b] + qmb +
                          [x[1] for x in gb_rot.items + t8_rot.items + mbt_rot.items + P_rot.items + sb_rot.items + ost_rot.items])
                S.dma("sp", [(QTa[0:64, :].rearrange("d (r t) -> d r t", r=4), ld_q(1 + i, 0))],
                      rd=[mine1_b[1 + i]], wr=[qab], owner=qab)
                S.dma("sp", [(KTa[0:64, :].rearrange("d (r t) -> d r t", r=4), ld_q(1 + i, 1))],
                      rd=[mine1_b[1 + i]], wr=[kab], owner=kab)
                S.dma("pool", [(KTa[64:96, :], blkind_d[:, :])], rd=[], wr=[kib], owner=kib)
                S.dma("sp", [(vst[:, :, :], ld_v(1 + i, 2))], rd=[mine1_b[1 + i]], wr=[vstb], owner=vstb)
                S.op("dve", lambda e: e.memset(Va[:, :, 64:65], 1.0), wr=[vab])
                S.op("dve", lambda e: e.tensor_copy(out=Va[:, :, 0:64], in_=vst[:, :, :].rearrange("p r (t m) -> p (r t) m", m=64)),
                     rd=[vstb], wr=[vab])
                S.op("dve", lambda e: e.tensor_reduce(out=km32[:, :], in_=KTa[0:64, :].rearrange("d (b k) -> d b k", k=256),
                                                      axis=AX.X, op=ALU.add), rd=[kab], wr=[kmb])
                S.op("dve", lambda e: e.tensor_copy(out=kmh[:, :], in_=km32[:, :]), rd=[kmb], wr=[kmb])
                S.op("dve", lambda e: e.tensor_tensor(out=kmd[:, :], in0=km32[:, :], in1=kmh[:, :], op=ALU.subtract), rd=[kmb], wr=[kmb])
                S.op("dve", lambda e: e.tensor_copy(out=kml[:, :], in_=kmd[:, :]), rd=[kmb], wr=[kmb])
                for tgi in range(16):
                    mt, mtb = mbt_rot.next()
                    for qq in range(4):
                        T = tgi * 4 + qq
                        cur = T // 2
                        if cur >= 3:
                            S.op("pe", lambda e, T=T, qq=qq: e.matmul(pbt[6][:, qq * 32:(qq + 1) * 32], lhsT=QTa[0:64, T * 128:(T + 1) * 128],
                                                                      rhs=kmh[:, :], start=True, stop=False), rd=[qab, kmb], wr=[pbb[6]], inc=False)
                            S.op("pe", lambda e, T=T, qq=qq: e.matmul(pbt[6][:, qq * 32:(qq + 1) * 32], lhsT=QTa[0:64, T * 128:(T + 1) * 128],
                                                                      rhs=kml[:, :], start=False, stop=True), rd=[qab, kmb], wr=[pbb[6]], inc=True)
                            gt, gtb = gb_rot.next()
                            S.op("dve", lambda e, gt=gt, qq=qq, cur=cur: e.tensor_tensor(
                                out=gt[:], in0=pbt[6][:, qq * 32:(qq + 1) * 32], in1=stair[:, 32 - cur:64 - cur], op=ALU.add),
                                rd=[pbb[6]] + CB, wr=[gtb])
                            t8, t8b = t8_rot.next()
                            S.op("dve", lambda e, gt=gt, t8=t8: e.max(out=t8[:], in_=gt[:]), rd=[gtb], wr=[t8b])
                            S.op("dve", lambda e, gt=gt, t8=t8, mt=mt, qq=qq: e.tensor_scalar(
                                out=mt[:, qq, 64:96], in0=gt[:], scalar1=t8[:, 2:3], scalar2=1.0, op0=ALU.is_ge, op1=ALU.subtract),
                                rd=[gtb, t8b], wr=[mtb])
                            S.op("dve", lambda e, mt=mt, qq=qq, cur=cur: e.memset(mt[:, qq, 64 + cur:65 + cur], 0.0), wr=[mtb])
                        else:
                            S.op("dve", lambda e, mt=mt, qq=qq, cur=cur: e.tensor_copy(
                                out=mt[:, qq, 64:96], in_=stair[:, 64 + 31 - cur:64 + 63 - cur]), rd=CB, wr=[mtb])
                        S.op("pe", lambda e, mt=mt, qq=qq: e.transpose(out=pbt[7][0:96, qq * 128:(qq + 1) * 128], in_=mt[:, qq, :],
                                                                       identity=ident[:, :]), rd=[mtb] + CB, wr=[pbb[7]], inc=True)
                    S.op("act", lambda e, tgi=tgi: e.activation(out=QTa[64:96, tgi * 512:(tgi + 1) * 512], in_=pbt[7][64:96, :], func=AF.Copy),
                         rd=[pbb[7]], wr=[qmb[tgi]])
                for tgi in range(16):
                    obk = 4 + (tgi % 2)
                    nkt = 4 * tgi + 4
                    for kt in range(nkt):
                        sbk = kt % 3
                        S.op("pe", lambda e, kt=kt, sbk=sbk, tgi=tgi: e.matmul(
                            pbt[sbk][:, :], lhsT=KTa[0:96, kt * 128:(kt + 1) * 128], rhs=QTa[0:96, tgi * 512:(tgi + 1) * 512],
                            start=True, stop=True), rd=[kab, kib, qab, qmb[tgi]], wr=[pbb[sbk]], inc=True)
                        pt, ptb = P_rot.next()
                        rel = kt - 4 * tgi
                        if rel >= -2:
                            j0 = i * 1152 + 384 - 128 * rel
                            st, stb = sb_rot.next()
                            S.op("dve", lambda e, st=st, sbk=sbk, j0=j0: e.scalar_tensor_tensor(
                                out=st[:], in0=pbt[sbk][:, :], scalar=0.125, in1=gmoba[:, j0:j0 + 512], op0=ALU.mult, op1=ALU.add),
                                rd=[pbb[sbk]] + CB, wr=[stb])
                            S.op("act", lambda e, st=st, pt=pt: e.activation(out=pt[:], in_=st[:], func=AF.Exp), rd=[stb], wr=[ptb])
                        else:
                            S.op("act", lambda e, pt=pt, sbk=sbk: e.activation(out=pt[:], in_=pbt[sbk][:, :], func=AF.Exp,
                                                                              bias=cfar[:, i:i + 1], scale=0.125), rd=[pbb[sbk]] + CB, wr=[ptb])
                        S.op("pe", lambda e, kt=kt, obk=obk, pt=pt, nkt=nkt: e.matmul(
                            pbt[obk][0:65, :], lhsT=Va[:, kt, 0:65], rhs=pt[:], start=(kt == 0), stop=(kt == nkt - 1)),
                            rd=[vab, ptb], wr=[pbb[obk]], inc=(kt == nkt - 1))
                    normalize_store(obk, None, 2 + i, tgi, ost_rot, 3, rden_t, rden_b, of_t, of_b)
                    if bg is not None:
                        next(bg, None)
                        next(bg, None)
                gather2(2 + i)
                S.release(scoped)
        if bg is not None:
            run_task(bg)
        if DEBUG_CUT == 14:
            return

        with ExitStack() as fs:
            wo = sb("wo", [128, 8192], BF16, fs)
            wob = B("wo")
            OT_rot = Rot([(sb(f"OT{k}", [128, 8, 512], BF16, fs), B(f"OT{k}")) for k in range(2)])
            osq = sb("osq", [128, 8, 512], BF16, fs)
            osqb = B("osq")
            OnT = sb("OnT", [128, 8, 512], BF16, fs)
            onb = [B(f"on{k}") for k in range(8)]
            ybf = sb("ybf", [128, 8, 512], BF16, fs)
            ybb = [B(f"yb{k}") for k in range(8)]
            ysq_rot = Rot([(sb(f"ysqo{k}", [128, 512], BF16, fs), B(f"ysqo{k}")) for k in range(2)])
            rsa_t, rsa_b = sb("rsa", [128, 512], F32, fs), B("rsa")
            scoped = [wob, osqb, rsa_b] + onb + ybb + [x[1] for x in OT_rot.items + ysq_rot.items]
            S.dma("pool", [(wo[:], wout_d[l * 128:(l + 1) * 128, :])], rd=[], wr=[wob], owner=wob)
            for tg in range(4):
                ot, otb = OT_rot.next()
                S.dma("sp", [(ot[:, :, :].rearrange("p (r par) t -> p r par t", par=2)[(sl % 2) * 64:(sl % 2) * 64 + 64, :, sl // 2, :], m2[sl, :, :, tg * 512:(tg + 1) * 512])
                             for sl in range(4)], rd=mine2_b, wr=[otb], owner=otb)
                S.op("dve", lambda e, ot=ot: e.tensor_tensor(out=osq[:, :, :], in0=ot[:, :, :], in1=ot[:, :, :], op=ALU.mult),
                     rd=[otb], wr=[osqb])
                for par in range(2):
                    for k in range(4):
                        kc = 2 * k + par
                        S.op("pe", lambda e, kc=kc, k=k, par=par: e.matmul(pbt[6 + par][:, :], lhsT=ones_bf[:], rhs=osq[:, kc, :],
                                                                         start=(k == 0), stop=(k == 3)),
                             rd=[osqb] + CB, wr=[pbb[6 + par]], inc=(k == 3))
                S.op("act", lambda e: e.activation(out=sd_t[:], in_=pbt[6][:, :], func=AF.Sqrt, bias=epsc[:, 0:1], scale=1.0 / 512.0),
                     rd=[pbb[6]] + CB, wr=[sd_b])
                S.op("dve", lambda e: e.reciprocal(out=rsa_t[:], in_=sd_t[:]), rd=[sd_b], wr=[rsa_b])
                rbt, rbb = rstd_from(7, 1.0 / 512.0)
                for kc in range(8):
                    rr, rrb = (rsa_t, rsa_b) if kc % 2 == 0 else (rbt, rbb)
                    gcol = vecs[:, l * 128 + 120 + kc: l * 128 + 121 + kc]
                    S.op("dve", lambda e, kc=kc, rr=rr, gcol=gcol, ot=ot: e.scalar_tensor_tensor(
                        out=OnT[:, kc, :], in0=ot[:, kc, :], scalar=gcol, in1=rr[:], op0=ALU.mult, op1=ALU.mult),
                        rd=[otb, rrb] + CB, wr=[onb[kc]])
                for dm in range(8):
                    bk = 4 + (dm % 2)
                    for kc in range(8):
                        S.op("pe", lambda e, kc=kc, dm=dm, bk=bk: e.matmul(
                            pbt[bk][:, :], lhsT=wo[:, kc * 1024 + dm * 128: kc * 1024 + (dm + 1) * 128], rhs=OnT[:, kc, :],
                            start=(kc == 0), stop=(kc == 7)), rd=[wob, onb[kc]], wr=[pbb[bk]], inc=(kc == 7))
                    yt, yb = ysq_rot.next()
                    S.op("act", lambda e, yt=yt, bk=bk: e.activation(out=yt[:], in_=pbt[bk][:, :], func=AF.Square), rd=[pbb[bk]], wr=[yb])
                    S.op("act", lambda e, dm=dm, bk=bk: e.activation(out=ybf[:, dm, :], in_=pbt[bk][:, :], func=AF.Copy),
                         rd=[pbb[bk]], wr=[ybb[dm]])
                    S.op("pe", lambda e, yt=yt, dm=dm: e.matmul(pbt[3][:, :], lhsT=ones_bf[:], rhs=yt[:], start=(dm == 0), stop=(dm == 7)),
                         rd=[yb] + CB, wr=[pbb[3]], inc=True)
                post_residual(l, s, tg, lambda dc: ybf[:, dc, :], ybb, 3)
            S.release(scoped)

    with ExitStack() as zs:
        zt = sb("zpad", [64, 2048], BF16, zs)
        zb = B("zpad")
        S.op("dve", lambda e: e.memset(zt[:], 0.0), wr=[zb])
        S.dma("sp", [(s1q[g_, ct, 3, :, :], zt[:]) for g_ in range(4) for ct in (1, 2)],
              rd=[zb] + send1_b, wr=[], owner=zb)
        S.release([zb])
    run_task(mod_task(0))
    sub = 0
    for l in range(L):
        if sub < nsub:
            ffn(l, 0, 0)
            sub += 1
        if sub < nsub:
            bg = mod_task(l + 1) if (l + 1 < L and 3 * (l + 1) < nsub) else None
            attention(l, bg)
            sub += 1
        elif l + 1 < L:
            pass
        if sub < nsub:
            ffn(l, 1, 2)
            sub += 1
    allx = [b for row in xb for b in row]
    outb = B("outb")
    S.dma("sp", [(out_d[dc * 128:(dc + 1) * 128, :], xT[:, dc, :]) for dc in range(8)], rd=allx, wr=[outb], owner=outb)
    S.wait_all("sp", [outb])
    es.close()
    return nc


def _t5_bucket_np(dist):
    n = np.maximum(dist, 0)
    nf = np.maximum(n, 1).astype(np.float32)
    large = 16 + (np.log(nf / np.float32(16)) / np.float32(math.log(128 / 16)) * np.float32(16)).astype(np.int32)
    large = np.minimum(large, 31)
    return np.where(n < 16, n, large)


def _slot_cols():
    qk, v = [], []
    for g in range(4):
        qk += [64 * (2 * g), 64 * (2 * g + 1), 512 + 64 * (g // 2), 768 + 64 * (2 * g), 1280 + 64 * (2 * g),
               768 + 64 * (2 * g + 1), 1280 + 64 * (2 * g + 1)]
        v += [640 + 64 * (g // 2), 1792 + 64 * (2 * g), 1792 + 64 * (2 * g + 1)]
    return qk, v


def prep_inputs(inp, L=DEPTH, l0=0, x=None):
    f = np.float32
    x = np.asarray(inp["x"], f) if x is None else x
    c = np.asarray(inp["c"], f)
    rel = np.asarray(inp["rel_bias"], f)
    ada_w = np.asarray(inp["ada_w"][l0:l0 + L], f)
    ada_b = np.asarray(inp["ada_b"][l0:l0 + L], f)
    npre = np.asarray(inp["norm_pre"][l0:l0 + L], f)
    npost = np.asarray(inp["norm_post"][l0:l0 + L], f)
    wg = np.asarray(inp["ffn_w_gate"][l0:l0 + L], f)
    wu = np.asarray(inp["ffn_w_up"][l0:l0 + L], f)
    wdn = np.asarray(inp["ffn_w_down"][l0:l0 + L], f)
    w_in = np.asarray(inp["mix_w_in"][l0:l0 + L], f)
    b_in = np.asarray(inp["mix_b_in"][l0:l0 + L], f)
    w_out = np.asarray(inp["mix_w_out"][l0:l0 + L], f)
    sinks = np.asarray(inp["attn_sinks"][l0:l0 + L], f)
    gain = np.asarray(inp["group_gain"][l0:l0 + L], f)

    g6 = wg.reshape(L, 2, 8, 128, 22, 128)
    u6 = wu.reshape(L, 2, 8, 128, 22, 128)
    gu = np.stack([g6, u6], axis=2)
    wgu = np.ascontiguousarray(gu.transpose(0, 1, 5, 4, 2, 3, 6)).reshape(L * 2 * 22 * 128, 2048)
    d6 = wdn.reshape(L, 2, 22, 128, 8, 128)
    wd = np.ascontiguousarray(d6.transpose(0, 1, 4, 3, 2, 5)).reshape(L * 2 * 8 * 128, 2816)
    qk_cols, v_cols = _slot_cols()
    qk_idx = np.concatenate([np.arange(cb, cb + 64) for cb in qk_cols])
    v_idx = np.concatenate([np.arange(cb, cb + 64) for cb in v_cols])
    wq = w_in[:, :, qk_idx].reshape(L, 8, 128, 4, 7, 64)
    wqk = np.ascontiguousarray(wq.transpose(0, 3, 2, 4, 1, 5)).reshape(L * 4 * 128, 3584)
    wvv = w_in[:, :, v_idx].reshape(L, 8, 128, 768)
    wv = np.ascontiguousarray(wvv.transpose(0, 2, 1, 3)).reshape(L * 128, 6144)
    bqk = np.ascontiguousarray(b_in[:, qk_idx].reshape(L, 28, 64).transpose(2, 0, 1)).reshape(64, L * 28)
    bv = np.ascontiguousarray(np.broadcast_to(b_in[:, None, v_idx], (L, 128, 768))).reshape(L * 128, 768)
    rowperm = np.concatenate([np.concatenate([np.arange(128 * g, 128 * g + 128), np.arange(512 + 128 * g, 512 + 128 * g + 128)])
                              for g in range(4)])
    wo = w_out[:, rowperm, :].reshape(L, 8, 128, 1024)
    wout = np.ascontiguousarray(wo.transpose(0, 2, 1, 3)).reshape(L * 128, 8192)
    aw = ada_w.reshape(L, 8, 128, 36, 256)
    adaw = np.ascontiguousarray(aw.transpose(0, 3, 2, 1, 4)).reshape(L * 36 * 128, 2048)
    vecs = np.zeros((128, L * 128), f)
    for l in range(L):
        vecs[:, l * 128: l * 128 + 72] = ada_b[l].reshape(72, 128).T
        vecs[:, l * 128 + 72: l * 128 + 96] = npre[l].reshape(24, 128).T
        vecs[:, l * 128 + 96: l * 128 + 120] = npost[l].reshape(24, 128).T
        vecs[:, l * 128 + 120: l * 128 + 128] = gain[l][rowperm].reshape(8, 128).T
    ident = np.eye(128, dtype=f)
    blkind = np.zeros((32, 8192), f)
    for b in range(32):
        blkind[b, b * 256:(b + 1) * 256] = MASKV
    stair = np.zeros((128, 128), f)
    stair[:, 32:64] = -1e30
    stair[:, 96:128] = -1.0

    kk = np.arange(128)[:, None]
    shared = dict(wgu=wgu, wd=wd, wqk=wqk, wv=wv, bqk=bqk, bv=bv, wout=wout, adaw=adaw, vecs=vecs,
                  ident=ident, blkind=blkind, stair=stair)
    in_maps = []
    for core in range(8):
        bt, g = core // 4, core % 4
        m = dict(shared)
        m["xT"] = np.ascontiguousarray(x[bt, g * 2048:(g + 1) * 2048, :].T)
        m["cT"] = np.ascontiguousarray(c[bt].reshape(8, 128).T)
        gm = np.zeros((128, 2 * 1152), f)
        cf = np.zeros((128, 2), f)
        for i in range(2):
            h = 8 + 2 * g + i
            d = np.arange(1152)[None, :] - 384 - kk
            val = rel[_t5_bucket_np(d), h]
            gm[:, i * 1152:(i + 1) * 1152] = np.where(d >= 0, val, f(-30000.0))
            cf[:, i] = rel[31, h]
        m["gmoba"] = gm
        m["cfar"] = cf
        gs = np.zeros((128, 4 * 256), f)
        qq = np.arange(128)[None, :]
        for i in range(2):
            h = 2 * g + i
            d0 = qq + 128 - kk
            d1 = qq - kk
            a0 = np.where((d0 >= 0) & (d0 < 128), rel[_t5_bucket_np(d0), h], f(-30000.0))
            a1 = np.where((d1 >= 0) & (d1 < 128), rel[_t5_bucket_np(d1), h], f(-30000.0))
            gs[:, (i * 2) * 256:(i * 2) * 256 + 128] = a0
            gs[:, (i * 2) * 256 + 128:(i * 2 + 1) * 256] = a1
            gs[:, (i * 2 + 1) * 256:(i * 2 + 1) * 256 + 128] = f(-30000.0)
            gs[:, (i * 2 + 1) * 256 + 128:(i * 2 + 2) * 256] = a1
        m["gswa"] = gs
        m["snk"] = np.ascontiguousarray(sinks[:, 2 * g:2 * g + 2].reshape(1, L * 2))
        in_maps.append(m)
    return in_maps


_NC_CACHE = {}


def run(inputs, L=DEPTH, nsub=None, l0=0, x=None):
    key = (L, nsub)
    if key not in _NC_CACHE:
        _NC_CACHE[key] = build(L, nsub)
    nc = _NC_CACHE[key]
    in_maps = prep_inputs(inputs, L, l0, x)
    res = run_bass_kernel_spmd(nc, in_maps, core_ids=list(range(8)))
    out = np.zeros((2, 8192, 1024), np.float32)
    for core in range(8):
        bt, g = core // 4, core % 4
        out[bt, g * 2048:(g + 1) * 2048, :] = np.asarray(res.results[core]["outT"], np.float32).T
    return out


LAYERS_PER_LAUNCH = 4


def kernel(**inputs):
    x = None
    for l0 in range(0, DEPTH, LAYERS_PER_LAUNCH):
        x = run(inputs, LAYERS_PER_LAUNCH, None, l0, x)
    return x
```

```python
import math
import numpy as np
from contextlib import ExitStack
import concourse.bass as bass
import concourse.mybir as mybir
from concourse.bass_utils import run_bass_kernel_spmd

F32 = mybir.dt.float32
BF16 = mybir.dt.bfloat16
ALU = mybir.AluOpType
AF = mybir.ActivationFunctionType
AX = mybir.AxisListType

DEPTH = 4
DEBUG_CUT = 99
EPS = 1e-6
MASKV = 240000.0


class Tok:
    __slots__ = ("sem", "key", "val", "eng")

    def __init__(self, sem, key, val, eng):
        self.sem, self.key, self.val, self.eng = sem, key, val, eng


class Buf:
    def __init__(self, name):
        self.name = name
        self.w = None
        self.r = {}
        self.dsem = None
        self.dkey = None
        self.dcnt = 0


class Sched:
    def __init__(self, nc, es):
        self.nc, self.es = nc, es
        self.E = {"pe": nc.tensor, "act": nc.scalar, "dve": nc.vector, "pool": nc.gpsimd, "sp": nc.sync}
        self.sem, self.key, self.cnt = {}, {}, {}
        self.known = {e: {} for e in self.E}
        self.nsem = 0
        self.bufs = {}
        for e in self.E:
            self.new_epoch(e)

    def newsem(self, name):
        self.nsem += 1
        nm = f"{name}_{self.nsem}"
        return self.es.enter_context(self.nc.semaphore(nm)), nm

    def new_epoch(self, e):
        self.sem[e], self.key[e] = self.newsem("e" + e)
        self.cnt[e] = 0

    def B(self, name):
        b = self.bufs.get(name)
        if b is None:
            b = self.bufs[name] = Buf(name)
        return b

    def _wait(self, e, toks, skip_pe=False, defer=False):
        need = {}
        for t in toks:
            if t is None:
                continue
            if skip_pe and t.eng == "pe":
                continue
            cur = need.get(t.key)
            if cur is None or cur.val < t.val:
                need[t.key] = t
        kn = self.known[e]
        todo = [t for k, t in need.items() if kn.get(k, 0) < t.val]
        last = None
        if defer and todo:
            last = todo.pop()
            kn[last.key] = last.val
        for t in todo:
            self.E[e].wait_ge(t.sem, t.val)
            kn[t.key] = t.val
        return last

    @staticmethod
    def _deps(rd, wr):
        toks = []
        for b in rd:
            toks.append(b.w)
        for b in wr:
            toks.append(b.w)
            toks.extend(b.r.values())
        return toks

    @staticmethod
    def _record(tok, rd, wr):
        for b in rd:
            c = b.r.get(tok.key)
            if c is None or c.val < tok.val:
                b.r[tok.key] = tok
        for b in wr:
            b.w = tok
            b.r = {}

    def op(self, e, fn, rd=(), wr=(), inc=True):
        last = self._wait(e, self._deps(rd, wr), skip_pe=(e == "pe"), defer=True)
        ins = fn(self.E[e])
        if last is not None:
            ins._wait_ge(last.sem, last.val)
        if inc:
            ins.then_inc(self.sem[e], 1)
            self.cnt[e] += 1
            tok = Tok(self.sem[e], self.key[e], self.cnt[e], e)
        else:
            tok = Tok(self.sem[e], self.key[e], self.cnt[e] + 1, e)
        self._record(tok, rd, wr)
        return tok

    def dma(self, q, pairs, rd, wr, owner):
        if owner.dsem is None:
            owner.dsem, owner.dkey = self.newsem("d")
        last = self._wait(q, self._deps(rd, wr), defer=True)
        for (o, i) in pairs:
            ins = self.E[q].dma_start(out=o, in_=i)
            if last is not None:
                ins._wait_ge(last.sem, last.val)
                last = None
            ins.then_inc(owner.dsem, 16)
            owner.dcnt += 16
        tok = Tok(owner.dsem, owner.dkey, owner.dcnt, "dma")
        self._record(tok, rd, wr)
        return tok

    def allgather(self, src_ap, dst_ap, rd, wr, name):
        if not hasattr(self, "ccs"):
            self.ccs = {}
        if name not in self.ccs:
            self.ccs[name] = list(self.newsem("cc")) + [0]
        ent = self.ccs[name]
        ent[2] += 1
        sem, key = ent[0], ent[1]
        self._wait("pool", self._deps(rd, wr))
        self.nc.gpsimd.collective_compute(
            "AllGather", ALU.bypass, replica_groups=[[0, 1, 2, 3], [4, 5, 6, 7]],
            ins=[src_ap.opt()], outs=[dst_ap.opt()],
        ).then_inc(sem)
        tok = Tok(sem, key, ent[2], "cc")
        self._record(tok, rd, wr)
        return tok

    def release(self, bufs):
        toks = []
        for b in bufs:
            toks.append(b.w)
            toks.extend(b.r.values())
        for e in self.E:
            self._wait(e, toks)

    def wait_all(self, e, bufs):
        toks = []
        for b in bufs:
            toks.append(b.w)
            toks.extend(b.r.values())
        self._wait(e, toks)


class Rot:
    def __init__(self, items):
        self.items, self.i = items, 0

    def next(self):
        it = self.items[self.i % len(self.items)]
        self.i += 1
        return it


class WStream:
    def __init__(self, S, slots, srcs):
        self.S, self.slots, self.srcs = S, slots, srcs
        self.issued = 0
        self.consumed = 0

    def prefetch(self, ahead=None):
        n = len(self.slots) if ahead is None else ahead
        while self.issued < len(self.srcs) and self.issued < self.consumed + n:
            t, b = self.slots[self.issued % len(self.slots)]
            self.S.dma("pool", [(t[:], self.srcs[self.issued])], rd=[], wr=[b], owner=b)
            self.issued += 1

    def get(self):
        self.prefetch()
        it = self.slots[self.consumed % len(self.slots)]
        self.consumed += 1
        return it


def build(L=DEPTH, nsub=None):
    if nsub is None:
        nsub = 3 * L
    nc = bass.Bass("TRN2", target_bir_lowering=False)
    es = ExitStack()
    es.enter_context(nc.allow_low_precision("bf16 matmuls with fp32 accumulation"))

    def din(name, shape, dt=F32):
        return nc.dram_tensor(name, shape, dt, kind="ExternalInput").ap()

    xT_d = din("xT", [1024, 2048])
    cT_d = din("cT", [128, 8])
    wgu_d = din("wgu", [L * 2 * 22 * 128, 2048])
    wd_d = din("wd", [L * 2 * 8 * 128, 2816])
    wqk_d = din("wqk", [L * 4 * 128, 3584])
    wv_d = din("wv", [L * 128, 6144])
    bqk_d = din("bqk", [64, L * 28])
    bv_d = din("bv", [L * 128, 768])
    wout_d = din("wout", [L * 128, 8192])
    adaw_d = din("adaw", [L * 36 * 128, 2048])
    vecs_d = din("vecs", [128, L * 128])
    ident_d = din("ident", [128, 128])
    blkind_d = din("blkind", [32, 8192])
    stair_d = din("stair", [128, 128])
    gmoba_d = din("gmoba", [128, 2 * 1152])
    cfar_d = din("cfar", [128, 2])
    gswa_d = din("gswa", [128, 4 * 256])
    snk_d = din("snk", [1, L * 2])
    out_d = nc.dram_tensor("outT", [1024, 2048], F32, kind="ExternalOutput").ap()
    send1 = nc.dram_tensor("send1", [3072, 2048], BF16)
    recv1 = nc.dram_tensor("recv1", [12 * 1024, 2048], BF16)
    send2 = nc.dram_tensor("send2", [256, 8192], BF16)
    recv2 = nc.dram_tensor("recv2", [4 * 256, 8192], BF16)
    mine1 = nc.dram_tensor("mine1", [3 * 1024, 2048], BF16)
    mine2 = nc.dram_tensor("mine2", [4 * 256, 2048], BF16)

    S = Sched(nc, es)
    B = S.B

    uid = [0]

    def sb(name, shape, dt, stack=es):
        uid[0] += 1
        return stack.enter_context(nc.sbuf_tensor(f"s{uid[0]}_{name}", shape, dt))

    xT = sb("xTs", [128, 8, 2048], F32)
    xb = [[B(f"x{dc}_{tg}") for tg in range(4)] for dc in range(8)]
    ident = sb("ident", [128, 128], F32)
    ones_bf = sb("ones_bf", [128, 128], BF16)
    onesf = sb("onesf", [128, 64], F32)
    stair = sb("stair", [128, 128], F32)
    vecs = sb("vecs", [128, L * 128], F32)
    cTs = sb("cTs", [128, 8], F32)
    cact = sb("cact", [128, 8], BF16)
    modt = sb("modt", [128, L * 72], F32)
    Acf = sb("Acf", [128, L * 24], F32)
    Bcf = sb("Bcf", [128, L * 24], F32)
    epsc = sb("epsc", [128, 1], F32)
    gmoba = sb("gmoba", [128, 2 * 1152], F32)
    cfar = sb("cfar", [128, 2], F32)
    gswa = sb("gswa", [128, 4 * 256], F32)
    snk = sb("snk", [65, L * 2], F32)
    esnk = sb("esnk", [65, L * 2], F32)
    bqk = sb("bqk", [64, L * 28], F32)
    constb = B("consts")
    wgu_slots = [(sb(f"wgu{i}", [128, 2048], BF16), B(f"wgu{i}")) for i in range(3)]
    wd_slots = [(sb(f"wd{i}", [128, 2816], BF16), B(f"wd{i}")) for i in range(2)]
    adw_slots = [(sb(f"adw{i}", [128, 2048], BF16), B(f"adw{i}")) for i in range(2)]
    sq_rot = Rot([(sb(f"sq{i}", [128, 512], BF16), B(f"sq{i}")) for i in range(2)])
    t32_rot = Rot([(sb(f"t32{i}", [128, 512], F32), B(f"t32{i}")) for i in range(2)])
    sd_t, sd_b = sb("sd", [128, 512], F32), B("sd")
    rs_rot = Rot([(sb(f"rs{i}", [128, 512], F32), B(f"rs{i}")) for i in range(2)])

    pbt = [es.enter_context(nc.psum_tensor(f"pb{i}", [128, 512], F32)) for i in range(8)]
    pbb = [B(f"pb{i}") for i in range(8)]

    pid = nc.partition_id()
    gidx = pid % 4

    wgu_srcs, wd_srcs = [], []
    for l in range(L):
        for wi in range(2):
            if 3 * l + 2 * wi >= nsub:
                continue
            for ps in range(2):
                for fc in range(22):
                    r0 = ((l * 2 + wi) * 22 + fc) * 128
                    wgu_srcs.append(wgu_d[r0:r0 + 128, :])
                for dc in range(8):
                    r0 = ((l * 2 + wi) * 8 + dc) * 128
                    wd_srcs.append(wd_d[r0:r0 + 128, :])
    wgu_st = WStream(S, wgu_slots, wgu_srcs)
    wd_st = WStream(S, wd_slots, wd_srcs)

    S.dma("sp", [(xT[:, dc, :], xT_d[dc * 128:(dc + 1) * 128, :]) for dc in range(8)],
          rd=[], wr=[b for row in xb for b in row], owner=B("xload"))
    S.dma("sp", [(ident[:], ident_d[:, :]), (stair[:], stair_d[:, :]), (vecs[:], vecs_d[:, :]),
                 (cTs[:], cT_d[:, :]), (gmoba[:], gmoba_d[:, :]), (cfar[:], cfar_d[:, :]),
                 (gswa[:], gswa_d[:, :]), (snk[64:65, :], snk_d[:, :]), (bqk[:], bqk_d[:, :])],
          rd=[], wr=[constb], owner=constb)
    cb2 = B("consts2")
    S.op("dve", lambda e: e.memset(ones_bf[:], 1.0), wr=[cb2])
    S.op("dve", lambda e: e.memset(onesf[:], 1.0), wr=[cb2])
    S.op("dve", lambda e: e.memset(epsc[:], EPS), wr=[cb2])
    S.op("act", lambda e: e.activation(out=cact[:], in_=cTs[:], func=AF.Silu), rd=[constb], wr=[cb2])
    S.op("act", lambda e: e.activation(out=esnk[64:65, :], in_=snk[64:65, :], func=AF.Exp), rd=[constb], wr=[cb2])
    CB = [constb, cb2]

    def mod_task(l):
        mb = B(f"mod{l}")
        adw_st = WStream(S, adw_slots, [adaw_d[(l * 36 + hb) * 128:(l * 36 + hb + 1) * 128, :] for hb in range(36)])
        for hb in range(36):
            wt, wb = adw_st.get()
            for j4 in range(2):
                col = hb * 2 + j4
                for dc in range(8):
                    S.op("pe", lambda e, dc=dc, j4=j4, col=col: e.matmul(
                        pbt[6][:, 128 + col:129 + col], lhsT=wt[:, dc * 256 + j4 * 128: dc * 256 + (j4 + 1) * 128],
                        rhs=cact[:, dc:dc + 1], start=(dc == 0), stop=(dc == 7)),
                        rd=[wb] + CB, wr=[pbb[6]], inc=(dc == 7))
            yield
        mo = modt[:, l * 72:(l + 1) * 72]
        S.op("dve", lambda e: e.tensor_tensor(out=mo, in0=pbt[6][:, 128:200], in1=vecs[:, l * 128: l * 128 + 72], op=ALU.add),
             rd=[pbb[6]] + CB, wr=[mb])
        coef = [0.5, 1.0, 0.5]
        for s in range(3):
            a = Acf[:, (l * 3 + s) * 8:(l * 3 + s + 1) * 8]
            bb = Bcf[:, (l * 3 + s) * 8:(l * 3 + s + 1) * 8]
            sc = modt[:, l * 72 + s * 24 + 8: l * 72 + s * 24 + 16]
            gt = modt[:, l * 72 + s * 24 + 16: l * 72 + s * 24 + 24]
            npre = vecs[:, l * 128 + 72 + s * 8: l * 128 + 72 + s * 8 + 8]
            npost = vecs[:, l * 128 + 96 + s * 8: l * 128 + 96 + s * 8 + 8]
            S.op("dve", lambda e, a=a, sc=sc, npre=npre: e.scalar_tensor_tensor(
                out=a, in0=sc, scalar=1.0, in1=npre, op0=ALU.add, op1=ALU.mult), rd=[mb] + CB, wr=[mb])
            S.op("dve", lambda e, bb=bb, gt=gt, npost=npost, cf=coef[s]: e.scalar_tensor_tensor(
                out=bb, in0=gt, scalar=cf, in1=npost, op0=ALU.mult, op1=ALU.mult), rd=[mb] + CB, wr=[mb])
        yield

    def run_task(t):
        for _ in t:
            pass

    def rstd_from(ssbank_i, scale):
        S.op("act", lambda e: e.activation(out=sd_t[:], in_=pbt[ssbank_i][:, :], func=AF.Sqrt,
                                           bias=epsc[:, 0:1], scale=scale), rd=[pbb[ssbank_i]] + CB, wr=[sd_b])
        rt, rb = rs_rot.next()
        S.op("dve", lambda e: e.reciprocal(out=rt[:], in_=sd_t[:]), rd=[sd_b], wr=[rb])
        return rt, rb

    def norm_x_to_h(l, s, tg, dst_fn, dst_bufs, ssbank_i):
        mb = B(f"mod{l}")
        tok = slice(tg * 512, (tg + 1) * 512)
        for dc in range(8):
            qt, qb = sq_rot.next()
            S.op("act", lambda e, dc=dc, qt=qt: e.activation(out=qt[:], in_=xT[:, dc, tok], func=AF.Square),
                 rd=[xb[dc][tg]], wr=[qb])
            S.op("pe", lambda e, dc=dc, qt=qt: e.matmul(pbt[ssbank_i][:, :], lhsT=ones_bf[:], rhs=qt[:],
                                                         start=(dc == 0), stop=(dc == 7)),
                 rd=[qb] + CB, wr=[pbb[ssbank_i]], inc=True)
        rt, rb = rstd_from(ssbank_i, 1.0 / 1024.0)
        for dc in range(8):
            tt, tb = t32_rot.next()
            acol = Acf[:, (l * 3 + s) * 8 + dc:(l * 3 + s) * 8 + dc + 1]
            shcol = modt[:, l * 72 + s * 24 + dc: l * 72 + s * 24 + dc + 1]
            S.op("dve", lambda e, dc=dc, tt=tt, acol=acol: e.scalar_tensor_tensor(
                out=tt[:], in0=xT[:, dc, tok], scalar=acol, in1=rt[:], op0=ALU.mult, op1=ALU.mult),
                rd=[xb[dc][tg], rb, mb], wr=[tb])
            S.op("act", lambda e, dc=dc, tt=tt, shcol=shcol: e.activation(
                out=dst_fn(dc), in_=tt[:], func=AF.Identity, bias=shcol, scale=1.0),
                rd=[tb, mb], wr=[dst_bufs[dc]])

    def post_residual(l, s, tg, ybf_fn, ybufs, ssbank_i):
        mb = B(f"mod{l}")
        tok = slice(tg * 512, (tg + 1) * 512)
        rt, rb = rstd_from(ssbank_i, 1.0 / 1024.0)
        for dc in range(8):
            tt, tb = t32_rot.next()
            bcol = Bcf[:, (l * 3 + s) * 8 + dc:(l * 3 + s) * 8 + dc + 1]
            S.op("dve", lambda e, dc=dc, tt=tt, bcol=bcol: e.scalar_tensor_tensor(
                out=tt[:], in0=ybf_fn(dc), scalar=bcol, in1=rt[:], op0=ALU.mult, op1=ALU.mult),
                rd=[ybufs[dc], rb, mb], wr=[tb])
            S.op("dve", lambda e, dc=dc, tt=tt: e.tensor_tensor(out=xT[:, dc, tok], in0=xT[:, dc, tok], in1=tt[:], op=ALU.add),
                 rd=[tb, xb[dc][tg]], wr=[xb[dc][tg]])

    def ffn(l, wi, s):
        with ExitStack() as fs:
            hT = sb("hT", [128, 8, 1024], BF16, fs)
            aT = sb("aT", [128, 22, 1024], BF16, fs)
            sg_rot = Rot([(sb(f"sg{i}", [128, 512], BF16, fs), B(f"sg{i}")) for i in range(2)])
            ysq_rot = Rot([(sb(f"ysq{i}", [128, 512], BF16, fs), B(f"ysq{i}")) for i in range(2)])
            hb = [[B(f"h{dc}_{t}") for t in range(2)] for dc in range(8)]
            ab = [[B(f"a{fc}_{t}") for t in range(2)] for fc in range(22)]
            scoped = [b for r in hb for b in r] + [b for r in ab for b in r] + [x[1] for x in sg_rot.items + ysq_rot.items]
            for ps in range(2):
                if DEBUG_CUT < 99 and ps == 1:
                    break
                wd_st.prefetch()
                for t in range(2):
                    tg = 2 * ps + t
                    norm_x_to_h(l, s, tg, lambda dc, t=t: hT[:, dc, t * 512:(t + 1) * 512], [hb[dc][t] for dc in range(8)], 6 + t)
                if DEBUG_CUT <= 1:
                    break
                for fc in range(22):
                    wt, wb = wgu_st.get()
                    for t in range(2):
                        for gu in range(2):
                            bk = 2 * gu + t
                            for dc in range(8):
                                S.op("pe", lambda e, dc=dc, gu=gu, bk=bk, t=t: e.matmul(
                                    pbt[bk][:, :], lhsT=wt[:, gu * 1024 + dc * 128: gu * 1024 + (dc + 1) * 128],
                                    rhs=hT[:, dc, t * 512:(t + 1) * 512], start=(dc == 0), stop=(dc == 7)),
                                    rd=[wb, hb[dc][t]], wr=[pbb[bk]], inc=(dc == 7))
                        st, sbf = sg_rot.next()
                        S.op("act", lambda e, st=st, t=t: e.activation(out=st[:], in_=pbt[t][:, :], func=AF.Silu),
                             rd=[pbb[t]], wr=[sbf])
                        S.op("dve", lambda e, st=st, t=t, fc=fc: e.tensor_tensor(
                            out=aT[:, fc, t * 512:(t + 1) * 512], in0=st[:], in1=pbt[2 + t][:, :], op=ALU.mult),
                            rd=[sbf, pbb[2 + t]], wr=[ab[fc][t]])
                wgu_st.prefetch()
                if DEBUG_CUT <= 2:
                    break
                for dc in range(8):
                    wt, wb = wd_st.get()
                    for t in range(2):
                        bk = 4 + t
                        for fc in range(22):
                            S.op("pe", lambda e, fc=fc, bk=bk, t=t: e.matmul(
                                pbt[bk][:, :], lhsT=wt[:, fc * 128:(fc + 1) * 128],
                                rhs=aT[:, fc, t * 512:(t + 1) * 512], start=(fc == 0), stop=(fc == 21)),
                                rd=[wb, ab[fc][t]], wr=[pbb[bk]], inc=(fc == 21))
                        yt, yb = ysq_rot.next()
                        S.op("act", lambda e, yt=yt, bk=bk: e.activation(out=yt[:], in_=pbt[bk][:, :], func=AF.Square),
                             rd=[pbb[bk]], wr=[yb])
                        S.op("act", lambda e, bk=bk, dc=dc, t=t: e.activation(
                            out=hT[:, dc, t * 512:(t + 1) * 512], in_=pbt[bk][:, :], func=AF.Copy),
                            rd=[pbb[bk]], wr=[hb[dc][t]])
                        S.op("pe", lambda e, yt=yt, t=t, dc=dc: e.matmul(
                            pbt[6 + t][:, :], lhsT=ones_bf[:], rhs=yt[:], start=(dc == 0), stop=(dc == 7)),
                            rd=[yb] + CB, wr=[pbb[6 + t]], inc=True)
                if DEBUG_CUT <= 3:
                    break
                for t in range(2):
                    post_residual(l, s, 2 * ps + t, lambda dc, t=t: hT[:, dc, t * 512:(t + 1) * 512],
                                  [hb[dc][t] for dc in range(8)], 6 + t)
            S.release(scoped)

    send1_b = [B(f"send1_{c}") for c in range(12)]
    recv1_b = [B(f"recv1_{c}") for c in range(12)]
    send2_b = [B(f"send2_{c}") for c in range(4)]
    recv2_b = [B(f"recv2_{c}") for c in range(4)]
    s1 = send1.ap()
    s1q = s1.rearrange("(g c k d) t -> g c k d t", g=4, c=3, k=4, d=64)
    s1v = s1.rearrange("(g c k d) (pl t m) -> g c k (d pl) t m", g=4, c=3, k=4, d=64, pl=2, t=16, m=64)
    r1g = recv1.ap().rearrange("(g c x) t -> g c x t", g=4, c=3)
    m1q = mine1.ap().rearrange("(c r k d) t -> c k d r t", c=3, r=4, k=4, d=64)
    m1v = mine1.ap().rearrange("(c r k d) (pl x) -> c k (d pl) r x", c=3, r=4, k=4, d=64, pl=2)
    s2 = send2.ap()
    m2 = mine2.ap().rearrange("(c r d) t -> c d r t", c=4, r=4, d=64)
    mine1_b = [B(f"mine1_{c}") for c in range(3)]
    mine2_b = [B(f"mine2_{c}") for c in range(4)]

    def ld_q(ct, k):
        return m1q[ct, k, :, :, :]

    def ld_v(ct, k):
        return m1v[ct, k, :, :, :]

    def stage_copy(ct):
        S.dma("sp", [(mine1.ap()[ct * 1024:(ct + 1) * 1024, :],
                      r1g[bass.ds(gidx, 1), ct, :, :].rearrange("o x t -> (o x) t"))],
              rd=[recv1_b[g_ * 3 + ct] for g_ in range(4)], wr=[mine1_b[ct]], owner=mine1_b[ct])

    def gather1():
        for ct in range(3):
            for g_ in range(4):
                c = g_ * 3 + ct
                S.allgather(s1[c * 256:(c + 1) * 256, :], recv1.ap()[c * 1024:(c + 1) * 1024, :], rd=[], wr=[send1_b[c], recv1_b[c]], name=f"g1_{c}")
        stage_copy(0)

    def gather2(c):
        S.allgather(s2[c * 64:(c + 1) * 64, :], recv2.ap()[c * 256:(c + 1) * 256, :], rd=[], wr=[send2_b[c], recv2_b[c]], name=f"g2_{c}")
        S.dma("sp", [(mine2.ap()[c * 256:(c + 1) * 256, :],
                      recv2.ap()[c * 256:(c + 1) * 256, :].rearrange("x (g t) -> x g t", g=4)[:, bass.ds(gidx, 1), :].rearrange("x o t -> x (o t)"))],
              rd=[recv2_b[c]], wr=[mine2_b[c]], owner=mine2_b[c])

    def normalize_store(obank_i, exps_col, slot, tgi, ost_rot, bcbank_i, rden_t, rden_b, of_t, of_b):
        ob = pbb[obank_i]
        if exps_col is not None:
            S.op("dve", lambda e: e.tensor_scalar(out=rden_t[64:65, :], in0=pbt[obank_i][64:65, :], scalar1=exps_col,
                                                  scalar2=None, op0=ALU.add), rd=[ob] + CB, wr=[rden_b])
        else:
            S.op("dve", lambda e: e.tensor_copy(out=rden_t[64:65, :], in_=pbt[obank_i][64:65, :]), rd=[ob], wr=[rden_b])
        S.op("dve", lambda e: e.reciprocal(out=rden_t[64:65, :], in_=rden_t[64:65, :]), rd=[rden_b], wr=[rden_b])
        S.op("pe", lambda e: e.matmul(pbt[bcbank_i][0:64, :], lhsT=onesf[64:65, 0:64], rhs=rden_t[64:65, :],
                                      start=True, stop=True), rd=[rden_b] + CB, wr=[pbb[bcbank_i]], inc=True)
        S.op("act", lambda e: e.activation(out=of_t[0:64, :], in_=pbt[obank_i][0:64, :], func=AF.Copy), rd=[ob], wr=[of_b])
        ot, obf = ost_rot.next()
        S.op("dve", lambda e: e.tensor_tensor(out=ot[0:64, :], in0=of_t[0:64, :], in1=pbt[bcbank_i][0:64, :], op=ALU.mult),
             rd=[of_b, pbb[bcbank_i]], wr=[obf])
        S.dma("sp", [(s2[slot * 64:(slot + 1) * 64, tgi * 512:(tgi + 1) * 512], ot[0:64, :])],
              rd=[obf, send2_b[slot]], wr=[], owner=obf)

    def attention(l, bg):
        s = 1
        mb = B(f"mod{l}")
        with ExitStack() as fs:
            hT4 = sb("hT4", [128, 8, 1024], BF16, fs)
            h4b = [[B(f"h4_{dc}_{t}") for t in range(2)] for dc in range(8)]
            wv = sb("wv", [128, 6144], BF16, fs)
            wvb = B("wv")
            bv = sb("bv", [128, 768], F32, fs)
            bvb = B("bv")
            wqk_slots = [(sb(f"wqk{i}", [128, 3584], BF16, fs), B(f"wqk{i}")) for i in range(2)]
            stq_rot = Rot([(sb(f"stq{i}", [64, 7, 512], BF16, fs), B(f"stq{i}")) for i in range(2)])
            stv_rot = Rot([(sb(f"stv{i}", [128, 12, 4, 64], BF16, fs), B(f"stv{i}")) for i in range(2)])
            scoped = [b for r in h4b for b in r] + [wvb, bvb] + [x[1] for x in wqk_slots + stq_rot.items + stv_rot.items]
            S.dma("pool", [(wv[:], wv_d[l * 128:(l + 1) * 128, :])], rd=[], wr=[wvb], owner=wvb)
            S.dma("sp", [(bv[:], bv_d[l * 128:(l + 1) * 128, :])], rd=[], wr=[bvb], owner=bvb)
            wqk_st = WStream(S, wqk_slots, [wqk_d[(l * 4 + gq) * 128:(l * 4 + gq + 1) * 128, :] for hf in range(2) for gq in range(4)])
            wqk_st.prefetch()
            for hf in range(2):
                for t in range(2):
                    tg = 2 * hf + t
                    norm_x_to_h(l, s, tg, lambda dc, t=t: hT4[:, dc, t * 512:(t + 1) * 512],
                                [h4b[dc][t] for dc in range(8)], 6 + t)
                for t in range(2):
                    tg = 2 * hf + t
                    vt, vb_ = stv_rot.next()
                    for tl in range(4):
                        c0 = t * 512 + tl * 128
                        for (bk, lo, hi) in ((0 + 2 * (tl % 2), 0, 512), (1 + 2 * (tl % 2), 512, 768)):
                            for dc in range(8):
                                S.op("pe", lambda e, dc=dc, bk=bk, lo=lo, hi=hi, c0=c0: e.matmul(
                                    pbt[bk][:, 0:hi - lo], lhsT=hT4[:, dc, c0:c0 + 128], rhs=wv[:, dc * 768 + lo: dc * 768 + hi],
                                    start=(dc == 0), stop=(dc == 7)), rd=[h4b[dc][t], wvb], wr=[pbb[bk]], inc=(dc == 7))
                            nvs = (hi - lo) // 64
                            S.op("dve", lambda e, bk=bk, lo=lo, hi=hi, nvs=nvs, tl=tl, vt=vt: e.tensor_tensor(
                                out=vt[:, lo // 64: lo // 64 + nvs, tl, :],
                                in0=pbt[bk][:, 0:hi - lo].rearrange("p (v m) -> p v m", m=64),
                                in1=bv[:, lo:hi].rearrange("p (v m) -> p v m", m=64), op=ALU.add),
                                rd=[pbb[bk], bvb], wr=[vb_])
                    vt5 = vt[:, :, :, :].rearrange("p (g j) t m -> p g j t m", j=3)
                    S.dma("sp", [(s1v[:, cj, kj, :, tg * 4:(tg + 1) * 4, :].rearrange("g p t m -> p g t m"), vt5[:, :, j, :, :])
                                 for (j, cj, kj) in ((0, 0, 3), (1, 1, 2), (2, 2, 2))],
                          rd=[vb_] + send1_b, wr=[], owner=vb_)
                for gq in range(4):
                    wt, wb = wqk_st.get()
                    for t in range(2):
                        tg = 2 * hf + t
                        qt, qb = stq_rot.next()
                        for sl in range(7):
                            bk = 4 + (sl % 2)
                            for dc in range(8):
                                S.op("pe", lambda e, dc=dc, sl=sl, bk=bk, t=t: e.matmul(
                                    pbt[bk][0:64, :], lhsT=wt[:, (sl * 8 + dc) * 64:(sl * 8 + dc + 1) * 64],
                                    rhs=hT4[:, dc, t * 512:(t + 1) * 512], start=(dc == 0), stop=(dc == 7)),
                                    rd=[wb, h4b[dc][t]], wr=[pbb[bk]], inc=(dc == 7))
                            bcol = bqk[:, l * 28 + gq * 7 + sl: l * 28 + gq * 7 + sl + 1]
                            S.op("act", lambda e, sl=sl, bk=bk, qt=qt, bcol=bcol: e.activation(
                                out=qt[:, sl, :], in_=pbt[bk][0:64, :], func=AF.Identity, bias=bcol, scale=1.0),
                                rd=[pbb[bk]] + CB, wr=[qb])
                        cs = slice(tg * 512, (tg + 1) * 512)
                        S.dma("sp", [(s1q[gq, 0, 0:3, :, cs].rearrange("k d t -> d k t"), qt[:, 0:3, :]),
                                     (s1q[gq, 1, 0:2, :, cs].rearrange("k d t -> d k t"), qt[:, 3:5, :]),
                                     (s1q[gq, 2, 0:2, :, cs].rearrange("k d t -> d k t"), qt[:, 5:7, :])],
                              rd=[qb] + send1_b[gq * 3:gq * 3 + 3], wr=[], owner=qb)
            S.release(scoped)
        if DEBUG_CUT == 11:
            return
        gather1()
        if DEBUG_CUT == 12:
            stage_copy(1)
            stage_copy(2)
            S.wait_all("sp", mine1_b)
            return

        with ExitStack() as fs:
            QTs = [sb(f"QTs{i}", [64, 8192], BF16, fs) for i in range(2)]
            KTs = sb("KTs", [64, 128 + 8192], BF16, fs)
            Vs = sb("Vs", [128, 65, 65], BF16, fs)
            vst = sb("vst", [128, 4, 1024], BF16, fs)
            qsb, ksb, vsb, vstb = B("QTs"), B("KTs"), B("Vs"), B("vst")
            P_rot = Rot([(sb(f"Ps{i}", [128, 256], BF16, fs), B(f"Ps{i}")) for i in range(3)])
            sb_rot = Rot([(sb(f"sbs{i}", [128, 256], F32, fs), B(f"sbs{i}")) for i in range(2)])
            ost_rot = Rot([(sb(f"ost{i}", [64, 512], BF16, fs), B(f"ost{i}")) for i in range(2)])
            rden_t, rden_b = sb("rden", [65, 512], F32, fs), B("rden")
            of_t, of_b = sb("of", [64, 512], F32, fs), B("of")
            scoped = [qsb, ksb, vsb, vstb, rden_b, of_b] + [x[1] for x in P_rot.items + sb_rot.items + ost_rot.items]
            S.dma("sp", [(QTs[i][:, :].rearrange("d (r t) -> d r t", r=4), ld_q(0, i)) for i in range(2)],
                  rd=[mine1_b[0]], wr=[qsb], owner=qsb)
            S.op("dve", lambda e: e.memset(KTs[:, 0:128], 0.0), wr=[ksb])
            S.dma("sp", [(KTs[:, 128:].rearrange("d (r t) -> d r t", r=4), ld_q(0, 2))],
                  rd=[mine1_b[0]], wr=[ksb], owner=ksb)
            S.dma("sp", [(vst[:, :, :], ld_v(0, 3))], rd=[mine1_b[0]], wr=[vstb], owner=vstb)
            S.op("dve", lambda e: e.memset(Vs[:, :, 64:65], 1.0), wr=[vsb])
            S.op("dve", lambda e: e.memset(Vs[:, 0, 0:64], 0.0), wr=[vsb])
            S.op("dve", lambda e: e.tensor_copy(out=Vs[:, 1:65, 0:64], in_=vst[:, :, :].rearrange("p r (t m) -> p (r t) m", m=64)),
                 rd=[vstb], wr=[vsb])
            for i in range(2):
                for tgi in range(16):
                    obk = 4 + (tgi % 2)
                    for qq in range(4):
                        T = tgi * 4 + qq
                        sbk = T % 3
                        for half in range(2):
                            S.op("pe", lambda e, T=T, half=half, sbk=sbk: e.matmul(
                                pbt[sbk][:, half * 128:(half + 1) * 128], lhsT=KTs[:, (T + half) * 128:(T + half + 1) * 128],
                                rhs=QTs[i][:, T * 128:(T + 1) * 128], start=True, stop=True),
                                rd=[ksb, qsb], wr=[pbb[sbk]], inc=(half == 1))
                        st, stb = sb_rot.next()
                        var = i * 2 + (1 if T == 0 else 0)
                        S.op("dve", lambda e, st=st, sbk=sbk, var=var: e.scalar_tensor_tensor(
                            out=st[:], in0=pbt[sbk][:, 0:256], scalar=0.125, in1=gswa[:, var * 256:(var + 1) * 256],
                            op0=ALU.mult, op1=ALU.add), rd=[pbb[sbk]] + CB, wr=[stb])
                        pt, ptb = P_rot.next()
                        S.op("act", lambda e, st=st, pt=pt: e.activation(out=pt[:], in_=st[:], func=AF.Exp), rd=[stb], wr=[ptb])
                        for half in range(2):
                            S.op("pe", lambda e, T=T, half=half, obk=obk, pt=pt, qq=qq: e.matmul(
                                pbt[obk][0:65, qq * 128:(qq + 1) * 128], lhsT=Vs[:, T + half, 0:65],
                                rhs=pt[:, half * 128:(half + 1) * 128], start=(half == 0), stop=(half == 1)),
                                rd=[vsb, ptb], wr=[pbb[obk]], inc=(half == 1))
                    normalize_store(obk, esnk[64:65, l * 2 + i: l * 2 + i + 1], i, tgi, ost_rot, 6 + (tgi % 2),
                                    rden_t, rden_b, of_t, of_b)
                gather2(i)
            S.release(scoped)

        if DEBUG_CUT == 13:
            return
        for i in range(2):
            with ExitStack() as fs:
                QTa = sb("QTa", [96, 8192], BF16, fs)
                KTa = sb("KTa", [96, 8192], BF16, fs)
                Va = sb("Va", [128, 64, 65], BF16, fs)
                vst = sb("vstm", [128, 4, 1024], BF16, fs)
                qab, kab, kib, vab, vstb = B("QTa"), B("KTa"), B("KTi"), B("Va"), B("vstm")
                qmb = [B(f"qm{t}") for t in range(16)]
                km32 = sb("km32", [64, 32], F32, fs)
                kmd = sb("kmd", [64, 32], F32, fs)
                kmh = sb("kmh", [64, 32], BF16, fs)
                kml = sb("kml", [64, 32], BF16, fs)
                kmb = B("km")
                gb_rot = Rot([(sb(f"gb{k}", [128, 32], F32, fs), B(f"gb{k}")) for k in range(2)])
                t8_rot = Rot([(sb(f"t8{k}", [128, 8], F32, fs), B(f"t8{k}")) for k in range(2)])
                mbt_rot = Rot([(sb(f"mbt{k}", [128, 4, 96], F32, fs), B(f"mbt{k}")) for k in range(2)])
                P_rot = Rot([(sb(f"Pm{k}", [128, 512], BF16, fs), B(f"Pm{k}")) for k in range(3)])
                sb_rot = Rot([(sb(f"sbm{k}", [128, 512], F32, fs), B(f"sbm{k}")) for k in range(2)])
                ost_rot = Rot([(sb(f"ostm{k}", [64, 512], BF16, fs), B(f"ostm{k}")) for k in range(2)])
                rden_t, rden_b = sb("rdenm", [65, 512], F32, fs), B("rdenm")
                of_t, of_b = sb("ofm", [64, 512], F32, fs), B("ofm")
                scoped = ([qab, kab, kib, vab, vstb, kmb, rden_b, of_b] + qmb +
                          [x[1] for x in gb_rot.items + t8_rot.items + mbt_rot.items + P_rot.items + sb_rot.items + ost_rot.items])
                stage_copy(1 + i)
                S.dma("sp", [(QTa[0:64, :].rearrange("d (r t) -> d r t", r=4), ld_q(1 + i, 0))],
                      rd=[mine1_b[1 + i]], wr=[qab], owner=qab)
                S.dma("sp", [(KTa[0:64, :].rearrange("d (r t) -> d r t", r=4), ld_q(1 + i, 1))],
                      rd=[mine1_b[1 + i]], wr=[kab], owner=kab)
                S.dma("pool", [(KTa[64:96, :], blkind_d[:, :])], rd=[], wr=[kib], owner=kib)
                S.dma("sp", [(vst[:, :, :], ld_v(1 + i, 2))], rd=[mine1_b[1 + i]], wr=[vstb], owner=vstb)
                S.op("dve", lambda e: e.memset(Va[:, :, 64:65], 1.0), wr=[vab])
                S.op("dve", lambda e: e.tensor_copy(out=Va[:, :, 0:64], in_=vst[:, :, :].rearrange("p r (t m) -> p (r t) m", m=64)),
                     rd=[vstb], wr=[vab])
                S.op("dve", lambda e: e.tensor_reduce(out=km32[:, :], in_=KTa[0:64, :].rearrange("d (b k) -> d b k", k=256),
                                                      axis=AX.X, op=ALU.add), rd=[kab], wr=[kmb])
                S.op("dve", lambda e: e.tensor_copy(out=kmh[:, :], in_=km32[:, :]), rd=[kmb], wr=[kmb])
                S.op("dve", lambda e: e.tensor_tensor(out=kmd[:, :], in0=km32[:, :], in1=kmh[:, :], op=ALU.subtract), rd=[kmb], wr=[kmb])
                S.op("dve", lambda e: e.tensor_copy(out=kml[:, :], in_=kmd[:, :]), rd=[kmb], wr=[kmb])
                for tgi in range(16):
                    mt, mtb = mbt_rot.next()
                    for qq in range(4):
                        T = tgi * 4 + qq
                        cur = T // 2
                        if cur >= 3:
                            S.op("pe", lambda e, T=T, qq=qq: e.matmul(pbt[6][:, qq * 32:(qq + 1) * 32], lhsT=QTa[0:64, T * 128:(T + 1) * 128],
                                                                      rhs=kmh[:, :], start=True, stop=False), rd=[qab, kmb], wr=[pbb[6]], inc=False)
                            S.op("pe", lambda e, T=T, qq=qq: e.matmul(pbt[6][:, qq * 32:(qq + 1) * 32], lhsT=QTa[0:64, T * 128:(T + 1) * 128],
                                                                      rhs=kml[:, :], start=False, stop=True), rd=[qab, kmb], wr=[pbb[6]], inc=True)
                            gt, gtb = gb_rot.next()
                            S.op("dve", lambda e, gt=gt, qq=qq, cur=cur: e.tensor_tensor(
                                out=gt[:], in0=pbt[6][:, qq * 32:(qq + 1) * 32], in1=stair[:, 32 - cur:64 - cur], op=ALU.add),
                                rd=[pbb[6]] + CB, wr=[gtb])
                            t8, t8b = t8_rot.next()
                            S.op("dve", lambda e, gt=gt, t8=t8: e.max(out=t8[:], in_=gt[:]), rd=[gtb], wr=[t8b])
                            S.op("dve", lambda e, gt=gt, t8=t8, mt=mt, qq=qq: e.tensor_scalar(
                                out=mt[:, qq, 64:96], in0=gt[:], scalar1=t8[:, 2:3], scalar2=1.0, op0=ALU.is_ge, op1=ALU.subtract),
                                rd=[gtb, t8b], wr=[mtb])
                            S.op("dve", lambda e, mt=mt, qq=qq, cur=cur: e.memset(mt[:, qq, 64 + cur:65 + cur], 0.0), wr=[mtb])
                        else:
                            S.op("dve", lambda e, mt=mt, qq=qq, cur=cur: e.tensor_copy(
                                out=mt[:, qq, 64:96], in_=stair[:, 64 + 31 - cur:64 + 63 - cur]), rd=CB, wr=[mtb])
                        S.op("pe", lambda e, mt=mt, qq=qq: e.transpose(out=pbt[7][0:96, qq * 128:(qq + 1) * 128], in_=mt[:, qq, :],
                                                                       identity=ident[:, :]), rd=[mtb] + CB, wr=[pbb[7]], inc=True)
                    S.op("act", lambda e, tgi=tgi: e.activation(out=QTa[64:96, tgi * 512:(tgi + 1) * 512], in_=pbt[7][64:96, :], func=AF.Copy),
                         rd=[pbb[7]], wr=[qmb[tgi]])
                for tgi in range(16):
                    obk = 4 + (tgi % 2)
                    nkt = 4 * tgi + 4
                    for kt in range(nkt):
                        sbk = kt % 3
                        S.op("pe", lambda e, kt=kt, sbk=sbk, tgi=tgi: e.matmul(
                            pbt[sbk][:, :], lhsT=KTa[0:96, kt * 128:(kt + 1) * 128], rhs=QTa[0:96, tgi * 512:(tgi + 1) * 512],
                            start=True, stop=True), rd=[kab, kib, qab, qmb[tgi]], wr=[pbb[sbk]], inc=True)
                        pt, ptb = P_rot.next()
                        rel = kt - 4 * tgi
                        if rel >= -2:
                            j0 = i * 1152 + 384 - 128 * rel
                            st, stb = sb_rot.next()
                            S.op("dve", lambda e, st=st, sbk=sbk, j0=j0: e.scalar_tensor_tensor(
                                out=st[:], in0=pbt[sbk][:, :], scalar=0.125, in1=gmoba[:, j0:j0 + 512], op0=ALU.mult, op1=ALU.add),
                                rd=[pbb[sbk]] + CB, wr=[stb])
                            S.op("act", lambda e, st=st, pt=pt: e.activation(out=pt[:], in_=st[:], func=AF.Exp), rd=[stb], wr=[ptb])
                        else:
                            S.op("act", lambda e, pt=pt, sbk=sbk: e.activation(out=pt[:], in_=pbt[sbk][:, :], func=AF.Exp,
                                                                              bias=cfar[:, i:i + 1], scale=0.125), rd=[pbb[sbk]] + CB, wr=[ptb])
                        S.op("pe", lambda e, kt=kt, obk=obk, pt=pt, nkt=nkt: e.matmul(
                            pbt[obk][0:65, :], lhsT=Va[:, kt, 0:65], rhs=pt[:], start=(kt == 0), stop=(kt == nkt - 1)),
                            rd=[vab, ptb], wr=[pbb[obk]], inc=(kt == nkt - 1))
                    normalize_store(obk, None, 2 + i, tgi, ost_rot, 3, rden_t, rden_b, of_t, of_b)
                    if bg is not None:
                        next(bg, None)
                        next(bg, None)
                gather2(2 + i)
                S.release(scoped)
        if bg is not None:
            run_task(bg)
        if DEBUG_CUT == 14:
            return

        with ExitStack() as fs:
            wo = sb("wo", [128, 8192], BF16, fs)
            wob = B("wo")
            OT_rot = Rot([(sb(f"OT{k}", [128, 8, 512], BF16, fs), B(f"OT{k}")) for k in range(2)])
            osq = sb("osq", [128, 8, 512], BF16, fs)
            osqb = B("osq")
            OnT = sb("OnT", [128, 8, 512], BF16, fs)
            onb = [B(f"on{k}") for k in range(8)]
            ybf = sb("ybf", [128, 8, 512], BF16, fs)
            ybb = [B(f"yb{k}") for k in range(8)]
            ysq_rot = Rot([(sb(f"ysqo{k}", [128, 512], BF16, fs), B(f"ysqo{k}")) for k in range(2)])
            rsa_t, rsa_b = sb("rsa", [128, 512], F32, fs), B("rsa")
            scoped = [wob, osqb, rsa_b] + onb + ybb + [x[1] for x in OT_rot.items + ysq_rot.items]
            S.dma("pool", [(wo[:], wout_d[l * 128:(l + 1) * 128, :])], rd=[], wr=[wob], owner=wob)
            for tg in range(4):
                ot, otb = OT_rot.next()
                S.dma("sp", [(ot[:, :, :].rearrange("p (r par) t -> p r par t", par=2)[(sl % 2) * 64:(sl % 2) * 64 + 64, :, sl // 2, :], m2[sl, :, :, tg * 512:(tg + 1) * 512])
                             for sl in range(4)], rd=mine2_b, wr=[otb], owner=otb)
                S.op("dve", lambda e, ot=ot: e.tensor_tensor(out=osq[:, :, :], in0=ot[:, :, :], in1=ot[:, :, :], op=ALU.mult),
                     rd=[otb], wr=[osqb])
                for par in range(2):
                    for k in range(4):
                        kc = 2 * k + par
                        S.op("pe", lambda e, kc=kc, k=k, par=par: e.matmul(pbt[6 + par][:, :], lhsT=ones_bf[:], rhs=osq[:, kc, :],
                                                                         start=(k == 0), stop=(k == 3)),
                             rd=[osqb] + CB, wr=[pbb[6 + par]], inc=(k == 3))
                S.op("act", lambda e: e.activation(out=sd_t[:], in_=pbt[6][:, :], func=AF.Sqrt, bias=epsc[:, 0:1], scale=1.0 / 512.0),
                     rd=[pbb[6]] + CB, wr=[sd_b])
                S.op("dve", lambda e: e.reciprocal(out=rsa_t[:], in_=sd_t[:]), rd=[sd_b], wr=[rsa_b])
                rbt, rbb = rstd_from(7, 1.0 / 512.0)
                for kc in range(8):
                    rr, rrb = (rsa_t, rsa_b) if kc % 2 == 0 else (rbt, rbb)
                    gcol = vecs[:, l * 128 + 120 + kc: l * 128 + 121 + kc]
                    S.op("dve", lambda e, kc=kc, rr=rr, gcol=gcol, ot=ot: e.scalar_tensor_tensor(
                        out=OnT[:, kc, :], in0=ot[:, kc, :], scalar=gcol, in1=rr[:], op0=ALU.mult, op1=ALU.mult),
                        rd=[otb, rrb] + CB, wr=[onb[kc]])
                for dm in range(8):
                    bk = 4 + (dm % 2)
                    for kc in range(8):
                        S.op("pe", lambda e, kc=kc, dm=dm, bk=bk: e.matmul(
                            pbt[bk][:, :], lhsT=wo[:, kc * 1024 + dm * 128: kc * 1024 + (dm + 1) * 128], rhs=OnT[:, kc, :],
                            start=(kc == 0), stop=(kc == 7)), rd=[wob, onb[kc]], wr=[pbb[bk]], inc=(kc == 7))
                    yt, yb = ysq_rot.next()
                    S.op("act", lambda e, yt=yt, bk=bk: e.activation(out=yt[:], in_=pbt[bk][:, :], func=AF.Square), rd=[pbb[bk]], wr=[yb])
                    S.op("act", lambda e, dm=dm, bk=bk: e.activation(out=ybf[:, dm, :], in_=pbt[bk][:, :], func=AF.Copy),
                         rd=[pbb[bk]], wr=[ybb[dm]])
                    S.op("pe", lambda e, yt=yt, dm=dm: e.matmul(pbt[3][:, :], lhsT=ones_bf[:], rhs=yt[:], start=(dm == 0), stop=(dm == 7)),
                         rd=[yb] + CB, wr=[pbb[3]], inc=True)
                post_residual(l, s, tg, lambda dc: ybf[:, dc, :], ybb, 3)
            S.release(scoped)

    with ExitStack() as zs:
        zt = sb("zpad", [64, 2048], BF16, zs)
        zb = B("zpad")
        S.op("dve", lambda e: e.memset(zt[:], 0.0), wr=[zb])
        S.dma("sp", [(s1q[g_, ct, 3, :, :], zt[:]) for g_ in range(4) for ct in (1, 2)],
              rd=[zb] + send1_b, wr=[], owner=zb)
        S.release([zb])
    run_task(mod_task(0))
    sub = 0
    for l in range(L):
        if sub < nsub:
            ffn(l, 0, 0)
            sub += 1
        if sub < nsub:
            bg = mod_task(l + 1) if (l + 1 < L and 3 * (l + 1) < nsub) else None
            attention(l, bg)
            sub += 1
        elif l + 1 < L:
            pass
        if sub < nsub:
            ffn(l, 1, 2)
            sub += 1
    allx = [b for row in xb for b in row]
    outb = B("outb")
    S.dma("sp", [(out_d[dc * 128:(dc + 1) * 128, :], xT[:, dc, :]) for dc in range(8)], rd=allx, wr=[outb], owner=outb)
    S.wait_all("sp", [outb])
    es.close()
    return nc


def _t5_bucket_np(dist):
    n = np.maximum(dist, 0)
    nf = np.maximum(n, 1).astype(np.float32)
    large = 16 + (np.log(nf / np.float32(16)) / np.float32(math.log(128 / 16)) * np.float32(16)).astype(np.int32)
    large = np.minimum(large, 31)
    return np.where(n < 16, n, large)


def _slot_cols():
    qk, v = [], []
    for g in range(4):
        qk += [64 * (2 * g), 64 * (2 * g + 1), 512 + 64 * (g // 2), 768 + 64 * (2 * g), 1280 + 64 * (2 * g),
               768 + 64 * (2 * g + 1), 1280 + 64 * (2 * g + 1)]
        v += [640 + 64 * (g // 2), 1792 + 64 * (2 * g), 1792 + 64 * (2 * g + 1)]
    return qk, v


def prep_inputs(inp, L=DEPTH, l0=0, x=None):
    f = np.float32
    x = np.asarray(inp["x"], f) if x is None else x
    c = np.asarray(inp["c"], f)
    rel = np.asarray(inp["rel_bias"], f)
    ada_w = np.asarray(inp["ada_w"][l0:l0 + L], f)
    ada_b = np.asarray(inp["ada_b"][l0:l0 + L], f)
    npre = np.asarray(inp["norm_pre"][l0:l0 + L], f)
    npost = np.asarray(inp["norm_post"][l0:l0 + L], f)
    wg = np.asarray(inp["ffn_w_gate"][l0:l0 + L], f)
    wu = np.asarray(inp["ffn_w_up"][l0:l0 + L], f)
    wdn = np.asarray(inp["ffn_w_down"][l0:l0 + L], f)
    w_in = np.asarray(inp["mix_w_in"][l0:l0 + L], f)
    b_in = np.asarray(inp["mix_b_in"][l0:l0 + L], f)
    w_out = np.asarray(inp["mix_w_out"][l0:l0 + L], f)
    sinks = np.asarray(inp["attn_sinks"][l0:l0 + L], f)
    gain = np.asarray(inp["group_gain"][l0:l0 + L], f)

    g6 = wg.reshape(L, 2, 8, 128, 22, 128)
    u6 = wu.reshape(L, 2, 8, 128, 22, 128)
    gu = np.stack([g6, u6], axis=2)
    wgu = np.ascontiguousarray(gu.transpose(0, 1, 5, 4, 2, 3, 6)).reshape(L * 2 * 22 * 128, 2048)
    d6 = wdn.reshape(L, 2, 22, 128, 8, 128)
    wd = np.ascontiguousarray(d6.transpose(0, 1, 4, 3, 2, 5)).reshape(L * 2 * 8 * 128, 2816)
    qk_cols, v_cols = _slot_cols()
    qk_idx = np.concatenate([np.arange(cb, cb + 64) for cb in qk_cols])
    v_idx = np.concatenate([np.arange(cb, cb + 64) for cb in v_cols])
    wq = w_in[:, :, qk_idx].reshape(L, 8, 128, 4, 7, 64)
    wqk = np.ascontiguousarray(wq.transpose(0, 3, 2, 4, 1, 5)).reshape(L * 4 * 128, 3584)
    wvv = w_in[:, :, v_idx].reshape(L, 8, 128, 768)
    wv = np.ascontiguousarray(wvv.transpose(0, 2, 1, 3)).reshape(L * 128, 6144)
    bqk = np.ascontiguousarray(b_in[:, qk_idx].reshape(L, 28, 64).transpose(2, 0, 1)).reshape(64, L * 28)
    bv = np.ascontiguousarray(np.broadcast_to(b_in[:, None, v_idx], (L, 128, 768))).reshape(L * 128, 768)
    rowperm = np.concatenate([np.concatenate([np.arange(128 * g, 128 * g + 128), np.arange(512 + 128 * g, 512 + 128 * g + 128)])
                              for g in range(4)])
    wo = w_out[:, rowperm, :].reshape(L, 8, 128, 1024)
    wout = np.ascontiguousarray(wo.transpose(0, 2, 1, 3)).reshape(L * 128, 8192)
    aw = ada_w.reshape(L, 8, 128, 36, 256)
    adaw = np.ascontiguousarray(aw.transpose(0, 3, 2, 1, 4)).reshape(L * 36 * 128, 2048)
    vecs = np.zeros((128, L * 128), f)
    for l in range(L):
        vecs[:, l * 128: l * 128 + 72] = ada_b[l].reshape(72, 128).T
        vecs[:, l * 128 + 72: l * 128 + 96] = npre[l].reshape(24, 128).T
        vecs[:, l * 128 + 96: l * 128 + 120] = npost[l].reshape(24, 128).T
        vecs[:, l * 128 + 120: l * 128 + 128] = gain[l][rowperm].reshape(8, 128).T
    ident = np.eye(128, dtype=f)
    blkind = np.zeros((32, 8192), f)
    for b in range(32):
        blkind[b, b * 256:(b + 1) * 256] = MASKV
    stair = np.zeros((128, 128), f)
    stair[:, 32:64] = -1e30
    stair[:, 96:128] = -1.0

    kk = np.arange(128)[:, None]
    shared = dict(wgu=wgu, wd=wd, wqk=wqk, wv=wv, bqk=bqk, bv=bv, wout=wout, adaw=adaw, vecs=vecs,
                  ident=ident, blkind=blkind, stair=stair)
    in_maps = []
    for core in range(8):
        bt, g = core // 4, core % 4
        m = dict(shared)
        m["xT"] = np.ascontiguousarray(x[bt, g * 2048:(g + 1) * 2048, :].T)
        m["cT"] = np.ascontiguousarray(c[bt].reshape(8, 128).T)
        gm = np.zeros((128, 2 * 1152), f)
        cf = np.zeros((128, 2), f)
        for i in range(2):
            h = 8 + 2 * g + i
            d = np.arange(1152)[None, :] - 384 - kk
            val = rel[_t5_bucket_np(d), h]
            gm[:, i * 1152:(i + 1) * 1152] = np.where(d >= 0, val, f(-30000.0))
            cf[:, i] = rel[31, h]
        m["gmoba"] = gm
        m["cfar"] = cf
        gs = np.zeros((128, 4 * 256), f)
        qq = np.arange(128)[None, :]
        for i in range(2):
            h = 2 * g + i
            d0 = qq + 128 - kk
            d1 = qq - kk
            a0 = np.where((d0 >= 0) & (d0 < 128), rel[_t5_bucket_np(d0), h], f(-30000.0))
            a1 = np.where((d1 >= 0) & (d1 < 128), rel[_t5_bucket_np(d1), h], f(-30000.0))
            gs[:, (i * 2) * 256:(i * 2) * 256 + 128] = a0
            gs[:, (i * 2) * 256 + 128:(i * 2 + 1) * 256] = a1
            gs[:, (i * 2 + 1) * 256:(i * 2 + 1) * 256 + 128] = f(-30000.0)
            gs[:, (i * 2 + 1) * 256 + 128:(i * 2 + 2) * 256] = a1
        m["gswa"] = gs
        m["snk"] = np.ascontiguousarray(sinks[:, 2 * g:2 * g + 2].reshape(1, L * 2))
        in_maps.append(m)
    return in_maps


_NC_CACHE = {}


def run(inputs, L=DEPTH, nsub=None, l0=0, x=None):
    key = (L, nsub)
    if key not in _NC_CACHE:
        _NC_CACHE[key] = build(L, nsub)
    nc = _NC_CACHE[key]
    in_maps = prep_inputs(inputs, L, l0, x)
    res = run_bass_kernel_spmd(nc, in_maps, core_ids=list(range(8)))
    out = np.zeros((2, 8192, 1024), np.float32)
    for core in range(8):
        bt, g = core // 4, core % 4
        out[bt, g * 2048:(g + 1) * 2048, :] = np.asarray(res.results[core]["outT"], np.float32).T
    return out


LAYERS_PER_LAUNCH = 4


def kernel(**inputs):
    x = None
    for l0 in range(0, DEPTH, LAYERS_PER_LAUNCH):
        x = run(inputs, LAYERS_PER_LAUNCH, None, l0, x)
    return x
```

```python
import math
import numpy as np
from contextlib import ExitStack
import concourse.bass as bass
import concourse.mybir as mybir
from concourse.bass_utils import run_bass_kernel_spmd

F32 = mybir.dt.float32
BF16 = mybir.dt.bfloat16
ALU = mybir.AluOpType
AF = mybir.ActivationFunctionType
AX = mybir.AxisListType

DEPTH = 4
DEBUG_CUT = 99
EPS = 1e-6
MASKV = 240000.0


class Tok:
    __slots__ = ("sem", "key", "val", "eng")

    def __init__(self, sem, key, val, eng):
        self.sem, self.key, self.val, self.eng = sem, key, val, eng


class Buf:
    def __init__(self, name):
        self.name = name
        self.w = None
        self.r = {}
        self.dsem = None
        self.dkey = None
        self.dcnt = 0


class Sched:
    def __init__(self, nc, es):
        self.nc, self.es = nc, es
        self.E = {"pe": nc.tensor, "act": nc.scalar, "dve": nc.vector, "pool": nc.gpsimd, "sp": nc.sync}
        self.sem, self.key, self.cnt = {}, {}, {}
        self.known = {e: {} for e in self.E}
        self.nsem = 0
        self.bufs = {}
        for e in self.E:
            self.new_epoch(e)

    def newsem(self, name):
        self.nsem += 1
        nm = f"{name}_{self.nsem}"
        return self.es.enter_context(self.nc.semaphore(nm)), nm

    def new_epoch(self, e):
        self.sem[e], self.key[e] = self.newsem("e" + e)
        self.cnt[e] = 0

    def B(self, name):
        b = self.bufs.get(name)
        if b is None:
            b = self.bufs[name] = Buf(name)
        return b

    def _wait(self, e, toks, skip_pe=False, defer=False):
        need = {}
        for t in toks:
            if t is None:
                continue
            if skip_pe and t.eng == "pe":
                continue
            cur = need.get(t.key)
            if cur is None or cur.val < t.val:
                need[t.key] = t
        kn = self.known[e]
        todo = [t for k, t in need.items() if kn.get(k, 0) < t.val]
        last = None
        if defer and todo:
            last = todo.pop()
            kn[last.key] = last.val
        for t in todo:
            self.E[e].wait_ge(t.sem, t.val)
            kn[t.key] = t.val
        return last

    @staticmethod
    def _deps(rd, wr):
        toks = []
        for b in rd:
            toks.append(b.w)
        for b in wr:
            toks.append(b.w)
            toks.extend(b.r.values())
        return toks

    @staticmethod
    def _record(tok, rd, wr):
        for b in rd:
            c = b.r.get(tok.key)
            if c is None or c.val < tok.val:
                b.r[tok.key] = tok
        for b in wr:
            b.w = tok
            b.r = {}

    def op(self, e, fn, rd=(), wr=(), inc=True):
        last = self._wait(e, self._deps(rd, wr), skip_pe=(e == "pe"), defer=True)
        ins = fn(self.E[e])
        if last is not None:
            ins._wait_ge(last.sem, last.val)
        if inc:
            ins.then_inc(self.sem[e], 1)
            self.cnt[e] += 1
            tok = Tok(self.sem[e], self.key[e], self.cnt[e], e)
        else:
            tok = Tok(self.sem[e], self.key[e], self.cnt[e] + 1, e)
        self._record(tok, rd, wr)
        return tok

    def dma(self, q, pairs, rd, wr, owner):
        if owner.dsem is None:
            owner.dsem, owner.dkey = self.newsem("d")
        last = self._wait(q, self._deps(rd, wr), defer=True)
        for (o, i) in pairs:
            ins = self.E[q].dma_start(out=o, in_=i)
            if last is not None:
                ins._wait_ge(last.sem, last.val)
                last = None
            ins.then_inc(owner.dsem, 16)
            owner.dcnt += 16
        tok = Tok(owner.dsem, owner.dkey, owner.dcnt, "dma")
        self._record(tok, rd, wr)
        return tok

    def allgather(self, src_ap, dst_ap, rd, wr, name):
        if not hasattr(self, "ccs"):
            self.ccs = {}
        if name not in self.ccs:
            self.ccs[name] = list(self.newsem("cc")) + [0]
        ent = self.ccs[name]
        ent[2] += 1
        sem, key = ent[0], ent[1]
        self._wait("pool", self._deps(rd, wr))
        self.nc.gpsimd.collective_compute(
            "AllGather", ALU.bypass, replica_groups=[[0, 1, 2, 3], [4, 5, 6, 7]],
            ins=[src_ap.opt()], outs=[dst_ap.opt()],
        ).then_inc(sem)
        tok = Tok(sem, key, ent[2], "cc")
        self._record(tok, rd, wr)
        return tok

    def release(self, bufs):
        toks = []
        for b in bufs:
            toks.append(b.w)
            toks.extend(b.r.values())
        for e in self.E:
            self._wait(e, toks)

    def wait_all(self, e, bufs):
        toks = []
        for b in bufs:
            toks.append(b.w)
            toks.extend(b.r.values())
        self._wait(e, toks)


class Rot:
    def __init__(self, items):
        self.items, self.i = items, 0

    def next(self):
        it = self.items[self.i % len(self.items)]
        self.i += 1
        return it


class WStream:
    def __init__(self, S, slots, srcs):
        self.S, self.slots, self.srcs = S, slots, srcs
        self.issued = 0
        self.consumed = 0

    def prefetch(self, ahead=None):
        n = len(self.slots) if ahead is None else ahead
        while self.issued < len(self.srcs) and self.issued < self.consumed + n:
            t, b = self.slots[self.issued % len(self.slots)]
            self.S.dma("pool", [(t[:], self.srcs[self.issued])], rd=[], wr=[b], owner=b)
            self.issued += 1

    def get(self):
        self.prefetch()
        it = self.slots[self.consumed % len(self.slots)]
        self.consumed += 1
        return it


def build(L=DEPTH, nsub=None):
    if nsub is None:
        nsub = 3 * L
    nc = bass.Bass("TRN2", target_bir_lowering=False)
    es = ExitStack()
    es.enter_context(nc.allow_low_precision("bf16 matmuls with fp32 accumulation"))

    def din(name, shape, dt=F32):
        return nc.dram_tensor(name, shape, dt, kind="ExternalInput").ap()

    xT_d = din("xT", [1024, 2048])
    cT_d = din("cT", [128, 8])
    wgu_d = din("wgu", [L * 2 * 22 * 128, 2048])
    wd_d = din("wd", [L * 2 * 8 * 128, 2816])
    wqk_d = din("wqk", [L * 4 * 128, 3584])
    wv_d = din("wv", [L * 128, 6144])
    bqk_d = din("bqk", [64, L * 28])
    bv_d = din("bv", [L * 128, 768])
    wout_d = din("wout", [L * 128, 8192])
    adaw_d = din("adaw", [L * 36 * 128, 2048])
    vecs_d = din("vecs", [128, L * 128])
    ident_d = din("ident", [128, 128])
    blkind_d = din("blkind", [32, 8192])
    stair_d = din("stair", [128, 128])
    gmoba_d = din("gmoba", [128, 2 * 1152])
    cfar_d = din("cfar", [128, 2])
    gswa_d = din("gswa", [128, 4 * 256])
    snk_d = din("snk", [1, L * 2])
    out_d = nc.dram_tensor("outT", [1024, 2048], F32, kind="ExternalOutput").ap()
    send1 = nc.dram_tensor("send1", [3072, 2048], BF16)
    recv1 = nc.dram_tensor("recv1", [12 * 1024, 2048], BF16)
    send2 = nc.dram_tensor("send2", [256, 8192], BF16)
    recv2 = nc.dram_tensor("recv2", [4 * 256, 8192], BF16)
    mine1 = nc.dram_tensor("mine1", [3 * 1024, 2048], BF16)
    mine2 = nc.dram_tensor("mine2", [4 * 256, 2048], BF16)

    S = Sched(nc, es)
    B = S.B

    uid = [0]

    def sb(name, shape, dt, stack=es):
        uid[0] += 1
        return stack.enter_context(nc.sbuf_tensor(f"s{uid[0]}_{name}", shape, dt))

    xT = sb("xTs", [128, 8, 2048], F32)
    xb = [[B(f"x{dc}_{tg}") for tg in range(4)] for dc in range(8)]
    ident = sb("ident", [128, 128], F32)
    ones_bf = sb("ones_bf", [128, 128], BF16)
    onesf = sb("onesf", [128, 64], F32)
    stair = sb("stair", [128, 128], F32)
    vecs = sb("vecs", [128, L * 128], F32)
    cTs = sb("cTs", [128, 8], F32)
    cact = sb("cact", [128, 8], BF16)
    modt = sb("modt", [128, L * 72], F32)
    Acf = sb("Acf", [128, L * 24], F32)
    Bcf = sb("Bcf", [128, L * 24], F32)
    epsc = sb("epsc", [128, 1], F32)
    gmoba = sb("gmoba", [128, 2 * 1152], F32)
    cfar = sb("cfar", [128, 2], F32)
    gswa = sb("gswa", [128, 4 * 256], F32)
    snk = sb("snk", [65, L * 2], F32)
    esnk = sb("esnk", [65, L * 2], F32)
    bqk = sb("bqk", [64, L * 28], F32)
    constb = B("consts")
    wgu_slots = [(sb(f"wgu{i}", [128, 2048], BF16), B(f"wgu{i}")) for i in range(3)]
    wd_slots = [(sb(f"wd{i}", [128, 2816], BF16), B(f"wd{i}")) for i in range(2)]
    adw_slots = [(sb(f"adw{i}", [128, 2048], BF16), B(f"adw{i}")) for i in range(2)]
    sq_rot = Rot([(sb(f"sq{i}", [128, 512], BF16), B(f"sq{i}")) for i in range(2)])
    t32_rot = Rot([(sb(f"t32{i}", [128, 512], F32), B(f"t32{i}")) for i in range(2)])
    sd_t, sd_b = sb("sd", [128, 512], F32), B("sd")
    rs_rot = Rot([(sb(f"rs{i}", [128, 512], F32), B(f"rs{i}")) for i in range(2)])

    pbt = [es.enter_context(nc.psum_tensor(f"pb{i}", [128, 512], F32)) for i in range(8)]
    pbb = [B(f"pb{i}") for i in range(8)]

    pid = nc.partition_id()
    gidx = pid % 4

    wgu_srcs, wd_srcs = [], []
    for l in range(L):
        for wi in range(2):
            if 3 * l + 2 * wi >= nsub:
                continue
            for ps in range(2):
                for fc in range(22):
                    r0 = ((l * 2 + wi) * 22 + fc) * 128
                    wgu_srcs.append(wgu_d[r0:r0 + 128, :])
                for dc in range(8):
                    r0 = ((l * 2 + wi) * 8 + dc) * 128
                    wd_srcs.append(wd_d[r0:r0 + 128, :])
    wgu_st = WStream(S, wgu_slots, wgu_srcs)
    wd_st = WStream(S, wd_slots, wd_srcs)

    S.dma("sp", [(xT[:, dc, :], xT_d[dc * 128:(dc + 1) * 128, :]) for dc in range(8)],
          rd=[], wr=[b for row in xb for b in row], owner=B("xload"))
    S.dma("sp", [(ident[:], ident_d[:, :]), (stair[:], stair_d[:, :]), (vecs[:], vecs_d[:, :]),
                 (cTs[:], cT_d[:, :]), (gmoba[:], gmoba_d[:, :]), (cfar[:], cfar_d[:, :]),
                 (gswa[:], gswa_d[:, :]), (snk[64:65, :], snk_d[:, :]), (bqk[:], bqk_d[:, :])],
          rd=[], wr=[constb], owner=constb)
    cb2 = B("consts2")
    S.op("dve", lambda e: e.memset(ones_bf[:], 1.0), wr=[cb2])
    S.op("dve", lambda e: e.memset(onesf[:], 1.0), wr=[cb2])
    S.op("dve", lambda e: e.memset(epsc[:], EPS), wr=[cb2])
    S.op("act", lambda e: e.activation(out=cact[:], in_=cTs[:], func=AF.Silu), rd=[constb], wr=[cb2])
    S.op("act", lambda e: e.activation(out=esnk[64:65, :], in_=snk[64:65, :], func=AF.Exp), rd=[constb], wr=[cb2])
    CB = [constb, cb2]

    def mod_task(l):
        mb = B(f"mod{l}")
        adw_st = WStream(S, adw_slots, [adaw_d[(l * 36 + hb) * 128:(l * 36 + hb + 1) * 128, :] for hb in range(36)])
        for hb in range(36):
            wt, wb = adw_st.get()
            for j4 in range(2):
                col = hb * 2 + j4
                for dc in range(8):
                    S.op("pe", lambda e, dc=dc, j4=j4, col=col: e.matmul(
                        pbt[6][:, 128 + col:129 + col], lhsT=wt[:, dc * 256 + j4 * 128: dc * 256 + (j4 + 1) * 128],
                        rhs=cact[:, dc:dc + 1], start=(dc == 0), stop=(dc == 7)),
                        rd=[wb] + CB, wr=[pbb[6]], inc=(dc == 7))
            yield
        mo = modt[:, l * 72:(l + 1) * 72]
        S.op("dve", lambda e: e.tensor_tensor(out=mo, in0=pbt[6][:, 128:200], in1=vecs[:, l * 128: l * 128 + 72], op=ALU.add),
             rd=[pbb[6]] + CB, wr=[mb])
        coef = [0.5, 1.0, 0.5]
        for s in range(3):
            a = Acf[:, (l * 3 + s) * 8:(l * 3 + s + 1) * 8]
            bb = Bcf[:, (l * 3 + s) * 8:(l * 3 + s + 1) * 8]
            sc = modt[:, l * 72 + s * 24 + 8: l * 72 + s * 24 + 16]
            gt = modt[:, l * 72 + s * 24 + 16: l * 72 + s * 24 + 24]
            npre = vecs[:, l * 128 + 72 + s * 8: l * 128 + 72 + s * 8 + 8]
            npost = vecs[:, l * 128 + 96 + s * 8: l * 128 + 96 + s * 8 + 8]
            S.op("dve", lambda e, a=a, sc=sc, npre=npre: e.scalar_tensor_tensor(
                out=a, in0=sc, scalar=1.0, in1=npre, op0=ALU.add, op1=ALU.mult), rd=[mb] + CB, wr=[mb])
            S.op("dve", lambda e, bb=bb, gt=gt, npost=npost, cf=coef[s]: e.scalar_tensor_tensor(
                out=bb, in0=gt, scalar=cf, in1=npost, op0=ALU.mult, op1=ALU.mult), rd=[mb] + CB, wr=[mb])
        yield

    def run_task(t):
        for _ in t:
            pass

    def rstd_from(ssbank_i, scale):
        S.op("act", lambda e: e.activation(out=sd_t[:], in_=pbt[ssbank_i][:, :], func=AF.Sqrt,
                                           bias=epsc[:, 0:1], scale=scale), rd=[pbb[ssbank_i]] + CB, wr=[sd_b])
        rt, rb = rs_rot.next()
        S.op("dve", lambda e: e.reciprocal(out=rt[:], in_=sd_t[:]), rd=[sd_b], wr=[rb])
        return rt, rb

    def norm_x_to_h(l, s, tg, dst_fn, dst_bufs, ssbank_i):
        mb = B(f"mod{l}")
        tok = slice(tg * 512, (tg + 1) * 512)
        for dc in range(8):
            qt, qb = sq_rot.next()
            S.op("act", lambda e, dc=dc, qt=qt: e.activation(out=qt[:], in_=xT[:, dc, tok], func=AF.Square),
                 rd=[xb[dc][tg]], wr=[qb])
            S.op("pe", lambda e, dc=dc, qt=qt: e.matmul(pbt[ssbank_i][:, :], lhsT=ones_bf[:], rhs=qt[:],
                                                         start=(dc == 0), stop=(dc == 7)),
                 rd=[qb] + CB, wr=[pbb[ssbank_i]], inc=True)
        rt, rb = rstd_from(ssbank_i, 1.0 / 1024.0)
        for dc in range(8):
            tt, tb = t32_rot.next()
            acol = Acf[:, (l * 3 + s) * 8 + dc:(l * 3 + s) * 8 + dc + 1]
            shcol = modt[:, l * 72 + s * 24 + dc: l * 72 + s * 24 + dc + 1]
            S.op("dve", lambda e, dc=dc, tt=tt, acol=acol: e.scalar_tensor_tensor(
                out=tt[:], in0=xT[:, dc, tok], scalar=acol, in1=rt[:], op0=ALU.mult, op1=ALU.mult),
                rd=[xb[dc][tg], rb, mb], wr=[tb])
            S.op("act", lambda e, dc=dc, tt=tt, shcol=shcol: e.activation(
                out=dst_fn(dc), in_=tt[:], func=AF.Identity, bias=shcol, scale=1.0),
                rd=[tb, mb], wr=[dst_bufs[dc]])

    def post_residual(l, s, tg, ybf_fn, ybufs, ssbank_i):
        mb = B(f"mod{l}")
        tok = slice(tg * 512, (tg + 1) * 512)
        rt, rb = rstd_from(ssbank_i, 1.0 / 1024.0)
        for dc in range(8):
            tt, tb = t32_rot.next()
            bcol = Bcf[:, (l * 3 + s) * 8 + dc:(l * 3 + s) * 8 + dc + 1]
            S.op("dve", lambda e, dc=dc, tt=tt, bcol=bcol: e.scalar_tensor_tensor(
                out=tt[:], in0=ybf_fn(dc), scalar=bcol, in1=rt[:], op0=ALU.mult, op1=ALU.mult),
                rd=[ybufs[dc], rb, mb], wr=[tb])
            S.op("dve", lambda e, dc=dc, tt=tt: e.tensor_tensor(out=xT[:, dc, tok], in0=xT[:, dc, tok], in1=tt[:], op=ALU.add),
                 rd=[tb, xb[dc][tg]], wr=[xb[dc][tg]])

    def ffn(l, wi, s):
        with ExitStack() as fs:
            hT = sb("hT", [128, 8, 1024], BF16, fs)
            aT = sb("aT", [128, 22, 1024], BF16, fs)
            sg_rot = Rot([(sb(f"sg{i}", [128, 512], BF16, fs), B(f"sg{i}")) for i in range(2)])
            ysq_rot = Rot([(sb(f"ysq{i}", [128, 512], BF16, fs), B(f"ysq{i}")) for i in range(2)])
            hb = [[B(f"h{dc}_{t}") for t in range(2)] for dc in range(8)]
            ab = [[B(f"a{fc}_{t}") for t in range(2)] for fc in range(22)]
            scoped = [b for r in hb for b in r] + [b for r in ab for b in r] + [x[1] for x in sg_rot.items + ysq_rot.items]
            for ps in range(2):
                if DEBUG_CUT < 99 and ps == 1:
                    break
                wd_st.prefetch()
                for t in range(2):
                    tg = 2 * ps + t
                    norm_x_to_h(l, s, tg, lambda dc, t=t: hT[:, dc, t * 512:(t + 1) * 512], [hb[dc][t] for dc in range(8)], 6 + t)
                if DEBUG_CUT <= 1:
                    break
                for fc in range(22):
                    wt, wb = wgu_st.get()
                    for t in range(2):
                        for gu in range(2):
                            bk = 2 * gu + t
                            for dc in range(8):
                                S.op("pe", lambda e, dc=dc, gu=gu, bk=bk, t=t: e.matmul(
                                    pbt[bk][:, :], lhsT=wt[:, gu * 1024 + dc * 128: gu * 1024 + (dc + 1) * 128],
                                    rhs=hT[:, dc, t * 512:(t + 1) * 512], start=(dc == 0), stop=(dc == 7)),
                                    rd=[wb, hb[dc][t]], wr=[pbb[bk]], inc=(dc == 7))
                        st, sbf = sg_rot.next()
                        S.op("act", lambda e, st=st, t=t: e.activation(out=st[:], in_=pbt[t][:, :], func=AF.Silu),
                             rd=[pbb[t]], wr=[sbf])
                        S.op("dve", lambda e, st=st, t=t, fc=fc: e.tensor_tensor(
                            out=aT[:, fc, t * 512:(t + 1) * 512], in0=st[:], in1=pbt[2 + t][:, :], op=ALU.mult),
                            rd=[sbf, pbb[2 + t]], wr=[ab[fc][t]])
                wgu_st.prefetch()
                if DEBUG_CUT <= 2:
                    break
                for dc in range(8):
                    wt, wb = wd_st.get()
                    for t in range(2):
                        bk = 4 + t
                        for fc in range(22):
                            S.op("pe", lambda e, fc=fc, bk=bk, t=t: e.matmul(
                                pbt[bk][:, :], lhsT=wt[:, fc * 128:(fc + 1) * 128],
                                rhs=aT[:, fc, t * 512:(t + 1) * 512], start=(fc == 0), stop=(fc == 21)),
                                rd=[wb, ab[fc][t]], wr=[pbb[bk]], inc=(fc == 21))
                        yt, yb = ysq_rot.next()
                        S.op("act", lambda e, yt=yt, bk=bk: e.activation(out=yt[:], in_=pbt[bk][:, :], func=AF.Square),
                             rd=[pbb[bk]], wr=[yb])
                        S.op("act", lambda e, bk=bk, dc=dc, t=t: e.activation(
                            out=hT[:, dc, t * 512:(t + 1) * 512], in_=pbt[bk][:, :], func=AF.Copy),
                            rd=[pbb[bk]], wr=[hb[dc][t]])
                        S.op("pe", lambda e, yt=yt, t=t, dc=dc: e.matmul(
                            pbt[6 + t][:, :], lhsT=ones_bf[:], rhs=yt[:], start=(dc == 0), stop=(dc == 7)),
                            rd=[yb] + CB, wr=[pbb[6 + t]], inc=True)
                if DEBUG_CUT <= 3:
                    break
                for t in range(2):
                    post_residual(l, s, 2 * ps + t, lambda dc, t=t: hT[:, dc, t * 512:(t + 1) * 512],
                                  [hb[dc][t] for dc in range(8)], 6 + t)
            S.release(scoped)

    send1_b = [B(f"send1_{c}") for c in range(12)]
    recv1_b = [B(f"recv1_{c}") for c in range(12)]
    send2_b = [B(f"send2_{c}") for c in range(4)]
    recv2_b = [B(f"recv2_{c}") for c in range(4)]
    s1 = send1.ap()
    s1q = s1.rearrange("(g c k d) t -> g c k d t", g=4, c=3, k=4, d=64)
    s1v = s1.rearrange("(g c k d) (pl t m) -> g c k (d pl) t m", g=4, c=3, k=4, d=64, pl=2, t=16, m=64)
    r1g = recv1.ap().rearrange("(g c x) t -> g c x t", g=4, c=3)
    m1q = mine1.ap().rearrange("(c r k d) t -> c k d r t", c=3, r=4, k=4, d=64)
    m1v = mine1.ap().rearrange("(c r k d) (pl x) -> c k (d pl) r x", c=3, r=4, k=4, d=64, pl=2)
    s2 = send2.ap()
    m2 = mine2.ap().rearrange("(c r d) t -> c d r t", c=4, r=4, d=64)
    mine1_b = [B(f"mine1_{c}") for c in range(3)]
    mine2_b = [B(f"mine2_{c}") for c in range(4)]

    def ld_q(ct, k):
        return m1q[ct, k, :, :, :]

    def ld_v(ct, k):
        return m1v[ct, k, :, :, :]

    def stage_copy(ct):
        S.dma("sp", [(mine1.ap()[ct * 1024:(ct + 1) * 1024, :],
                      r1g[bass.ds(gidx, 1), ct, :, :].rearrange("o x t -> (o x) t"))],
              rd=[recv1_b[g_ * 3 + ct] for g_ in range(4)], wr=[mine1_b[ct]], owner=mine1_b[ct])

    def gather1():
        for ct in range(3):
            for g_ in range(4):
                c = g_ * 3 + ct
                S.allgather(s1[c * 256:(c + 1) * 256, :], recv1.ap()[c * 1024:(c + 1) * 1024, :], rd=[], wr=[send1_b[c], recv1_b[c]], name=f"g1_{c}")
        stage_copy(0)

    def gather2(c):
        S.allgather(s2[c * 64:(c + 1) * 64, :], recv2.ap()[c * 256:(c + 1) * 256, :], rd=[], wr=[send2_b[c], recv2_b[c]], name=f"g2_{c}")
        S.dma("sp", [(mine2.ap()[c * 256:(c + 1) * 256, :],
                      recv2.ap()[c * 256:(c + 1) * 256, :].rearrange("x (g t) -> x g t", g=4)[:, bass.ds(gidx, 1), :].rearrange("x o t -> x (o t)"))],
              rd=[recv2_b[c]], wr=[mine2_b[c]], owner=mine2_b[c])

    def normalize_store(obank_i, exps_col, slot, tgi, ost_rot, bcbank_i, rden_t, rden_b, of_t, of_b):
        ob = pbb[obank_i]
        if exps_col is not None:
            S.op("dve", lambda e: e.tensor_scalar(out=rden_t[64:65, :], in0=pbt[obank_i][64:65, :], scalar1=exps_col,
                                                  scalar2=None, op0=ALU.add), rd=[ob] + CB, wr=[rden_b])
        else:
            S.op("dve", lambda e: e.tensor_copy(out=rden_t[64:65, :], in_=pbt[obank_i][64:65, :]), rd=[ob], wr=[rden_b])
        S.op("dve", lambda e: e.reciprocal(out=rden_t[64:65, :], in_=rden_t[64:65, :]), rd=[rden_b], wr=[rden_b])
        S.op("pe", lambda e: e.matmul(pbt[bcbank_i][0:64, :], lhsT=onesf[64:65, 0:64], rhs=rden_t[64:65, :],
                                      start=True, stop=True), rd=[rden_b] + CB, wr=[pbb[bcbank_i]], inc=True)
        S.op("act", lambda e: e.activation(out=of_t[0:64, :], in_=pbt[obank_i][0:64, :], func=AF.Copy), rd=[ob], wr=[of_b])
        ot, obf = ost_rot.next()
        S.op("dve", lambda e: e.tensor_tensor(out=ot[0:64, :], in0=of_t[0:64, :], in1=pbt[bcbank_i][0:64, :], op=ALU.mult),
             rd=[of_b, pbb[bcbank_i]], wr=[obf])
        S.dma("sp", [(s2[slot * 64:(slot + 1) * 64, tgi * 512:(tgi + 1) * 512], ot[0:64, :])],
              rd=[obf, send2_b[slot]], wr=[], owner=obf)

    def attention(l, bg):
        s = 1
        mb = B(f"mod{l}")
        with ExitStack() as fs:
            hT4 = sb("hT4", [128, 8, 1024], BF16, fs)
            h4b = [[B(f"h4_{dc}_{t}") for t in range(2)] for dc in range(8)]
            wv = sb("wv", [128, 6144], BF16, fs)
            wvb = B("wv")
            bv = sb("bv", [128, 768], F32, fs)
            bvb = B("bv")
            wqk_slots = [(sb(f"wqk{i}", [128, 3584], BF16, fs), B(f"wqk{i}")) for i in range(2)]
            stq_rot = Rot([(sb(f"stq{i}", [64, 7, 512], BF16, fs), B(f"stq{i}")) for i in range(2)])
            stv_rot = Rot([(sb(f"stv{i}", [128, 12, 4, 64], BF16, fs), B(f"stv{i}")) for i in range(2)])
            scoped = [b for r in h4b for b in r] + [wvb, bvb] + [x[1] for x in wqk_slots + stq_rot.items + stv_rot.items]
            S.dma("pool", [(wv[:], wv_d[l * 128:(l + 1) * 128, :])], rd=[], wr=[wvb], owner=wvb)
            S.dma("sp", [(bv[:], bv_d[l * 128:(l + 1) * 128, :])], rd=[], wr=[bvb], owner=bvb)
            wqk_st = WStream(S, wqk_slots, [wqk_d[(l * 4 + gq) * 128:(l * 4 + gq + 1) * 128, :] for hf in range(2) for gq in range(4)])
            wqk_st.prefetch()
            for hf in range(2):
                for t in range(2):
                    tg = 2 * hf + t
                    norm_x_to_h(l, s, tg, lambda dc, t=t: hT4[:, dc, t * 512:(t + 1) * 512],
                                [h4b[dc][t] for dc in range(8)], 6 + t)
                for t in range(2):
                    tg = 2 * hf + t
                    vt, vb_ = stv_rot.next()
                    for tl in range(4):
                        c0 = t * 512 + tl * 128
                        for (bk, lo, hi) in ((0 + 2 * (tl % 2), 0, 512), (1 + 2 * (tl % 2), 512, 768)):
                            for dc in range(8):
                                S.op("pe", lambda e, dc=dc, bk=bk, lo=lo, hi=hi, c0=c0: e.matmul(
                                    pbt[bk][:, 0:hi - lo], lhsT=hT4[:, dc, c0:c0 + 128], rhs=wv[:, dc * 768 + lo: dc * 768 + hi],
                                    start=(dc == 0), stop=(dc == 7)), rd=[h4b[dc][t], wvb], wr=[pbb[bk]], inc=(dc == 7))
                            nvs = (hi - lo) // 64
                            S.op("dve", lambda e, bk=bk, lo=lo, hi=hi, nvs=nvs, tl=tl, vt=vt: e.tensor_tensor(
                                out=vt[:, lo // 64: lo // 64 + nvs, tl, :],
                                in0=pbt[bk][:, 0:hi - lo].rearrange("p (v m) -> p v m", m=64),
                                in1=bv[:, lo:hi].rearrange("p (v m) -> p v m", m=64), op=ALU.add),
                                rd=[pbb[bk], bvb], wr=[vb_])
                    vt5 = vt[:, :, :, :].rearrange("p (g j) t m -> p g j t m", j=3)
                    S.dma("sp", [(s1v[:, cj, kj, :, tg * 4:(tg + 1) * 4, :].rearrange("g p t m -> p g t m"), vt5[:, :, j, :, :])
                                 for (j, cj, kj) in ((0, 0, 3), (1, 1, 2), (2, 2, 2))],
                          rd=[vb_] + send1_b, wr=[], owner=vb_)
                for gq in range(4):
                    wt, wb = wqk_st.get()
                    for t in range(2):
                        tg = 2 * hf + t
                        qt, qb = stq_rot.next()
                        for sl in range(7):
                            bk = 4 + (sl % 2)
                            for dc in range(8):
                                S.op("pe", lambda e, dc=dc, sl=sl, bk=bk, t=t: e.matmul(
                                    pbt[bk][0:64, :], lhsT=wt[:, (sl * 8 + dc) * 64:(sl * 8 + dc + 1) * 64],
                                    rhs=hT4[:, dc, t * 512:(t + 1) * 512], start=(dc == 0), stop=(dc == 7)),
                                    rd=[wb, h4b[dc][t]], wr=[pbb[bk]], inc=(dc == 7))
                            bcol = bqk[:, l * 28 + gq * 7 + sl: l * 28 + gq * 7 + sl + 1]
                            S.op("act", lambda e, sl=sl, bk=bk, qt=qt, bcol=bcol: e.activation(
                                out=qt[:, sl, :], in_=pbt[bk][0:64, :], func=AF.Identity, bias=bcol, scale=1.0),
                                rd=[pbb[bk]] + CB, wr=[qb])
                        cs = slice(tg * 512, (tg + 1) * 512)
                        S.dma("sp", [(s1q[gq, 0, 0:3, :, cs].rearrange("k d t -> d k t"), qt[:, 0:3, :]),
                                     (s1q[gq, 1, 0:2, :, cs].rearrange("k d t -> d k t"), qt[:, 3:5, :]),
                                     (s1q[gq, 2, 0:2, :, cs].rearrange("k d t -> d k t"), qt[:, 5:7, :])],
                              rd=[qb] + send1_b[gq * 3:gq * 3 + 3], wr=[], owner=qb)
            S.release(scoped)
        if DEBUG_CUT == 11:
            return
        gather1()
        if DEBUG_CUT == 12:
            stage_copy(1)
            stage_copy(2)
            S.wait_all("sp", mine1_b)
            return

        with ExitStack() as fs:
            QTs = [sb(f"QTs{i}", [64, 8192], BF16, fs) for i in range(2)]
            KTs = sb("KTs", [64, 128 + 8192], BF16, fs)
            Vs = sb("Vs", [128, 65, 65], BF16, fs)
            vst = sb("vst", [128, 4, 1024], BF16, fs)
            qsb, ksb, vsb, vstb = B("QTs"), B("KTs"), B("Vs"), B("vst")
            P_rot = Rot([(sb(f"Ps{i}", [128, 256], BF16, fs), B(f"Ps{i}")) for i in range(3)])
            sb_rot = Rot([(sb(f"sbs{i}", [128, 256], F32, fs), B(f"sbs{i}")) for i in range(2)])
            ost_rot = Rot([(sb(f"ost{i}", [64, 512], BF16, fs), B(f"ost{i}")) for i in range(2)])
            rden_t, rden_b = sb("rden", [65, 512], F32, fs), B("rden")
            of_t, of_b = sb("of", [64, 512], F32, fs), B("of")
            scoped = [qsb, ksb, vsb, vstb, rden_b, of_b] + [x[1] for x in P_rot.items + sb_rot.items + ost_rot.items]
            S.dma("sp", [(QTs[i][:, :].rearrange("d (r t) -> d r t", r=4), ld_q(0, i)) for i in range(2)],
                  rd=[mine1_b[0]], wr=[qsb], owner=qsb)
            S.op("dve", lambda e: e.memset(KTs[:, 0:128], 0.0), wr=[ksb])
            S.dma("sp", [(KTs[:, 128:].rearrange("d (r t) -> d r t", r=4), ld_q(0, 2))],
                  rd=[mine1_b[0]], wr=[ksb], owner=ksb)
            S.dma("sp", [(vst[:, :, :], ld_v(0, 3))], rd=[mine1_b[0]], wr=[vstb], owner=vstb)
            S.op("dve", lambda e: e.memset(Vs[:, :, 64:65], 1.0), wr=[vsb])
            S.op("dve", lambda e: e.memset(Vs[:, 0, 0:64], 0.0), wr=[vsb])
            S.op("dve", lambda e: e.tensor_copy(out=Vs[:, 1:65, 0:64], in_=vst[:, :, :].rearrange("p r (t m) -> p (r t) m", m=64)),
                 rd=[vstb], wr=[vsb])
            for i in range(2):
                for tgi in range(16):
                    obk = 4 + (tgi % 2)
                    for qq in range(4):
                        T = tgi * 4 + qq
                        sbk = T % 3
                        for half in range(2):
                            S.op("pe", lambda e, T=T, half=half, sbk=sbk: e.matmul(
                                pbt[sbk][:, half * 128:(half + 1) * 128], lhsT=KTs[:, (T + half) * 128:(T + half + 1) * 128],
                                rhs=QTs[i][:, T * 128:(T + 1) * 128], start=True, stop=True),
                                rd=[ksb, qsb], wr=[pbb[sbk]], inc=(half == 1))
                        st, stb = sb_rot.next()
                        var = i * 2 + (1 if T == 0 else 0)
                        S.op("dve", lambda e, st=st, sbk=sbk, var=var: e.scalar_tensor_tensor(
                            out=st[:], in0=pbt[sbk][:, 0:256], scalar=0.125, in1=gswa[:, var * 256:(var + 1) * 256],
                            op0=ALU.mult, op1=ALU.add), rd=[pbb[sbk]] + CB, wr=[stb])
                        pt, ptb = P_rot.next()
                        S.op("act", lambda e, st=st, pt=pt: e.activation(out=pt[:], in_=st[:], func=AF.Exp), rd=[stb], wr=[ptb])
                        for half in range(2):
                            S.op("pe", lambda e, T=T, half=half, obk=obk, pt=pt, qq=qq: e.matmul(
                                pbt[obk][0:65, qq * 128:(qq + 1) * 128], lhsT=Vs[:, T + half, 0:65],
                                rhs=pt[:, half * 128:(half + 1) * 128], start=(half == 0), stop=(half == 1)),
                                rd=[vsb, ptb], wr=[pbb[obk]], inc=(half == 1))
                    normalize_store(obk, esnk[64:65, l * 2 + i: l * 2 + i + 1], i, tgi, ost_rot, 6 + (tgi % 2),
                                    rden_t, rden_b, of_t, of_b)
                gather2(i)
            S.release(scoped)

        if DEBUG_CUT == 13:
            return
        for i in range(2):
            with ExitStack() as fs:
                QTa = sb("QTa", [96, 8192], BF16, fs)
                KTa = sb("KTa", [96, 8192], BF16, fs)
                Va = sb("Va", [128, 64, 65], BF16, fs)
                vst = sb("vstm", [128, 4, 1024], BF16, fs)
                qab, kab, kib, vab, vstb = B("QTa"), B("KTa"), B("KTi"), B("Va"), B("vstm")
                qmb = [B(f"qm{t}") for t in range(16)]
                km32 = sb("km32", [64, 32], F32, fs)
                kmd = sb("kmd", [64, 32], F32, fs)
                kmh = sb("kmh", [64, 32], BF16, fs)
                kml = sb("kml", [64, 32], BF16, fs)
                kmb = B("km")
                gb_rot = Rot([(sb(f"gb{k}", [128, 32], F32, fs), B(f"gb{k}")) for k in range(2)])
                t8_rot = Rot([(sb(f"t8{k}", [128, 8], F32, fs), B(f"t8{k}")) for k in range(2)])
                mbt_rot = Rot([(sb(f"mbt{k}", [128, 4, 96], F32, fs), B(f"mbt{k}")) for k in range(2)])
                P_rot = Rot([(sb(f"Pm{k}", [128, 512], BF16, fs), B(f"Pm{k}")) for k in range(3)])
                sb_rot = Rot([(sb(f"sbm{k}", [128, 512], F32, fs), B(f"sbm{k}")) for k in range(2)])
                ost_rot = Rot([(sb(f"ostm{k}", [64, 512], BF16, fs), B(f"ostm{k}")) for k in range(2)])
                rden_t, rden_b = sb("rdenm", [65, 512], F32, fs), B("rdenm")
                of_t, of_b = sb("ofm", [64, 512], F32, fs), B("ofm")
                scoped = ([qab, kab, kib, vab, vstb, kmb, rden_b, of_b] + qmb +
                          [x[1] for x in gb_rot.items + t8_rot.items + mbt_rot.items + P_rot.items + sb_rot.items + ost_rot.items])
                stage_copy(1 + i)
                S.dma("sp", [(QTa[0:64, :].rearrange("d (r t) -> d r t", r=4), ld_q(1 + i, 0))],
                      rd=[mine1_b[1 + i]], wr=[qab], owner=qab)
                S.dma("sp", [(KTa[0:64, :].rearrange("d (r t) -> d r t", r=4), ld_q(1 + i, 1))],
                      rd=[mine1_b[1 + i]], wr=[kab], owner=kab)
                S.dma("pool", [(KTa[64:96, :], blkind_d[:, :])], rd=[], wr=[kib], owner=kib)
                S.dma("sp", [(vst[:, :, :], ld_v(1 + i, 2))], rd=[mine1_b[1 + i]], wr=[vstb], owner=vstb)
                S.op("dve", lambda e: e.memset(Va[:, :, 64:65], 1.0), wr=[vab])
                S.op("dve", lambda e: e.tensor_copy(out=Va[:, :, 0:64], in_=vst[:, :, :].rearrange("p r (t m) -> p (r t) m", m=64)),
                     rd=[vstb], wr=[vab])
                S.op("dve", lambda e: e.tensor_reduce(out=km32[:, :], in_=KTa[0:64, :].rearrange("d (b k) -> d b k", k=256),
                                                      axis=AX.X, op=ALU.add), rd=[kab], wr=[kmb])
                S.op("dve", lambda e: e.tensor_copy(out=kmh[:, :], in_=km32[:, :]), rd=[kmb], wr=[kmb])
                S.op("dve", lambda e: e.tensor_tensor(out=kmd[:, :], in0=km32[:, :], in1=kmh[:, :], op=ALU.subtract), rd=[kmb], wr=[kmb])
                S.op("dve", lambda e: e.tensor_copy(out=kml[:, :], in_=kmd[:, :]), rd=[kmb], wr=[kmb])
                mts = {}

                def g1(tgi):
                    mt, mtb = mbt_rot.next()
                    mts[tgi] = (mt, mtb)
                    for qq in range(4):
                        T = tgi * 4 + qq
                        cur = T // 2
                        if cur >= 3:
                            S.op("pe", lambda e, T=T, qq=qq: e.matmul(pbt[6][:, qq * 32:(qq + 1) * 32], lhsT=QTa[0:64, T * 128:(T + 1) * 128],
                                                                      rhs=kmh[:, :], start=True, stop=False), rd=[qab, kmb], wr=[pbb[6]], inc=False)
                            S.op("pe", lambda e, T=T, qq=qq: e.matmul(pbt[6][:, qq * 32:(qq + 1) * 32], lhsT=QTa[0:64, T * 128:(T + 1) * 128],
                                                                      rhs=kml[:, :], start=False, stop=True), rd=[qab, kmb], wr=[pbb[6]], inc=True)
                            gt, gtb = gb_rot.next()
                            S.op("dve", lambda e, gt=gt, qq=qq, cur=cur: e.tensor_tensor(
                                out=gt[:], in0=pbt[6][:, qq * 32:(qq + 1) * 32], in1=stair[:, 32 - cur:64 - cur], op=ALU.add),
                                rd=[pbb[6]] + CB, wr=[gtb])
                            t8, t8b = t8_rot.next()
                            S.op("dve", lambda e, gt=gt, t8=t8: e.max(out=t8[:], in_=gt[:]), rd=[gtb], wr=[t8b])
                            S.op("dve", lambda e, gt=gt, t8=t8, mt=mt, qq=qq: e.tensor_scalar(
                                out=mt[:, qq, 64:96], in0=gt[:], scalar1=t8[:, 2:3], scalar2=1.0, op0=ALU.is_ge, op1=ALU.subtract),
                                rd=[gtb, t8b], wr=[mtb])
                            S.op("dve", lambda e, mt=mt, qq=qq, cur=cur: e.memset(mt[:, qq, 64 + cur:65 + cur], 0.0), wr=[mtb])
                        else:
                            S.op("dve", lambda e, mt=mt, qq=qq, cur=cur: e.tensor_copy(
                                out=mt[:, qq, 64:96], in_=stair[:, 64 + 31 - cur:64 + 63 - cur]), rd=CB, wr=[mtb])

                def g2(tgi):
                    mt, mtb = mts.pop(tgi)
                    for qq in range(4):
                        S.op("pe", lambda e, mt=mt, qq=qq: e.transpose(out=pbt[7][0:96, qq * 128:(qq + 1) * 128], in_=mt[:, qq, :],
                                                                       identity=ident[:, :]), rd=[mtb] + CB, wr=[pbb[7]], inc=True)
                    S.op("act", lambda e, tgi=tgi: e.activation(out=QTa[64:96, tgi * 512:(tgi + 1) * 512], in_=pbt[7][64:96, :], func=AF.Copy),
                         rd=[pbb[7]], wr=[qmb[tgi]])

                def main(tgi):
                    obk = 4 + (tgi % 2)
                    nkt = 4 * tgi + 4
                    for kt in range(nkt):
                        sbk = kt % 3
                        S.op("pe", lambda e, kt=kt, sbk=sbk, tgi=tgi: e.matmul(
                            pbt[sbk][:, :], lhsT=KTa[0:96, kt * 128:(kt + 1) * 128], rhs=QTa[0:96, tgi * 512:(tgi + 1) * 512],
                            start=True, stop=True), rd=[kab, kib, qab, qmb[tgi]], wr=[pbb[sbk]], inc=True)
                        pt, ptb = P_rot.next()
                        rel = kt - 4 * tgi
                        if rel >= -2:
                            j0 = i * 1152 + 384 - 128 * rel
                            st, stb = sb_rot.next()
                            S.op("dve", lambda e, st=st, sbk=sbk, j0=j0: e.scalar_tensor_tensor(
                                out=st[:], in0=pbt[sbk][:, :], scalar=0.125, in1=gmoba[:, j0:j0 + 512], op0=ALU.mult, op1=ALU.add),
                                rd=[pbb[sbk]] + CB, wr=[stb])
                            S.op("act", lambda e, st=st, pt=pt: e.activation(out=pt[:], in_=st[:], func=AF.Exp), rd=[stb], wr=[ptb])
                        else:
                            S.op("act", lambda e, pt=pt, sbk=sbk: e.activation(out=pt[:], in_=pbt[sbk][:, :], func=AF.Exp,
                                                                              bias=cfar[:, i:i + 1], scale=0.125), rd=[pbb[sbk]] + CB, wr=[ptb])
                        S.op("pe", lambda e, kt=kt, obk=obk, pt=pt, nkt=nkt: e.matmul(
                            pbt[obk][0:65, :], lhsT=Va[:, kt, 0:65], rhs=pt[:], start=(kt == 0), stop=(kt == nkt - 1)),
                            rd=[vab, ptb], wr=[pbb[obk]], inc=(kt == nkt - 1))
                    normalize_store(obk, None, 2 + i, tgi, ost_rot, 3, rden_t, rden_b, of_t, of_b)
                    if bg is not None:
                        next(bg, None)
                        next(bg, None)

                g1(0)
                g2(0)
                for tgi in range(16):
                    if tgi + 1 < 16:
                        g1(tgi + 1)
                    main(tgi)
                    if tgi + 1 < 16:
                        g2(tgi + 1)
                gather2(2 + i)
                S.release(scoped)
        if bg is not None:
            run_task(bg)
        if DEBUG_CUT == 14:
            return

        with ExitStack() as fs:
            wo = sb("wo", [128, 8192], BF16, fs)
            wob = B("wo")
            OT_rot = Rot([(sb(f"OT{k}", [128, 8, 512], BF16, fs), B(f"OT{k}")) for k in range(2)])
            osq = sb("osq", [128, 8, 512], BF16, fs)
            osqb = B("osq")
            OnT = sb("OnT", [128, 8, 512], BF16, fs)
            onb = [B(f"on{k}") for k in range(8)]
            ybf = sb("ybf", [128, 8, 512], BF16, fs)
            ybb = [B(f"yb{k}") for k in range(8)]
            ysq_rot = Rot([(sb(f"ysqo{k}", [128, 512], BF16, fs), B(f"ysqo{k}")) for k in range(2)])
            rsa_t, rsa_b = sb("rsa", [128, 512], F32, fs), B("rsa")
            scoped = [wob, osqb, rsa_b] + onb + ybb + [x[1] for x in OT_rot.items + ysq_rot.items]
            S.dma("pool", [(wo[:], wout_d[l * 128:(l + 1) * 128, :])], rd=[], wr=[wob], owner=wob)
            for tg in range(4):
                ot, otb = OT_rot.next()
                S.dma("sp", [(ot[:, :, :].rearrange("p (r par) t -> p r par t", par=2)[(sl % 2) * 64:(sl % 2) * 64 + 64, :, sl // 2, :], m2[sl, :, :, tg * 512:(tg + 1) * 512])
                             for sl in range(4)], rd=mine2_b, wr=[otb], owner=otb)
                S.op("dve", lambda e, ot=ot: e.tensor_tensor(out=osq[:, :, :], in0=ot[:, :, :], in1=ot[:, :, :], op=ALU.mult),
                     rd=[otb], wr=[osqb])
                for par in range(2):
                    for k in range(4):
                        kc = 2 * k + par
                        S.op("pe", lambda e, kc=kc, k=k, par=par: e.matmul(pbt[6 + par][:, :], lhsT=ones_bf[:], rhs=osq[:, kc, :],
                                                                         start=(k == 0), stop=(k == 3)),
                             rd=[osqb] + CB, wr=[pbb[6 + par]], inc=(k == 3))
                S.op("act", lambda e: e.activation(out=sd_t[:], in_=pbt[6][:, :], func=AF.Sqrt, bias=epsc[:, 0:1], scale=1.0 / 512.0),
                     rd=[pbb[6]] + CB, wr=[sd_b])
                S.op("dve", lambda e: e.reciprocal(out=rsa_t[:], in_=sd_t[:]), rd=[sd_b], wr=[rsa_b])
                rbt, rbb = rstd_from(7, 1.0 / 512.0)
                for kc in range(8):
                    rr, rrb = (rsa_t, rsa_b) if kc % 2 == 0 else (rbt, rbb)
                    gcol = vecs[:, l * 128 + 120 + kc: l * 128 + 121 + kc]
                    S.op("dve", lambda e, kc=kc, rr=rr, gcol=gcol, ot=ot: e.scalar_tensor_tensor(
                        out=OnT[:, kc, :], in0=ot[:, kc, :], scalar=gcol, in1=rr[:], op0=ALU.mult, op1=ALU.mult),
                        rd=[otb, rrb] + CB, wr=[onb[kc]])
                for dm in range(8):
                    bk = 4 + (dm % 2)
                    for kc in range(8):
                        S.op("pe", lambda e, kc=kc, dm=dm, bk=bk: e.matmul(
                            pbt[bk][:, :], lhsT=wo[:, kc * 1024 + dm * 128: kc * 1024 + (dm + 1) * 128], rhs=OnT[:, kc, :],
                            start=(kc == 0), stop=(kc == 7)), rd=[wob, onb[kc]], wr=[pbb[bk]], inc=(kc == 7))
                    yt, yb = ysq_rot.next()
                    S.op("act", lambda e, yt=yt, bk=bk: e.activation(out=yt[:], in_=pbt[bk][:, :], func=AF.Square), rd=[pbb[bk]], wr=[yb])
                    S.op("act", lambda e, dm=dm, bk=bk: e.activation(out=ybf[:, dm, :], in_=pbt[bk][:, :], func=AF.Copy),
                         rd=[pbb[bk]], wr=[ybb[dm]])
                    S.op("pe", lambda e, yt=yt, dm=dm: e.matmul(pbt[3][:, :], lhsT=ones_bf[:], rhs=yt[:], start=(dm == 0), stop=(dm == 7)),
                         rd=[yb] + CB, wr=[pbb[3]], inc=True)
                post_residual(l, s, tg, lambda dc: ybf[:, dc, :], ybb, 3)
            S.release(scoped)

    with ExitStack() as zs:
        zt = sb("zpad", [64, 2048], BF16, zs)
        zb = B("zpad")
        S.op("dve", lambda e: e.memset(zt[:], 0.0), wr=[zb])
        S.dma("sp", [(s1q[g_, ct, 3, :, :], zt[:]) for g_ in range(4) for ct in (1, 2)],
              rd=[zb] + send1_b, wr=[], owner=zb)
        S.release([zb])
    run_task(mod_task(0))
    sub = 0
    for l in range(L):
        if sub < nsub:
            ffn(l, 0, 0)
            sub += 1
        if sub < nsub:
            bg = mod_task(l + 1) if (l + 1 < L and 3 * (l + 1) < nsub) else None
            attention(l, bg)
            sub += 1
        elif l + 1 < L:
            pass
        if sub < nsub:
            ffn(l, 1, 2)
            sub += 1
    allx = [b for row in xb for b in row]
    outb = B("outb")
    S.dma("sp", [(out_d[dc * 128:(dc + 1) * 128, :], xT[:, dc, :]) for dc in range(8)], rd=allx, wr=[outb], owner=outb)
    S.wait_all("sp", [outb])
    es.close()
    return nc


def _t5_bucket_np(dist):
    n = np.maximum(dist, 0)
    nf = np.maximum(n, 1).astype(np.float32)
    large = 16 + (np.log(nf / np.float32(16)) / np.float32(math.log(128 / 16)) * np.float32(16)).astype(np.int32)
    large = np.minimum(large, 31)
    return np.where(n < 16, n, large)


def _slot_cols():
    qk, v = [], []
    for g in range(4):
        qk += [64 * (2 * g), 64 * (2 * g + 1), 512 + 64 * (g // 2), 768 + 64 * (2 * g), 1280 + 64 * (2 * g),
               768 + 64 * (2 * g + 1), 1280 + 64 * (2 * g + 1)]
        v += [640 + 64 * (g // 2), 1792 + 64 * (2 * g), 1792 + 64 * (2 * g + 1)]
    return qk, v


def prep_inputs(inp, L=DEPTH, l0=0, x=None):
    f = np.float32
    x = np.asarray(inp["x"], f) if x is None else x
    c = np.asarray(inp["c"], f)
    rel = np.asarray(inp["rel_bias"], f)
    ada_w = np.asarray(inp["ada_w"][l0:l0 + L], f)
    ada_b = np.asarray(inp["ada_b"][l0:l0 + L], f)
    npre = np.asarray(inp["norm_pre"][l0:l0 + L], f)
    npost = np.asarray(inp["norm_post"][l0:l0 + L], f)
    wg = np.asarray(inp["ffn_w_gate"][l0:l0 + L], f)
    wu = np.asarray(inp["ffn_w_up"][l0:l0 + L], f)
    wdn = np.asarray(inp["ffn_w_down"][l0:l0 + L], f)
    w_in = np.asarray(inp["mix_w_in"][l0:l0 + L], f)
    b_in = np.asarray(inp["mix_b_in"][l0:l0 + L], f)
    w_out = np.asarray(inp["mix_w_out"][l0:l0 + L], f)
    sinks = np.asarray(inp["attn_sinks"][l0:l0 + L], f)
    gain = np.asarray(inp["group_gain"][l0:l0 + L], f)

    g6 = wg.reshape(L, 2, 8, 128, 22, 128)
    u6 = wu.reshape(L, 2, 8, 128, 22, 128)
    gu = np.stack([g6, u6], axis=2)
    wgu = np.ascontiguousarray(gu.transpose(0, 1, 5, 4, 2, 3, 6)).reshape(L * 2 * 22 * 128, 2048)
    d6 = wdn.reshape(L, 2, 22, 128, 8, 128)
    wd = np.ascontiguousarray(d6.transpose(0, 1, 4, 3, 2, 5)).reshape(L * 2 * 8 * 128, 2816)
    qk_cols, v_cols = _slot_cols()
    qk_idx = np.concatenate([np.arange(cb, cb + 64) for cb in qk_cols])
    v_idx = np.concatenate([np.arange(cb, cb + 64) for cb in v_cols])
    wq = w_in[:, :, qk_idx].reshape(L, 8, 128, 4, 7, 64)
    wqk = np.ascontiguousarray(wq.transpose(0, 3, 2, 4, 1, 5)).reshape(L * 4 * 128, 3584)
    wvv = w_in[:, :, v_idx].reshape(L, 8, 128, 768)
    wv = np.ascontiguousarray(wvv.transpose(0, 2, 1, 3)).reshape(L * 128, 6144)
    bqk = np.ascontiguousarray(b_in[:, qk_idx].reshape(L, 28, 64).transpose(2, 0, 1)).reshape(64, L * 28)
    bv = np.ascontiguousarray(np.broadcast_to(b_in[:, None, v_idx], (L, 128, 768))).reshape(L * 128, 768)
    rowperm = np.concatenate([np.concatenate([np.arange(128 * g, 128 * g + 128), np.arange(512 + 128 * g, 512 + 128 * g + 128)])
                              for g in range(4)])
    wo = w_out[:, rowperm, :].reshape(L, 8, 128, 1024)
    wout = np.ascontiguousarray(wo.transpose(0, 2, 1, 3)).reshape(L * 128, 8192)
    aw = ada_w.reshape(L, 8, 128, 36, 256)
    adaw = np.ascontiguousarray(aw.transpose(0, 3, 2, 1, 4)).reshape(L * 36 * 128, 2048)
    vecs = np.zeros((128, L * 128), f)
    for l in range(L):
        vecs[:, l * 128: l * 128 + 72] = ada_b[l].reshape(72, 128).T
        vecs[:, l * 128 + 72: l * 128 + 96] = npre[l].reshape(24, 128).T
        vecs[:, l * 128 + 96: l * 128 + 120] = npost[l].reshape(24, 128).T
        vecs[:, l * 128 + 120: l * 128 + 128] = gain[l][rowperm].reshape(8, 128).T
    ident = np.eye(128, dtype=f)
    blkind = np.zeros((32, 8192), f)
    for b in range(32):
        blkind[b, b * 256:(b + 1) * 256] = MASKV
    stair = np.zeros((128, 128), f)
    stair[:, 32:64] = -1e30
    stair[:, 96:128] = -1.0

    kk = np.arange(128)[:, None]
    shared = dict(wgu=wgu, wd=wd, wqk=wqk, wv=wv, bqk=bqk, bv=bv, wout=wout, adaw=adaw, vecs=vecs,
                  ident=ident, blkind=blkind, stair=stair)
    in_maps = []
    for core in range(8):
        bt, g = core // 4, core % 4
        m = dict(shared)
        m["xT"] = np.ascontiguousarray(x[bt, g * 2048:(g + 1) * 2048, :].T)
        m["cT"] = np.ascontiguousarray(c[bt].reshape(8, 128).T)
        gm = np.zeros((128, 2 * 1152), f)
        cf = np.zeros((128, 2), f)
        for i in range(2):
            h = 8 + 2 * g + i
            d = np.arange(1152)[None, :] - 384 - kk
            val = rel[_t5_bucket_np(d), h]
            gm[:, i * 1152:(i + 1) * 1152] = np.where(d >= 0, val, f(-30000.0))
            cf[:, i] = rel[31, h]
        m["gmoba"] = gm
        m["cfar"] = cf
        gs = np.zeros((128, 4 * 256), f)
        qq = np.arange(128)[None, :]
        for i in range(2):
            h = 2 * g + i
            d0 = qq + 128 - kk
            d1 = qq - kk
            a0 = np.where((d0 >= 0) & (d0 < 128), rel[_t5_bucket_np(d0), h], f(-30000.0))
            a1 = np.where((d1 >= 0) & (d1 < 128), rel[_t5_bucket_np(d1), h], f(-30000.0))
            gs[:, (i * 2) * 256:(i * 2) * 256 + 128] = a0
            gs[:, (i * 2) * 256 + 128:(i * 2 + 1) * 256] = a1
            gs[:, (i * 2 + 1) * 256:(i * 2 + 1) * 256 + 128] = f(-30000.0)
            gs[:, (i * 2 + 1) * 256 + 128:(i * 2 + 2) * 256] = a1
        m["gswa"] = gs
        m["snk"] = np.ascontiguousarray(sinks[:, 2 * g:2 * g + 2].reshape(1, L * 2))
        in_maps.append(m)
    return in_maps


_NC_CACHE = {}


def run(inputs, L=DEPTH, nsub=None, l0=0, x=None):
    key = (L, nsub)
    if key not in _NC_CACHE:
        _NC_CACHE[key] = build(L, nsub)
    nc = _NC_CACHE[key]
    in_maps = prep_inputs(inputs, L, l0, x)
    res = run_bass_kernel_spmd(nc, in_maps, core_ids=list(range(8)))
    out = np.zeros((2, 8192, 1024), np.float32)
    for core in range(8):
        bt, g = core // 4, core % 4
        out[bt, g * 2048:(g + 1) * 2048, :] = np.asarray(res.results[core]["outT"], np.float32).T
    return out


LAYERS_PER_LAUNCH = 4


def kernel(**inputs):
    x = None
    for l0 in range(0, DEPTH, LAYERS_PER_LAUNCH):
        x = run(inputs, LAYERS_PER_LAUNCH, None, l0, x)
    return x
```

```python
import math
import numpy as np
from contextlib import ExitStack
import concourse.bass as bass
import concourse.mybir as mybir
from concourse.bass_utils import run_bass_kernel_spmd

F32 = mybir.dt.float32
BF16 = mybir.dt.bfloat16
ALU = mybir.AluOpType
AF = mybir.ActivationFunctionType
AX = mybir.AxisListType

DEPTH = 4
DEBUG_CUT = 99
EPS = 1e-6
MASKV = 240000.0


class Tok:
    __slots__ = ("sem", "key", "val", "eng")

    def __init__(self, sem, key, val, eng):
        self.sem, self.key, self.val, self.eng = sem, key, val, eng


class Buf:
    def __init__(self, name):
        self.name = name
        self.w = None
        self.r = {}
        self.dsem = None
        self.dkey = None
        self.dcnt = 0


class Sched:
    def __init__(self, nc, es):
        self.nc, self.es = nc, es
        self.E = {"pe": nc.tensor, "act": nc.scalar, "dve": nc.vector, "pool": nc.gpsimd, "sp": nc.sync}
        self.sem, self.key, self.cnt = {}, {}, {}
        self.known = {e: {} for e in self.E}
        self.nsem = 0
        self.bufs = {}
        for e in self.E:
            self.new_epoch(e)

    def newsem(self, name):
        self.nsem += 1
        nm = f"{name}_{self.nsem}"
        return self.es.enter_context(self.nc.semaphore(nm)), nm

    def new_epoch(self, e):
        self.sem[e], self.key[e] = self.newsem("e" + e)
        self.cnt[e] = 0

    def B(self, name):
        b = self.bufs.get(name)
        if b is None:
            b = self.bufs[name] = Buf(name)
        return b

    def _wait(self, e, toks, skip_pe=False, defer=False):
        need = {}
        for t in toks:
            if t is None:
                continue
            if skip_pe and t.eng == "pe":
                continue
            cur = need.get(t.key)
            if cur is None or cur.val < t.val:
                need[t.key] = t
        kn = self.known[e]
        todo = [t for k, t in need.items() if kn.get(k, 0) < t.val]
        last = None
        if defer and todo:
            last = todo.pop()
            kn[last.key] = last.val
        for t in todo:
            self.E[e].wait_ge(t.sem, t.val)
            kn[t.key] = t.val
        return last

    @staticmethod
    def _deps(rd, wr):
        toks = []
        for b in rd:
            toks.append(b.w)
        for b in wr:
            toks.append(b.w)
            toks.extend(b.r.values())
        return toks

    @staticmethod
    def _record(tok, rd, wr):
        for b in rd:
            c = b.r.get(tok.key)
            if c is None or c.val < tok.val:
                b.r[tok.key] = tok
        for b in wr:
            b.w = tok
            b.r = {}

    def op(self, e, fn, rd=(), wr=(), inc=True):
        last = self._wait(e, self._deps(rd, wr), skip_pe=(e == "pe"), defer=True)
        ins = fn(self.E[e])
        if last is not None:
            ins._wait_ge(last.sem, last.val)
        if inc:
            ins.then_inc(self.sem[e], 1)
            self.cnt[e] += 1
            tok = Tok(self.sem[e], self.key[e], self.cnt[e], e)
        else:
            tok = Tok(self.sem[e], self.key[e], self.cnt[e] + 1, e)
        self._record(tok, rd, wr)
        return tok

    def dma(self, q, pairs, rd, wr, owner):
        if owner.dsem is None:
            owner.dsem, owner.dkey = self.newsem("d")
        last = self._wait(q, self._deps(rd, wr), defer=True)
        for (o, i) in pairs:
            ins = self.E[q].dma_start(out=o, in_=i)
            if last is not None:
                ins._wait_ge(last.sem, last.val)
                last = None
            ins.then_inc(owner.dsem, 16)
            owner.dcnt += 16
        tok = Tok(owner.dsem, owner.dkey, owner.dcnt, "dma")
        self._record(tok, rd, wr)
        return tok

    def allgather(self, src_ap, dst_ap, rd, wr, name):
        if not hasattr(self, "ccs"):
            self.ccs = {}
        if name not in self.ccs:
            self.ccs[name] = list(self.newsem("cc")) + [0]
        ent = self.ccs[name]
        ent[2] += 1
        sem, key = ent[0], ent[1]
        self._wait("pool", self._deps(rd, wr))
        self.nc.gpsimd.collective_compute(
            "AllGather", ALU.bypass, replica_groups=[[0, 1, 2, 3], [4, 5, 6, 7]],
            ins=[src_ap.opt()], outs=[dst_ap.opt()],
        ).then_inc(sem)
        tok = Tok(sem, key, ent[2], "cc")
        self._record(tok, rd, wr)
        return tok

    def release(self, bufs):
        toks = []
        for b in bufs:
            toks.append(b.w)
            toks.extend(b.r.values())
        for e in self.E:
            self._wait(e, toks)

    def wait_all(self, e, bufs):
        toks = []
        for b in bufs:
            toks.append(b.w)
            toks.extend(b.r.values())
        self._wait(e, toks)


class Rot:
    def __init__(self, items):
        self.items, self.i = items, 0

    def next(self):
        it = self.items[self.i % len(self.items)]
        self.i += 1
        return it


class WStream:
    def __init__(self, S, slots, srcs):
        self.S, self.slots, self.srcs = S, slots, srcs
        self.issued = 0
        self.consumed = 0

    def prefetch(self, ahead=None):
        n = len(self.slots) if ahead is None else ahead
        while self.issued < len(self.srcs) and self.issued < self.consumed + n:
            t, b = self.slots[self.issued % len(self.slots)]
            self.S.dma("pool", [(t[:], self.srcs[self.issued])], rd=[], wr=[b], owner=b)
            self.issued += 1

    def get(self):
        self.prefetch()
        it = self.slots[self.consumed % len(self.slots)]
        self.consumed += 1
        return it


def build(L=DEPTH, nsub=None):
    if nsub is None:
        nsub = 3 * L
    nc = bass.Bass("TRN2", target_bir_lowering=False)
    es = ExitStack()
    es.enter_context(nc.allow_low_precision("bf16 matmuls with fp32 accumulation"))

    def din(name, shape, dt=F32):
        return nc.dram_tensor(name, shape, dt, kind="ExternalInput").ap()

    xT_d = din("xT", [1024, 2048])
    cT_d = din("cT", [128, 8])
    wgu_d = din("wgu", [L * 2 * 22 * 128, 2048])
    wd_d = din("wd", [L * 2 * 8 * 128, 2816])
    wqk_d = din("wqk", [L * 4 * 128, 3584])
    wv_d = din("wv", [L * 128, 6144])
    bqk_d = din("bqk", [64, L * 28])
    bv_d = din("bv", [L * 128, 768])
    wout_d = din("wout", [L * 128, 8192])
    adaw_d = din("adaw", [L * 36 * 128, 2048])
    vecs_d = din("vecs", [128, L * 128])
    ident_d = din("ident", [128, 128])
    blkind_d = din("blkind", [32, 8192])
    stair_d = din("stair", [128, 128])
    gmoba_d = din("gmoba", [128, 2 * 1152])
    cfar_d = din("cfar", [128, 2])
    gswa_d = din("gswa", [128, 4 * 256])
    snk_d = din("snk", [1, L * 2])
    out_d = nc.dram_tensor("outT", [1024, 2048], F32, kind="ExternalOutput").ap()
    send1 = nc.dram_tensor("send1", [3072, 2048], BF16)
    recv1 = nc.dram_tensor("recv1", [12 * 1024, 2048], BF16)
    send2 = nc.dram_tensor("send2", [256, 8192], BF16)
    recv2 = nc.dram_tensor("recv2", [4 * 256, 8192], BF16)
    mine1 = nc.dram_tensor("mine1", [3 * 1024, 2048], BF16)
    mine2 = nc.dram_tensor("mine2", [4 * 256, 2048], BF16)

    S = Sched(nc, es)
    B = S.B

    uid = [0]

    def sb(name, shape, dt, stack=es):
        uid[0] += 1
        return stack.enter_context(nc.sbuf_tensor(f"s{uid[0]}_{name}", shape, dt))

    xT = sb("xTs", [128, 8, 2048], F32)
    xb = [[B(f"x{dc}_{tg}") for tg in range(4)] for dc in range(8)]
    ident = sb("ident", [128, 128], F32)
    ones_bf = sb("ones_bf", [128, 128], BF16)
    onesf = sb("onesf", [128, 64], F32)
    stair = sb("stair", [128, 128], F32)
    vecs = sb("vecs", [128, L * 128], F32)
    cTs = sb("cTs", [128, 8], F32)
    cact = sb("cact", [128, 8], BF16)
    modt = sb("modt", [128, L * 72], F32)
    Acf = sb("Acf", [128, L * 24], F32)
    Bcf = sb("Bcf", [128, L * 24], F32)
    epsc = sb("epsc", [128, 1], F32)
    gmoba = sb("gmoba", [128, 2 * 1152], F32)
    cfar = sb("cfar", [128, 2], F32)
    gswa = sb("gswa", [128, 4 * 256], F32)
    snk = sb("snk", [65, L * 2], F32)
    esnk = sb("esnk", [65, L * 2], F32)
    bqk = sb("bqk", [64, L * 28], F32)
    constb = B("consts")
    wgu_slots = [(sb(f"wgu{i}", [128, 2048], BF16), B(f"wgu{i}")) for i in range(3)]
    wd_slots = [(sb(f"wd{i}", [128, 2816], BF16), B(f"wd{i}")) for i in range(2)]
    adw_slots = [(sb(f"adw{i}", [128, 2048], BF16), B(f"adw{i}")) for i in range(2)]
    sq_rot = Rot([(sb(f"sq{i}", [128, 512], BF16), B(f"sq{i}")) for i in range(2)])
    t32_rot = Rot([(sb(f"t32{i}", [128, 512], F32), B(f"t32{i}")) for i in range(2)])
    sd_t, sd_b = sb("sd", [128, 512], F32), B("sd")
    rs_rot = Rot([(sb(f"rs{i}", [128, 512], F32), B(f"rs{i}")) for i in range(2)])

    pbt = [es.enter_context(nc.psum_tensor(f"pb{i}", [128, 512], F32)) for i in range(8)]
    pbb = [B(f"pb{i}") for i in range(8)]

    pid = nc.partition_id()
    gidx = pid % 4

    wgu_srcs, wd_srcs = [], []
    for l in range(L):
        for wi in range(2):
            if 3 * l + 2 * wi >= nsub:
                continue
            for ps in range(2):
                for fc in range(22):
                    r0 = ((l * 2 + wi) * 22 + fc) * 128
                    wgu_srcs.append(wgu_d[r0:r0 + 128, :])
                for dc in range(8):
                    r0 = ((l * 2 + wi) * 8 + dc) * 128
                    wd_srcs.append(wd_d[r0:r0 + 128, :])
    wgu_st = WStream(S, wgu_slots, wgu_srcs)
    wd_st = WStream(S, wd_slots, wd_srcs)

    S.dma("sp", [(xT[:, dc, :], xT_d[dc * 128:(dc + 1) * 128, :]) for dc in range(8)],
          rd=[], wr=[b for row in xb for b in row], owner=B("xload"))
    S.dma("sp", [(ident[:], ident_d[:, :]), (stair[:], stair_d[:, :]), (vecs[:], vecs_d[:, :]),
                 (cTs[:], cT_d[:, :]), (gmoba[:], gmoba_d[:, :]), (cfar[:], cfar_d[:, :]),
                 (gswa[:], gswa_d[:, :]), (snk[64:65, :], snk_d[:, :]), (bqk[:], bqk_d[:, :])],
          rd=[], wr=[constb], owner=constb)
    cb2 = B("consts2")
    S.op("dve", lambda e: e.memset(ones_bf[:], 1.0), wr=[cb2])
    S.op("dve", lambda e: e.memset(onesf[:], 1.0), wr=[cb2])
    S.op("dve", lambda e: e.memset(epsc[:], EPS), wr=[cb2])
    S.op("act", lambda e: e.activation(out=cact[:], in_=cTs[:], func=AF.Silu), rd=[constb], wr=[cb2])
    S.op("act", lambda e: e.activation(out=esnk[64:65, :], in_=snk[64:65, :], func=AF.Exp), rd=[constb], wr=[cb2])
    CB = [constb, cb2]

    def mod_task(l):
        mb = B(f"mod{l}")
        adw_st = WStream(S, adw_slots, [adaw_d[(l * 36 + hb) * 128:(l * 36 + hb + 1) * 128, :] for hb in range(36)])
        for hb in range(36):
            wt, wb = adw_st.get()
            for j4 in range(2):
                col = hb * 2 + j4
                for dc in range(8):
                    S.op("pe", lambda e, dc=dc, j4=j4, col=col: e.matmul(
                        pbt[6][:, 128 + col:129 + col], lhsT=wt[:, dc * 256 + j4 * 128: dc * 256 + (j4 + 1) * 128],
                        rhs=cact[:, dc:dc + 1], start=(dc == 0), stop=(dc == 7)),
                        rd=[wb] + CB, wr=[pbb[6]], inc=(dc == 7))
            yield
        mo = modt[:, l * 72:(l + 1) * 72]
        S.op("dve", lambda e: e.tensor_tensor(out=mo, in0=pbt[6][:, 128:200], in1=vecs[:, l * 128: l * 128 + 72], op=ALU.add),
             rd=[pbb[6]] + CB, wr=[mb])
        coef = [0.5, 1.0, 0.5]
        for s in range(3):
            a = Acf[:, (l * 3 + s) * 8:(l * 3 + s + 1) * 8]
            bb = Bcf[:, (l * 3 + s) * 8:(l * 3 + s + 1) * 8]
            sc = modt[:, l * 72 + s * 24 + 8: l * 72 + s * 24 + 16]
            gt = modt[:, l * 72 + s * 24 + 16: l * 72 + s * 24 + 24]
            npre = vecs[:, l * 128 + 72 + s * 8: l * 128 + 72 + s * 8 + 8]
            npost = vecs[:, l * 128 + 96 + s * 8: l * 128 + 96 + s * 8 + 8]
            S.op("dve", lambda e, a=a, sc=sc, npre=npre: e.scalar_tensor_tensor(
                out=a, in0=sc, scalar=1.0, in1=npre, op0=ALU.add, op1=ALU.mult), rd=[mb] + CB, wr=[mb])
            S.op("dve", lambda e, bb=bb, gt=gt, npost=npost, cf=coef[s]: e.scalar_tensor_tensor(
                out=bb, in0=gt, scalar=cf, in1=npost, op0=ALU.mult, op1=ALU.mult), rd=[mb] + CB, wr=[mb])
        yield

    def run_task(t):
        for _ in t:
            pass

    def rstd_from(ssbank_i, scale):
        S.op("act", lambda e: e.activation(out=sd_t[:], in_=pbt[ssbank_i][:, :], func=AF.Sqrt,
                                           bias=epsc[:, 0:1], scale=scale), rd=[pbb[ssbank_i]] + CB, wr=[sd_b])
        rt, rb = rs_rot.next()
        S.op("dve", lambda e: e.reciprocal(out=rt[:], in_=sd_t[:]), rd=[sd_b], wr=[rb])
        return rt, rb

    def norm_x_to_h(l, s, tg, dst_fn, dst_bufs, ssbank_i):
        mb = B(f"mod{l}")
        tok = slice(tg * 512, (tg + 1) * 512)
        for dc in range(8):
            qt, qb = sq_rot.next()
            S.op("act", lambda e, dc=dc, qt=qt: e.activation(out=qt[:], in_=xT[:, dc, tok], func=AF.Square),
                 rd=[xb[dc][tg]], wr=[qb])
            S.op("pe", lambda e, dc=dc, qt=qt: e.matmul(pbt[ssbank_i][:, :], lhsT=ones_bf[:], rhs=qt[:],
                                                         start=(dc == 0), stop=(dc == 7)),
                 rd=[qb] + CB, wr=[pbb[ssbank_i]], inc=True)
        rt, rb = rstd_from(ssbank_i, 1.0 / 1024.0)
        for dc in range(8):
            tt, tb = t32_rot.next()
            acol = Acf[:, (l * 3 + s) * 8 + dc:(l * 3 + s) * 8 + dc + 1]
            shcol = modt[:, l * 72 + s * 24 + dc: l * 72 + s * 24 + dc + 1]
            S.op("dve", lambda e, dc=dc, tt=tt, acol=acol: e.scalar_tensor_tensor(
                out=tt[:], in0=xT[:, dc, tok], scalar=acol, in1=rt[:], op0=ALU.mult, op1=ALU.mult),
                rd=[xb[dc][tg], rb, mb], wr=[tb])
            S.op("act", lambda e, dc=dc, tt=tt, shcol=shcol: e.activation(
                out=dst_fn(dc), in_=tt[:], func=AF.Identity, bias=shcol, scale=1.0),
                rd=[tb, mb], wr=[dst_bufs[dc]])

    def post_residual(l, s, tg, ybf_fn, ybufs, ssbank_i):
        mb = B(f"mod{l}")
        tok = slice(tg * 512, (tg + 1) * 512)
        rt, rb = rstd_from(ssbank_i, 1.0 / 1024.0)
        for dc in range(8):
            tt, tb = t32_rot.next()
            bcol = Bcf[:, (l * 3 + s) * 8 + dc:(l * 3 + s) * 8 + dc + 1]
            S.op("dve", lambda e, dc=dc, tt=tt, bcol=bcol: e.scalar_tensor_tensor(
                out=tt[:], in0=ybf_fn(dc), scalar=bcol, in1=rt[:], op0=ALU.mult, op1=ALU.mult),
                rd=[ybufs[dc], rb, mb], wr=[tb])
            S.op("dve", lambda e, dc=dc, tt=tt: e.tensor_tensor(out=xT[:, dc, tok], in0=xT[:, dc, tok], in1=tt[:], op=ALU.add),
                 rd=[tb, xb[dc][tg]], wr=[xb[dc][tg]])

    def ffn(l, wi, s):
        with ExitStack() as fs:
            hT = sb("hT", [128, 8, 1024], BF16, fs)
            aT = sb("aT", [128, 22, 1024], BF16, fs)
            sg_rot = Rot([(sb(f"sg{i}", [128, 512], BF16, fs), B(f"sg{i}")) for i in range(2)])
            ysq_rot = Rot([(sb(f"ysq{i}", [128, 512], BF16, fs), B(f"ysq{i}")) for i in range(2)])
            hb = [[B(f"h{dc}_{t}") for t in range(2)] for dc in range(8)]
            ab = [[B(f"a{fc}_{t}") for t in range(2)] for fc in range(22)]
            scoped = [b for r in hb for b in r] + [b for r in ab for b in r] + [x[1] for x in sg_rot.items + ysq_rot.items]
            for ps in range(2):
                if DEBUG_CUT < 99 and ps == 1:
                    break
                wd_st.prefetch()
                for t in range(2):
                    tg = 2 * ps + t
                    norm_x_to_h(l, s, tg, lambda dc, t=t: hT[:, dc, t * 512:(t + 1) * 512], [hb[dc][t] for dc in range(8)], 6 + t)
                if DEBUG_CUT <= 1:
                    break
                for fc in range(22):
                    wt, wb = wgu_st.get()
                    for t in range(2):
                        for gu in range(2):
                            bk = 2 * gu + t
                            for dc in range(8):
                                S.op("pe", lambda e, dc=dc, gu=gu, bk=bk, t=t: e.matmul(
                                    pbt[bk][:, :], lhsT=wt[:, gu * 1024 + dc * 128: gu * 1024 + (dc + 1) * 128],
                                    rhs=hT[:, dc, t * 512:(t + 1) * 512], start=(dc == 0), stop=(dc == 7)),
                                    rd=[wb, hb[dc][t]], wr=[pbb[bk]], inc=(dc == 7))
                        st, sbf = sg_rot.next()
                        S.op("act", lambda e, st=st, t=t: e.activation(out=st[:], in_=pbt[t][:, :], func=AF.Silu),
                             rd=[pbb[t]], wr=[sbf])
                        S.op("dve", lambda e, st=st, t=t, fc=fc: e.tensor_tensor(
                            out=aT[:, fc, t * 512:(t + 1) * 512], in0=st[:], in1=pbt[2 + t][:, :], op=ALU.mult),
                            rd=[sbf, pbb[2 + t]], wr=[ab[fc][t]])
                wgu_st.prefetch()
                if DEBUG_CUT <= 2:
                    break
                for dc in range(8):
                    wt, wb = wd_st.get()
                    for t in range(2):
                        bk = 4 + t
                        for fc in range(22):
                            S.op("pe", lambda e, fc=fc, bk=bk, t=t: e.matmul(
                                pbt[bk][:, :], lhsT=wt[:, fc * 128:(fc + 1) * 128],
                                rhs=aT[:, fc, t * 512:(t + 1) * 512], start=(fc == 0), stop=(fc == 21)),
                                rd=[wb, ab[fc][t]], wr=[pbb[bk]], inc=(fc == 21))
                        yt, yb = ysq_rot.next()
                        S.op("act", lambda e, yt=yt, bk=bk: e.activation(out=yt[:], in_=pbt[bk][:, :], func=AF.Square),
                             rd=[pbb[bk]], wr=[yb])
                        S.op("act", lambda e, bk=bk, dc=dc, t=t: e.activation(
                            out=hT[:, dc, t * 512:(t + 1) * 512], in_=pbt[bk][:, :], func=AF.Copy),
                            rd=[pbb[bk]], wr=[hb[dc][t]])
                        S.op("pe", lambda e, yt=yt, t=t, dc=dc: e.matmul(
                            pbt[6 + t][:, :], lhsT=ones_bf[:], rhs=yt[:], start=(dc == 0), stop=(dc == 7)),
                            rd=[yb] + CB, wr=[pbb[6 + t]], inc=True)
                if DEBUG_CUT <= 3:
                    break
                for t in range(2):
                    post_residual(l, s, 2 * ps + t, lambda dc, t=t: hT[:, dc, t * 512:(t + 1) * 512],
                                  [hb[dc][t] for dc in range(8)], 6 + t)
            S.release(scoped)

    send1_b = [B(f"send1_{c}") for c in range(12)]
    recv1_b = [B(f"recv1_{c}") for c in range(12)]
    send2_b = [B(f"send2_{c}") for c in range(4)]
    recv2_b = [B(f"recv2_{c}") for c in range(4)]
    s1 = send1.ap()
    s1q = s1.rearrange("(g c k d) t -> g c k d t", g=4, c=3, k=4, d=64)
    s1v = s1.rearrange("(g c k d) (pl t m) -> g c k (d pl) t m", g=4, c=3, k=4, d=64, pl=2, t=16, m=64)
    r1g = recv1.ap().rearrange("(g c x) t -> g c x t", g=4, c=3)
    m1q = mine1.ap().rearrange("(c r k d) t -> c k d r t", c=3, r=4, k=4, d=64)
    m1v = mine1.ap().rearrange("(c r k d) (pl x) -> c k (d pl) r x", c=3, r=4, k=4, d=64, pl=2)
    s2 = send2.ap()
    m2 = mine2.ap().rearrange("(c r d) t -> c d r t", c=4, r=4, d=64)
    mine1_b = [B(f"mine1_{c}") for c in range(3)]
    mine2_b = [B(f"mine2_{c}") for c in range(4)]

    def ld_q(ct, k):
        return m1q[ct, k, :, :, :]

    def ld_v(ct, k):
        return m1v[ct, k, :, :, :]

    def stage_copy(ct):
        S.dma("sp", [(mine1.ap()[ct * 1024:(ct + 1) * 1024, :],
                      r1g[bass.ds(gidx, 1), ct, :, :].rearrange("o x t -> (o x) t"))],
              rd=[recv1_b[g_ * 3 + ct] for g_ in range(4)], wr=[mine1_b[ct]], owner=mine1_b[ct])

    def gather1():
        for ct in range(3):
            for g_ in range(4):
                c = g_ * 3 + ct
                S.allgather(s1[c * 256:(c + 1) * 256, :], recv1.ap()[c * 1024:(c + 1) * 1024, :], rd=[], wr=[send1_b[c], recv1_b[c]], name=f"g1_{c}")
        stage_copy(0)

    def gather2(c):
        S.allgather(s2[c * 64:(c + 1) * 64, :], recv2.ap()[c * 256:(c + 1) * 256, :], rd=[], wr=[send2_b[c], recv2_b[c]], name=f"g2_{c}")
        S.dma("sp", [(mine2.ap()[c * 256:(c + 1) * 256, :],
                      recv2.ap()[c * 256:(c + 1) * 256, :].rearrange("x (g t) -> x g t", g=4)[:, bass.ds(gidx, 1), :].rearrange("x o t -> x (o t)"))],
              rd=[recv2_b[c]], wr=[mine2_b[c]], owner=mine2_b[c])

    def normalize_store(obank_i, exps_col, slot, tgi, ost_rot, bcbank_i, rden_t, rden_b, of_t, of_b):
        ob = pbb[obank_i]
        if exps_col is not None:
            S.op("dve", lambda e: e.tensor_scalar(out=rden_t[64:65, :], in0=pbt[obank_i][64:65, :], scalar1=exps_col,
                                                  scalar2=None, op0=ALU.add), rd=[ob] + CB, wr=[rden_b])
        else:
            S.op("dve", lambda e: e.tensor_copy(out=rden_t[64:65, :], in_=pbt[obank_i][64:65, :]), rd=[ob], wr=[rden_b])
        S.op("dve", lambda e: e.reciprocal(out=rden_t[64:65, :], in_=rden_t[64:65, :]), rd=[rden_b], wr=[rden_b])
        S.op("pe", lambda e: e.matmul(pbt[bcbank_i][0:64, :], lhsT=onesf[64:65, 0:64], rhs=rden_t[64:65, :],
                                      start=True, stop=True), rd=[rden_b] + CB, wr=[pbb[bcbank_i]], inc=True)
        S.op("act", lambda e: e.activation(out=of_t[0:64, :], in_=pbt[obank_i][0:64, :], func=AF.Copy), rd=[ob], wr=[of_b])
        ot, obf = ost_rot.next()
        S.op("dve", lambda e: e.tensor_tensor(out=ot[0:64, :], in0=of_t[0:64, :], in1=pbt[bcbank_i][0:64, :], op=ALU.mult),
             rd=[of_b, pbb[bcbank_i]], wr=[obf])
        S.dma("sp", [(s2[slot * 64:(slot + 1) * 64, tgi * 512:(tgi + 1) * 512], ot[0:64, :])],
              rd=[obf, send2_b[slot]], wr=[], owner=obf)

    def attention(l, bg):
        s = 1
        mb = B(f"mod{l}")
        with ExitStack() as fs:
            hT4 = sb("hT4", [128, 8, 1024], BF16, fs)
            h4b = [[B(f"h4_{dc}_{t}") for t in range(2)] for dc in range(8)]
            wv = sb("wv", [128, 6144], BF16, fs)
            wvb = B("wv")
            bv = sb("bv", [128, 768], F32, fs)
            bvb = B("bv")
            wqk_slots = [(sb(f"wqk{i}", [128, 3584], BF16, fs), B(f"wqk{i}")) for i in range(2)]
            stq_rot = Rot([(sb(f"stq{i}", [64, 7, 512], BF16, fs), B(f"stq{i}")) for i in range(2)])
            stv_rot = Rot([(sb(f"stv{i}", [128, 12, 4, 64], BF16, fs), B(f"stv{i}")) for i in range(2)])
            scoped = [b for r in h4b for b in r] + [wvb, bvb] + [x[1] for x in wqk_slots + stq_rot.items + stv_rot.items]
            S.dma("pool", [(wv[:], wv_d[l * 128:(l + 1) * 128, :])], rd=[], wr=[wvb], owner=wvb)
            S.dma("sp", [(bv[:], bv_d[l * 128:(l + 1) * 128, :])], rd=[], wr=[bvb], owner=bvb)
            wqk_st = WStream(S, wqk_slots, [wqk_d[(l * 4 + gq) * 128:(l * 4 + gq + 1) * 128, :] for hf in range(2) for gq in range(4)])
            wqk_st.prefetch()
            for hf in range(2):
                for t in range(2):
                    tg = 2 * hf + t
                    norm_x_to_h(l, s, tg, lambda dc, t=t: hT4[:, dc, t * 512:(t + 1) * 512],
                                [h4b[dc][t] for dc in range(8)], 6 + t)
                for t in range(2):
                    tg = 2 * hf + t
                    vt, vb_ = stv_rot.next()
                    for tl in range(4):
                        c0 = t * 512 + tl * 128
                        for (bk, lo, hi) in ((0 + 2 * (tl % 2), 0, 512), (1 + 2 * (tl % 2), 512, 768)):
                            for dc in range(8):
                                S.op("pe", lambda e, dc=dc, bk=bk, lo=lo, hi=hi, c0=c0: e.matmul(
                                    pbt[bk][:, 0:hi - lo], lhsT=hT4[:, dc, c0:c0 + 128], rhs=wv[:, dc * 768 + lo: dc * 768 + hi],
                                    start=(dc == 0), stop=(dc == 7)), rd=[h4b[dc][t], wvb], wr=[pbb[bk]], inc=(dc == 7))
                            nvs = (hi - lo) // 64
                            S.op("dve", lambda e, bk=bk, lo=lo, hi=hi, nvs=nvs, tl=tl, vt=vt: e.tensor_tensor(
                                out=vt[:, lo // 64: lo // 64 + nvs, tl, :],
                                in0=pbt[bk][:, 0:hi - lo].rearrange("p (v m) -> p v m", m=64),
                                in1=bv[:, lo:hi].rearrange("p (v m) -> p v m", m=64), op=ALU.add),
                                rd=[pbb[bk], bvb], wr=[vb_])
                    vt5 = vt[:, :, :, :].rearrange("p (g j) t m -> p g j t m", j=3)
                    S.dma("sp", [(s1v[:, cj, kj, :, tg * 4:(tg + 1) * 4, :].rearrange("g p t m -> p g t m"), vt5[:, :, j, :, :])
                                 for (j, cj, kj) in ((0, 0, 3), (1, 1, 2), (2, 2, 2))],
                          rd=[vb_] + send1_b, wr=[], owner=vb_)
                for gq in range(4):
                    wt, wb = wqk_st.get()
                    for t in range(2):
                        tg = 2 * hf + t
                        qt, qb = stq_rot.next()
                        for sl in range(7):
                            bk = 4 + (sl % 2)
                            for dc in range(8):
                                S.op("pe", lambda e, dc=dc, sl=sl, bk=bk, t=t: e.matmul(
                                    pbt[bk][0:64, :], lhsT=wt[:, (sl * 8 + dc) * 64:(sl * 8 + dc + 1) * 64],
                                    rhs=hT4[:, dc, t * 512:(t + 1) * 512], start=(dc == 0), stop=(dc == 7)),
                                    rd=[wb, h4b[dc][t]], wr=[pbb[bk]], inc=(dc == 7))
                            bcol = bqk[:, l * 28 + gq * 7 + sl: l * 28 + gq * 7 + sl + 1]
                            S.op("act", lambda e, sl=sl, bk=bk, qt=qt, bcol=bcol: e.activation(
                                out=qt[:, sl, :], in_=pbt[bk][0:64, :], func=AF.Identity, bias=bcol, scale=1.0),
                                rd=[pbb[bk]] + CB, wr=[qb])
                        cs = slice(tg * 512, (tg + 1) * 512)
                        S.dma("sp", [(s1q[gq, 0, 0:3, :, cs].rearrange("k d t -> d k t"), qt[:, 0:3, :]),
                                     (s1q[gq, 1, 0:2, :, cs].rearrange("k d t -> d k t"), qt[:, 3:5, :]),
                                     (s1q[gq, 2, 0:2, :, cs].rearrange("k d t -> d k t"), qt[:, 5:7, :])],
                              rd=[qb] + send1_b[gq * 3:gq * 3 + 3], wr=[], owner=qb)
            S.release(scoped)
        if DEBUG_CUT == 11:
            return
        gather1()
        if DEBUG_CUT == 12:
            stage_copy(1)
            stage_copy(2)
            S.wait_all("sp", mine1_b)
            return

        with ExitStack() as fs:
            QTs = [sb(f"QTs{i}", [64, 8192], BF16, fs) for i in range(2)]
            KTs = sb("KTs", [64, 128 + 8192], BF16, fs)
            Vs = sb("Vs", [128, 65, 65], BF16, fs)
            vst = sb("vst", [128, 4, 1024], BF16, fs)
            qsb, ksb, vsb, vstb = B("QTs"), B("KTs"), B("Vs"), B("vst")
            P_rot = Rot([(sb(f"Ps{i}", [128, 256], BF16, fs), B(f"Ps{i}")) for i in range(3)])
            sb_rot = Rot([(sb(f"sbs{i}", [128, 256], F32, fs), B(f"sbs{i}")) for i in range(2)])
            ost_rot = Rot([(sb(f"ost{i}", [64, 512], BF16, fs), B(f"ost{i}")) for i in range(2)])
            rden_t, rden_b = sb("rden", [65, 512], F32, fs), B("rden")
            of_t, of_b = sb("of", [64, 512], F32, fs), B("of")
            scoped = [qsb, ksb, vsb, vstb, rden_b, of_b] + [x[1] for x in P_rot.items + sb_rot.items + ost_rot.items]
            S.dma("sp", [(QTs[i][:, :].rearrange("d (r t) -> d r t", r=4), ld_q(0, i)) for i in range(2)],
                  rd=[mine1_b[0]], wr=[qsb], owner=qsb)
            S.op("dve", lambda e: e.memset(KTs[:, 0:128], 0.0), wr=[ksb])
            S.dma("sp", [(KTs[:, 128:].rearrange("d (r t) -> d r t", r=4), ld_q(0, 2))],
                  rd=[mine1_b[0]], wr=[ksb], owner=ksb)
            S.dma("sp", [(vst[:, :, :], ld_v(0, 3))], rd=[mine1_b[0]], wr=[vstb], owner=vstb)
            S.op("dve", lambda e: e.memset(Vs[:, :, 64:65], 1.0), wr=[vsb])
            S.op("dve", lambda e: e.memset(Vs[:, 0, 0:64], 0.0), wr=[vsb])
            S.op("dve", lambda e: e.tensor_copy(out=Vs[:, 1:65, 0:64], in_=vst[:, :, :].rearrange("p r (t m) -> p (r t) m", m=64)),
                 rd=[vstb], wr=[vsb])
            for i in range(2):
                spt = {}

                def swA(T):
                    sbk = T % 3
                    for half in range(2):
                        S.op("pe", lambda e, half=half: e.matmul(
                            pbt[sbk][:, half * 128:(half + 1) * 128], lhsT=KTs[:, (T + half) * 128:(T + half + 1) * 128],
                            rhs=QTs[i][:, T * 128:(T + 1) * 128], start=True, stop=True),
                            rd=[ksb, qsb], wr=[pbb[sbk]], inc=(half == 1))
                    st, stb = sb_rot.next()
                    var = i * 2 + (1 if T == 0 else 0)
                    S.op("dve", lambda e: e.scalar_tensor_tensor(
                        out=st[:], in0=pbt[sbk][:, 0:256], scalar=0.125, in1=gswa[:, var * 256:(var + 1) * 256],
                        op0=ALU.mult, op1=ALU.add), rd=[pbb[sbk]] + CB, wr=[stb])
                    pt, ptb = P_rot.next()
                    spt[T] = (pt, ptb)
                    S.op("act", lambda e: e.activation(out=pt[:], in_=st[:], func=AF.Exp), rd=[stb], wr=[ptb])

                def swB(T):
                    tgi, qq = T // 4, T % 4
                    obk = 4 + (tgi % 2)
                    pt, ptb = spt.pop(T)
                    for half in range(2):
                        S.op("pe", lambda e, half=half: e.matmul(
                            pbt[obk][0:65, qq * 128:(qq + 1) * 128], lhsT=Vs[:, T + half, 0:65],
                            rhs=pt[:, half * 128:(half + 1) * 128], start=(half == 0), stop=(half == 1)),
                            rd=[vsb, ptb], wr=[pbb[obk]], inc=(half == 1))
                    if qq == 3:
                        normalize_store(obk, esnk[64:65, l * 2 + i: l * 2 + i + 1], i, tgi, ost_rot, 6 + (tgi % 2),
                                        rden_t, rden_b, of_t, of_b)

                swA(0)
                for T in range(64):
                    if T + 1 < 64:
                        swA(T + 1)
                    swB(T)
                gather2(i)
            S.release(scoped)

        if DEBUG_CUT == 13:
            return
        for i in range(2):
            with ExitStack() as fs:
                QTa = sb("QTa", [96, 8192], BF16, fs)
                KTa = sb("KTa", [96, 8192], BF16, fs)
                Va = sb("Va", [128, 64, 65], BF16, fs)
                vst = sb("vstm", [128, 4, 1024], BF16, fs)
                qab, kab, kib, vab, vstb = B("QTa"), B("KTa"), B("KTi"), B("Va"), B("vstm")
                qmb = [B(f"qm{t}") for t in range(16)]
                km32 = sb("km32", [64, 32], F32, fs)
                kmd = sb("kmd", [64, 32], F32, fs)
                kmh = sb("kmh", [64, 32], BF16, fs)
                kml = sb("kml", [64, 32], BF16, fs)
                kmb = B("km")
                gb_rot = Rot([(sb(f"gb{k}", [128, 32], F32, fs), B(f"gb{k}")) for k in range(2)])
                t8_rot = Rot([(sb(f"t8{k}", [128, 8], F32, fs), B(f"t8{k}")) for k in range(2)])
                mbt_rot = Rot([(sb(f"mbt{k}", [128, 4, 96], F32, fs), B(f"mbt{k}")) for k in range(2)])
                P_rot = Rot([(sb(f"Pm{k}", [128, 512], BF16, fs), B(f"Pm{k}")) for k in range(3)])
                sb_rot = Rot([(sb(f"sbm{k}", [128, 512], F32, fs), B(f"sbm{k}")) for k in range(2)])
                ost_rot = Rot([(sb(f"ostm{k}", [64, 512], BF16, fs), B(f"ostm{k}")) for k in range(2)])
                rden_t, rden_b = sb("rdenm", [65, 512], F32, fs), B("rdenm")
                of_t, of_b = sb("ofm", [64, 512], F32, fs), B("ofm")
                scoped = ([qab, kab, kib, vab, vstb, kmb, rden_b, of_b] + qmb +
                          [x[1] for x in gb_rot.items + t8_rot.items + mbt_rot.items + P_rot.items + sb_rot.items + ost_rot.items])
                stage_copy(1 + i)
                S.dma("sp", [(QTa[0:64, :].rearrange("d (r t) -> d r t", r=4), ld_q(1 + i, 0))],
                      rd=[mine1_b[1 + i]], wr=[qab], owner=qab)
                S.dma("sp", [(KTa[0:64, :].rearrange("d (r t) -> d r t", r=4), ld_q(1 + i, 1))],
                      rd=[mine1_b[1 + i]], wr=[kab], owner=kab)
                S.dma("pool", [(KTa[64:96, :], blkind_d[:, :])], rd=[], wr=[kib], owner=kib)
                S.dma("sp", [(vst[:, :, :], ld_v(1 + i, 2))], rd=[mine1_b[1 + i]], wr=[vstb], owner=vstb)
                S.op("dve", lambda e: e.memset(Va[:, :, 64:65], 1.0), wr=[vab])
                S.op("dve", lambda e: e.tensor_copy(out=Va[:, :, 0:64], in_=vst[:, :, :].rearrange("p r (t m) -> p (r t) m", m=64)),
                     rd=[vstb], wr=[vab])
                S.op("dve", lambda e: e.tensor_reduce(out=km32[:, :], in_=KTa[0:64, :].rearrange("d (b k) -> d b k", k=256),
                                                      axis=AX.X, op=ALU.add), rd=[kab], wr=[kmb])
                S.op("dve", lambda e: e.tensor_copy(out=kmh[:, :], in_=km32[:, :]), rd=[kmb], wr=[kmb])
                S.op("dve", lambda e: e.tensor_tensor(out=kmd[:, :], in0=km32[:, :], in1=kmh[:, :], op=ALU.subtract), rd=[kmb], wr=[kmb])
                S.op("dve", lambda e: e.tensor_copy(out=kml[:, :], in_=kmd[:, :]), rd=[kmb], wr=[kmb])
                mts = {}

                def g1(tgi):
                    mt, mtb = mbt_rot.next()
                    mts[tgi] = (mt, mtb)
                    for qq in range(4):
                        T = tgi * 4 + qq
                        cur = T // 2
                        if cur >= 3:
                            S.op("pe", lambda e, T=T, qq=qq: e.matmul(pbt[6][:, qq * 32:(qq + 1) * 32], lhsT=QTa[0:64, T * 128:(T + 1) * 128],
                                                                      rhs=kmh[:, :], start=True, stop=False), rd=[qab, kmb], wr=[pbb[6]], inc=False)
                            S.op("pe", lambda e, T=T, qq=qq: e.matmul(pbt[6][:, qq * 32:(qq + 1) * 32], lhsT=QTa[0:64, T * 128:(T + 1) * 128],
                                                                      rhs=kml[:, :], start=False, stop=True), rd=[qab, kmb], wr=[pbb[6]], inc=True)
                            gt, gtb = gb_rot.next()
                            S.op("dve", lambda e, gt=gt, qq=qq, cur=cur: e.tensor_tensor(
                                out=gt[:], in0=pbt[6][:, qq * 32:(qq + 1) * 32], in1=stair[:, 32 - cur:64 - cur], op=ALU.add),
                                rd=[pbb[6]] + CB, wr=[gtb])
                            t8, t8b = t8_rot.next()
                            S.op("dve", lambda e, gt=gt, t8=t8: e.max(out=t8[:], in_=gt[:]), rd=[gtb], wr=[t8b])
                            S.op("dve", lambda e, gt=gt, t8=t8, mt=mt, qq=qq: e.tensor_scalar(
                                out=mt[:, qq, 64:96], in0=gt[:], scalar1=t8[:, 2:3], scalar2=1.0, op0=ALU.is_ge, op1=ALU.subtract),
                                rd=[gtb, t8b], wr=[mtb])
                            S.op("dve", lambda e, mt=mt, qq=qq, cur=cur: e.memset(mt[:, qq, 64 + cur:65 + cur], 0.0), wr=[mtb])
                        else:
                            S.op("dve", lambda e, mt=mt, qq=qq, cur=cur: e.tensor_copy(
                                out=mt[:, qq, 64:96], in_=stair[:, 64 + 31 - cur:64 + 63 - cur]), rd=CB, wr=[mtb])

                def g2(tgi):
                    mt, mtb = mts.pop(tgi)
                    for qq in range(4):
                        S.op("pe", lambda e, mt=mt, qq=qq: e.transpose(out=pbt[7][0:96, qq * 128:(qq + 1) * 128], in_=mt[:, qq, :],
                                                                       identity=ident[:, :]), rd=[mtb] + CB, wr=[pbb[7]], inc=True)
                    S.op("act", lambda e, tgi=tgi: e.activation(out=QTa[64:96, tgi * 512:(tgi + 1) * 512], in_=pbt[7][64:96, :], func=AF.Copy),
                         rd=[pbb[7]], wr=[qmb[tgi]])

                def main(tgi):
                    obk = 4 + (tgi % 2)
                    nkt = 4 * tgi + 4
                    pts = {}

                    def stA(kt):
                        sbk = kt % 3
                        S.op("pe", lambda e: e.matmul(
                            pbt[sbk][:, :], lhsT=KTa[0:96, kt * 128:(kt + 1) * 128], rhs=QTa[0:96, tgi * 512:(tgi + 1) * 512],
                            start=True, stop=True), rd=[kab, kib, qab, qmb[tgi]], wr=[pbb[sbk]], inc=True)
                        pt, ptb = P_rot.next()
                        pts[kt] = (pt, ptb)
                        rel = kt - 4 * tgi
                        if rel >= -2:
                            j0 = i * 1152 + 384 - 128 * rel
                            st, stb = sb_rot.next()
                            S.op("dve", lambda e: e.scalar_tensor_tensor(
                                out=st[:], in0=pbt[sbk][:, :], scalar=0.125, in1=gmoba[:, j0:j0 + 512], op0=ALU.mult, op1=ALU.add),
                                rd=[pbb[sbk]] + CB, wr=[stb])
                            S.op("act", lambda e: e.activation(out=pt[:], in_=st[:], func=AF.Exp), rd=[stb], wr=[ptb])
                        else:
                            S.op("act", lambda e: e.activation(out=pt[:], in_=pbt[sbk][:, :], func=AF.Exp,
                                                               bias=cfar[:, i:i + 1], scale=0.125), rd=[pbb[sbk]] + CB, wr=[ptb])

                    def stB(kt):
                        pt, ptb = pts.pop(kt)
                        S.op("pe", lambda e: e.matmul(
                            pbt[obk][0:65, :], lhsT=Va[:, kt, 0:65], rhs=pt[:], start=(kt == 0), stop=(kt == nkt - 1)),
                            rd=[vab, ptb], wr=[pbb[obk]], inc=(kt == nkt - 1))

                    stA(0)
                    for kt in range(nkt):
                        if kt + 1 < nkt:
                            stA(kt + 1)
                        stB(kt)
                    normalize_store(obk, None, 2 + i, tgi, ost_rot, 3, rden_t, rden_b, of_t, of_b)
                    if bg is not None:
                        next(bg, None)
                        next(bg, None)

                g1(0)
                g2(0)
                for tgi in range(16):
                    if tgi + 1 < 16:
                        g1(tgi + 1)
                    main(tgi)
                    if tgi + 1 < 16:
                        g2(tgi + 1)
                gather2(2 + i)
                S.release(scoped)
        if bg is not None:
            run_task(bg)
        if DEBUG_CUT == 14:
            return

        with ExitStack() as fs:
            wo = sb("wo", [128, 8192], BF16, fs)
            wob = B("wo")
            OT_rot = Rot([(sb(f"OT{k}", [128, 8, 512], BF16, fs), B(f"OT{k}")) for k in range(2)])
            osq = sb("osq", [128, 8, 512], BF16, fs)
            osqb = B("osq")
            OnT = sb("OnT", [128, 8, 512], BF16, fs)
            onb = [B(f"on{k}") for k in range(8)]
            ybf = sb("ybf", [128, 8, 512], BF16, fs)
            ybb = [B(f"yb{k}") for k in range(8)]
            ysq_rot = Rot([(sb(f"ysqo{k}", [128, 512], BF16, fs), B(f"ysqo{k}")) for k in range(2)])
            rsa_t, rsa_b = sb("rsa", [128, 512], F32, fs), B("rsa")
            scoped = [wob, osqb, rsa_b] + onb + ybb + [x[1] for x in OT_rot.items + ysq_rot.items]
            S.dma("pool", [(wo[:], wout_d[l * 128:(l + 1) * 128, :])], rd=[], wr=[wob], owner=wob)
            for tg in range(4):
                ot, otb = OT_rot.next()
                S.dma("sp", [(ot[:, :, :].rearrange("p (r par) t -> p r par t", par=2)[(sl % 2) * 64:(sl % 2) * 64 + 64, :, sl // 2, :], m2[sl, :, :, tg * 512:(tg + 1) * 512])
                             for sl in range(4)], rd=mine2_b, wr=[otb], owner=otb)
                S.op("dve", lambda e, ot=ot: e.tensor_tensor(out=osq[:, :, :], in0=ot[:, :, :], in1=ot[:, :, :], op=ALU.mult),
                     rd=[otb], wr=[osqb])
                for par in range(2):
                    for k in range(4):
                        kc = 2 * k + par
                        S.op("pe", lambda e, kc=kc, k=k, par=par: e.matmul(pbt[6 + par][:, :], lhsT=ones_bf[:], rhs=osq[:, kc, :],
                                                                         start=(k == 0), stop=(k == 3)),
                             rd=[osqb] + CB, wr=[pbb[6 + par]], inc=(k == 3))
                S.op("act", lambda e: e.activation(out=sd_t[:], in_=pbt[6][:, :], func=AF.Sqrt, bias=epsc[:, 0:1], scale=1.0 / 512.0),
                     rd=[pbb[6]] + CB, wr=[sd_b])
                S.op("dve", lambda e: e.reciprocal(out=rsa_t[:], in_=sd_t[:]), rd=[sd_b], wr=[rsa_b])
                rbt, rbb = rstd_from(7, 1.0 / 512.0)
                for kc in range(8):
                    rr, rrb = (rsa_t, rsa_b) if kc % 2 == 0 else (rbt, rbb)
                    gcol = vecs[:, l * 128 + 120 + kc: l * 128 + 121 + kc]
                    S.op("dve", lambda e, kc=kc, rr=rr, gcol=gcol, ot=ot: e.scalar_tensor_tensor(
                        out=OnT[:, kc, :], in0=ot[:, kc, :], scalar=gcol, in1=rr[:], op0=ALU.mult, op1=ALU.mult),
                        rd=[otb, rrb] + CB, wr=[onb[kc]])
                for dm in range(8):
                    bk = 4 + (dm % 2)
                    for kc in range(8):
                        S.op("pe", lambda e, kc=kc, dm=dm, bk=bk: e.matmul(
                            pbt[bk][:, :], lhsT=wo[:, kc * 1024 + dm * 128: kc * 1024 + (dm + 1) * 128], rhs=OnT[:, kc, :],
                            start=(kc == 0), stop=(kc == 7)), rd=[wob, onb[kc]], wr=[pbb[bk]], inc=(kc == 7))
                    yt, yb = ysq_rot.next()
                    S.op("act", lambda e, yt=yt, bk=bk: e.activation(out=yt[:], in_=pbt[bk][:, :], func=AF.Square), rd=[pbb[bk]], wr=[yb])
                    S.op("act", lambda e, dm=dm, bk=bk: e.activation(out=ybf[:, dm, :], in_=pbt[bk][:, :], func=AF.Copy),
                         rd=[pbb[bk]], wr=[ybb[dm]])
                    S.op("pe", lambda e, yt=yt, dm=dm: e.matmul(pbt[3][:, :], lhsT=ones_bf[:], rhs=yt[:], start=(dm == 0), stop=(dm == 7)),
                         rd=[yb] + CB, wr=[pbb[3]], inc=True)
                post_residual(l, s, tg, lambda dc: ybf[:, dc, :], ybb, 3)
            S.release(scoped)

    with ExitStack() as zs:
        zt = sb("zpad", [64, 2048], BF16, zs)
        zb = B("zpad")
        S.op("dve", lambda e: e.memset(zt[:], 0.0), wr=[zb])
        S.dma("sp", [(s1q[g_, ct, 3, :, :], zt[:]) for g_ in range(4) for ct in (1, 2)],
              rd=[zb] + send1_b, wr=[], owner=zb)
        S.release([zb])
    run_task(mod_task(0))
    sub = 0
    for l in range(L):
        if sub < nsub:
            ffn(l, 0, 0)
            sub += 1
        if sub < nsub:
            bg = mod_task(l + 1) if (l + 1 < L and 3 * (l + 1) < nsub) else None
            attention(l, bg)
            sub += 1
        elif l + 1 < L:
            pass
        if sub < nsub:
            ffn(l, 1, 2)
            sub += 1
    allx = [b for row in xb for b in row]
    outb = B("outb")
    S.dma("sp", [(out_d[dc * 128:(dc + 1) * 128, :], xT[:, dc, :]) for dc in range(8)], rd=allx, wr=[outb], owner=outb)
    S.wait_all("sp", [outb])
    es.close()
    return nc


def _t5_bucket_np(dist):
    n = np.maximum(dist, 0)
    nf = np.maximum(n, 1).astype(np.float32)
    large = 16 + (np.log(nf / np.float32(16)) / np.float32(math.log(128 / 16)) * np.float32(16)).astype(np.int32)
    large = np.minimum(large, 31)
    return np.where(n < 16, n, large)


def _slot_cols():
    qk, v = [], []
    for g in range(4):
        qk += [64 * (2 * g), 64 * (2 * g + 1), 512 + 64 * (g // 2), 768 + 64 * (2 * g), 1280 + 64 * (2 * g),
               768 + 64 * (2 * g + 1), 1280 + 64 * (2 * g + 1)]
        v += [640 + 64 * (g // 2), 1792 + 64 * (2 * g), 1792 + 64 * (2 * g + 1)]
    return qk, v


def prep_inputs(inp, L=DEPTH, l0=0, x=None):
    f = np.float32
    x = np.asarray(inp["x"], f) if x is None else x
    c = np.asarray(inp["c"], f)
    rel = np.asarray(inp["rel_bias"], f)
    ada_w = np.asarray(inp["ada_w"][l0:l0 + L], f)
    ada_b = np.asarray(inp["ada_b"][l0:l0 + L], f)
    npre = np.asarray(inp["norm_pre"][l0:l0 + L], f)
    npost = np.asarray(inp["norm_post"][l0:l0 + L], f)
    wg = np.asarray(inp["ffn_w_gate"][l0:l0 + L], f)
    wu = np.asarray(inp["ffn_w_up"][l0:l0 + L], f)
    wdn = np.asarray(inp["ffn_w_down"][l0:l0 + L], f)
    w_in = np.asarray(inp["mix_w_in"][l0:l0 + L], f)
    b_in = np.asarray(inp["mix_b_in"][l0:l0 + L], f)
    w_out = np.asarray(inp["mix_w_out"][l0:l0 + L], f)
    sinks = np.asarray(inp["attn_sinks"][l0:l0 + L], f)
    gain = np.asarray(inp["group_gain"][l0:l0 + L], f)

    g6 = wg.reshape(L, 2, 8, 128, 22, 128)
    u6 = wu.reshape(L, 2, 8, 128, 22, 128)
    gu = np.stack([g6, u6], axis=2)
    wgu = np.ascontiguousarray(gu.transpose(0, 1, 5, 4, 2, 3, 6)).reshape(L * 2 * 22 * 128, 2048)
    d6 = wdn.reshape(L, 2, 22, 128, 8, 128)
    wd = np.ascontiguousarray(d6.transpose(0, 1, 4, 3, 2, 5)).reshape(L * 2 * 8 * 128, 2816)
    qk_cols, v_cols = _slot_cols()
    qk_idx = np.concatenate([np.arange(cb, cb + 64) for cb in qk_cols])
    v_idx = np.concatenate([np.arange(cb, cb + 64) for cb in v_cols])
    wq = w_in[:, :, qk_idx].reshape(L, 8, 128, 4, 7, 64)
    wqk = np.ascontiguousarray(wq.transpose(0, 3, 2, 4, 1, 5)).reshape(L * 4 * 128, 3584)
    wvv = w_in[:, :, v_idx].reshape(L, 8, 128, 768)
    wv = np.ascontiguousarray(wvv.transpose(0, 2, 1, 3)).reshape(L * 128, 6144)
    bqk = np.ascontiguousarray(b_in[:, qk_idx].reshape(L, 28, 64).transpose(2, 0, 1)).reshape(64, L * 28)
    bv = np.ascontiguousarray(np.broadcast_to(b_in[:, None, v_idx], (L, 128, 768))).reshape(L * 128, 768)
    rowperm = np.concatenate([np.concatenate([np.arange(128 * g, 128 * g + 128), np.arange(512 + 128 * g, 512 + 128 * g + 128)])
                              for g in range(4)])
    wo = w_out[:, rowperm, :].reshape(L, 8, 128, 1024)
    wout = np.ascontiguousarray(wo.transpose(0, 2, 1, 3)).reshape(L * 128, 8192)
    aw = ada_w.reshape(L, 8, 128, 36, 256)
    adaw = np.ascontiguousarray(aw.transpose(0, 3, 2, 1, 4)).reshape(L * 36 * 128, 2048)
    vecs = np.zeros((128, L * 128), f)
    for l in range(L):
        vecs[:, l * 128: l * 128 + 72] = ada_b[l].reshape(72, 128).T
        vecs[:, l * 128 + 72: l * 128 + 96] = npre[l].reshape(24, 128).T
        vecs[:, l * 128 + 96: l * 128 + 120] = npost[l].reshape(24, 128).T
        vecs[:, l * 128 + 120: l * 128 + 128] = gain[l][rowperm].reshape(8, 128).T
    ident = np.eye(128, dtype=f)
    blkind = np.zeros((32, 8192), f)
    for b in range(32):
        blkind[b, b * 256:(b + 1) * 256] = MASKV
    stair = np.zeros((128, 128), f)
    stair[:, 32:64] = -1e30
    stair[:, 96:128] = -1.0

    kk = np.arange(128)[:, None]
    shared = dict(wgu=wgu, wd=wd, wqk=wqk, wv=wv, bqk=bqk, bv=bv, wout=wout, adaw=adaw, vecs=vecs,
                  ident=ident, blkind=blkind, stair=stair)
    in_maps = []
    for core in range(8):
        bt, g = core // 4, core % 4
        m = dict(shared)
        m["xT"] = np.ascontiguousarray(x[bt, g * 2048:(g + 1) * 2048, :].T)
        m["cT"] = np.ascontiguousarray(c[bt].reshape(8, 128).T)
        gm = np.zeros((128, 2 * 1152), f)
        cf = np.zeros((128, 2), f)
        for i in range(2):
            h = 8 + 2 * g + i
            d = np.arange(1152)[None, :] - 384 - kk
            val = rel[_t5_bucket_np(d), h]
            gm[:, i * 1152:(i + 1) * 1152] = np.where(d >= 0, val, f(-30000.0))
            cf[:, i] = rel[31, h]
        m["gmoba"] = gm
        m["cfar"] = cf
        gs = np.zeros((128, 4 * 256), f)
        qq = np.arange(128)[None, :]
        for i in range(2):
            h = 2 * g + i
            d0 = qq + 128 - kk
            d1 = qq - kk
            a0 = np.where((d0 >= 0) & (d0 < 128), rel[_t5_bucket_np(d0), h], f(-30000.0))
            a1 = np.where((d1 >= 0) & (d1 < 128), rel[_t5_bucket_np(d1), h], f(-30000.0))
            gs[:, (i * 2) * 256:(i * 2) * 256 + 128] = a0
            gs[:, (i * 2) * 256 + 128:(i * 2 + 1) * 256] = a1
            gs[:, (i * 2 + 1) * 256:(i * 2 + 1) * 256 + 128] = f(-30000.0)
            gs[:, (i * 2 + 1) * 256 + 128:(i * 2 + 2) * 256] = a1
        m["gswa"] = gs
        m["snk"] = np.ascontiguousarray(sinks[:, 2 * g:2 * g + 2].reshape(1, L * 2))
        in_maps.append(m)
    return in_maps


_NC_CACHE = {}


def run(inputs, L=DEPTH, nsub=None, l0=0, x=None):
    key = (L, nsub)
    if key not in _NC_CACHE:
        _NC_CACHE[key] = build(L, nsub)
    nc = _NC_CACHE[key]
    in_maps = prep_inputs(inputs, L, l0, x)
    res = run_bass_kernel_spmd(nc, in_maps, core_ids=list(range(8)))
    out = np.zeros((2, 8192, 1024), np.float32)
    for core in range(8):
        bt, g = core // 4, core % 4
        out[bt, g * 2048:(g + 1) * 2048, :] = np.asarray(res.results[core]["outT"], np.float32).T
    return out


LAYERS_PER_LAUNCH = 4


def kernel(**inputs):
    x = None
    for l0 in range(0, DEPTH, LAYERS_PER_LAUNCH):
        x = run(inputs, LAYERS_PER_LAUNCH, None, l0, x)
    return x
```
